# Optimizing a Trainium2 kernel written in Bass

```python
import math
import jax
import jax.numpy as jnp
from jax import lax
import numpy as np

D_MODEL = 1024
BATCH = 8
SEQ = 2048
DEPTH = 2

GRID_W = 64
CTX_LEN = 256
NORM_EPS = 1e-6
ROPE_BASE = 10000.0

RW_HEADS = 6
RW_HEAD_DIM = 64
RW_WIDTH = RW_HEADS * RW_HEAD_DIM
RW_DECAY_LORA = 64
RW_ICLR_LORA = 64
RW_GATE_LORA = 128
RW_GN_EPS = 64e-5
RW_IN = 3 * RW_WIDTH + 2 * RW_DECAY_LORA + 2 * RW_ICLR_LORA + RW_GATE_LORA
RW_SPLITS = (RW_WIDTH, 2 * RW_WIDTH, 3 * RW_WIDTH, 3 * RW_WIDTH + 2 * RW_DECAY_LORA,
             3 * RW_WIDTH + 2 * RW_DECAY_LORA + 2 * RW_ICLR_LORA)

HY_WIDTH = 256
HY_ORDER = 2
HY_SHORT_K = 3
HY_EMB_DIM = 33
HY_FILTER_HIDDEN = 64
HY_DECAY_TARGET = 1e-2
HY_FAST_DECAY = 0.3
HY_SLOW_DECAY = 1.5
HY_IN = (HY_ORDER + 1) * HY_WIDTH

NA_HEADS = 6
NA_HEAD_DIM = 64
NA_WIDTH = NA_HEADS * NA_HEAD_DIM
NA_WIN_ROWS = 8
NA_WIN_COLS = 16
NA_IN = 3 * NA_WIDTH

N_BRANCH = 3
GATE_IN = N_BRANCH * D_MODEL
P_IN = RW_IN + HY_IN + NA_IN + GATE_IN
IN_SPLITS = (RW_IN, RW_IN + HY_IN, RW_IN + HY_IN + NA_IN)

FF_DENSE = 2816
N_EXPERTS = 8
TOP_K = 2
FF_EXPERT = 3584
N_DENSE_LAYERS = (DEPTH + 1) // 2
N_MOE_LAYERS = DEPTH // 2

kernel_name = 'hybrid_rwkv7_hyena_natten_moe_dit'


def rms_norm(x, g, eps=NORM_EPS):
    xf = x.astype(jnp.float32)
    y = xf * lax.rsqrt(jnp.mean(xf * xf, axis=-1, keepdims=True) + eps)
    return (y * g.astype(jnp.float32)).astype(x.dtype)


def modulate(h, shift, scale):
    return h * (1 + scale) + shift


def centred_token_shift(u, mu):
    zero = jnp.zeros_like(u[:, :1])
    prev = jnp.concatenate([zero, u[:, :-1]], axis=1)
    nxt = jnp.concatenate([u[:, 1:], zero], axis=1)
    return u + mu[0] * (prev - u) + mu[1] * (nxt - u)


def centred_depthwise_conv(u, w, b):
    k = w.shape[0]
    pad = k // 2
    n = u.shape[1]
    up = jnp.pad(u, ((0, 0), (pad, pad), (0, 0)))
    out = b
    for j in range(k):
        out = out + w[j] * up[:, j:j + n]
    return out


def axial_rope(u):
    n, dh = u.shape[1], u.shape[-1]
    half = dh // 2
    nf = half // 2
    t = jnp.arange(n)
    inv = ROPE_BASE ** (-jnp.arange(nf, dtype=jnp.float32) / nf)
    uf = u.astype(jnp.float32)

    def rot(z, pos):
        ang = pos.astype(jnp.float32)[:, None] * inv
        cos = jnp.cos(ang)[None, :, None, :]
        sin = jnp.sin(ang)[None, :, None, :]
        z1, z2 = z[..., :nf], z[..., nf:]
        return jnp.concatenate([z1 * cos - z2 * sin, z1 * sin + z2 * cos], axis=-1)

    out = jnp.concatenate([rot(uf[..., :half], t // GRID_W), rot(uf[..., half:], t % GRID_W)], axis=-1)
    return out.astype(u.dtype)


def rwkv7_scan(r, w, k, v, a, b, s0):
    def step(S, inp):
        r_t, w_t, k_t, v_t, a_t, b_t = inp
        sa = jnp.einsum('bhvk,bhk->bhv', S, a_t)
        S = S * w_t[:, :, None, :] + sa[..., None] * b_t[:, :, None, :] + v_t[..., None] * k_t[:, :, None, :]
        return S, jnp.einsum('bhvk,bhk->bhv', S, r_t)

    xs = tuple(jnp.swapaxes(z, 0, 1) for z in (r, w, k, v, a, b))
    s_fin, ys = lax.scan(step, s0, xs)
    return jnp.swapaxes(ys, 0, 1), s_fin


def rwkv7_features(u, shift_mu, w0, w2, a0, a2, g2, k_k, k_a, use_rope):
    bsz, n, _ = u.shape
    heads = lambda z: z.reshape(bsz, n, RW_HEADS, RW_HEAD_DIM)
    u = centred_token_shift(u.astype(jnp.float32), shift_mu)
    r, k, v, lw, la, lg = jnp.split(u, RW_SPLITS, axis=-1)
    r, k, v = heads(r), heads(k), heads(v)
    if use_rope:
        r, k = axial_rope(r), axial_rope(k)
    g = jax.nn.sigmoid(lg) @ g2
    kk = k * k_k.reshape(RW_HEADS, RW_HEAD_DIM)
    kk = kk / jnp.maximum(jnp.sqrt(jnp.sum(kk * kk, axis=-1, keepdims=True)), 1e-12)
    lw = lw.reshape(bsz, n, 2, RW_DECAY_LORA)
    la = la.reshape(bsz, n, 2, RW_ICLR_LORA)
    k_a = k_a.reshape(RW_HEADS, RW_HEAD_DIM)
    per_dir = []
    for d in range(2):
        w_log = -jax.nn.softplus(-(w0[d] + jnp.tanh(lw[:, :, d]) @ w2[d])) - 0.5
        iclr = heads(jax.nn.sigmoid(a0[d] + la[:, :, d] @ a2[d]))
        per_dir.append((heads(jnp.exp(-jnp.exp(w_log))), k * (1 + (iclr - 1) * k_a), kk * iclr))
    return r, v, kk, g, per_dir


def rwkv7_direction(feats, d, s0):
    r, v, kk, _, per_dir = feats
    decay, k_d, b_d = per_dir[d]
    seq = (r, decay, k_d, v, -kk, b_d)
    if d == 1:
        seq = tuple(jnp.flip(z, axis=1) for z in seq)
    y, s_fin = rwkv7_scan(*seq, s0)
    if d == 1:
        y = jnp.flip(y, axis=1)
    return y, s_fin


def rwkv7_readout(feats, y, r_k, gn_g, gn_b):
    r, v, _, g, per_dir = feats
    bsz, n = r.shape[:2]
    mu = jnp.mean(y, axis=-1, keepdims=True)
    var = jnp.mean(jnp.square(y - mu), axis=-1, keepdims=True)
    yn = ((y - mu) * lax.rsqrt(var + RW_GN_EPS)).reshape(bsz, n, RW_WIDTH) * gn_g + gn_b
    k_sum = per_dir[0][1] + per_dir[1][1]
    bonus = jnp.sum(r * k_sum * r_k, axis=-1, keepdims=True) * v
    return (yn + bonus.reshape(bsz, n, RW_WIDTH)) * g


def rwkv7_mixer(u_ctx, u_lat, shift_mu, w0, w2, a0, a2, g2, k_k, k_a, r_k, gn_g, gn_b, with_ctx):
    args = (shift_mu, w0, w2, a0, a2, g2, k_k, k_a)
    f_ctx = rwkv7_features(u_ctx, *args, use_rope=False)
    f_lat = rwkv7_features(u_lat, *args, use_rope=True)
    s0 = jnp.zeros((u_lat.shape[0], RW_HEADS, RW_HEAD_DIM, RW_HEAD_DIM), jnp.float32)
    y_ctx, y_lat = 0.0, 0.0
    for d in range(2):
        yc, s_ctx = rwkv7_direction(f_ctx, d, s0)
        yl, _ = rwkv7_direction(f_lat, d, s_ctx)
        y_ctx = y_ctx + yc
        y_lat = y_lat + yl
    out_lat = rwkv7_readout(f_lat, y_lat, r_k, gn_g, gn_b)
    out_ctx = rwkv7_readout(f_ctx, y_ctx, r_k, gn_g, gn_b) if with_ctx else None
    return out_ctx, out_lat


def hyena_filters(n, w1, b1, w2, b2, w3, freq):
    pos = jnp.arange(n, dtype=jnp.float32)
    t = pos / max(n - 1, 1)
    bands = (HY_EMB_DIM - 1) // 2
    f = jnp.linspace(1e-4, bands - 1, bands, dtype=jnp.float32)
    ang = (2 * math.pi / n) * pos[:, None] * f[None, :]
    z = jnp.concatenate([t[:, None], jnp.cos(ang), -jnp.sin(ang)], axis=-1)
    h = jnp.sin(freq * (z @ w1 + b1))
    h = jnp.sin(freq * (h @ w2 + b2))
    h = (h @ w3).astype(jnp.float32).reshape(n, HY_ORDER, 2, HY_WIDTH)
    max_decay = math.log(HY_DECAY_TARGET) / HY_FAST_DECAY
    min_decay = math.log(HY_DECAY_TARGET) / HY_SLOW_DECAY
    deltas = jnp.abs(jnp.linspace(min_decay, max_decay, HY_WIDTH, dtype=jnp.float32))
    h = h * jnp.exp(-t[:, None] * deltas)[:, None, None, :]
    h_fwd = h[:, :, 0]
    h_bwd = h[1:, :, 1]
    l1 = jnp.sum(jnp.abs(h_fwd), axis=0) + jnp.sum(jnp.abs(h_bwd), axis=0)
    filt = jnp.concatenate([h_fwd, jnp.zeros_like(h_fwd[:1]), h_bwd[::-1]], axis=0) / l1
    return jnp.moveaxis(filt, 1, 0)


def fft_long_conv(z, filt):
    n = z.shape[1]
    zf = jnp.fft.rfft(z, n=2 * n, axis=1)
    hf = jnp.fft.rfft(filt, n=2 * n, axis=0)
    return jnp.fft.irfft(zf * hf[None], n=2 * n, axis=1)[:, :n]


def hyena_sequence(u, conv_w, conv_b, w1, b1, w2, b2, w3, freq, skip):
    n = u.shape[1]
    u = centred_depthwise_conv(u, conv_w, conv_b).astype(jnp.float32)
    streams = jnp.split(u, HY_ORDER + 1, axis=-1)
    filt = hyena_filters(n, w1, b1, w2, b2, w3, freq)
    z = streams[0]
    for o in range(HY_ORDER):
        z = streams[o + 1] * (fft_long_conv(z, filt[o]) + skip[o] * z)
    return z


def neighbourhood_attention(u_ctx, u_lat, q_gain, k_gain, rpb, with_ctx):
    bsz, n, _ = u_lat.shape
    lc = u_ctx.shape[1]
    scale = NA_HEAD_DIM ** -0.5

    def qkv(u):
        q, k, v = (z.reshape(bsz, u.shape[1], NA_HEADS, NA_HEAD_DIM)
                   for z in jnp.split(u.astype(jnp.float32), 3, axis=-1))
        return rms_norm(q, q_gain), rms_norm(k, k_gain), v

    qc, kc, vc = qkv(u_ctx)
    ql, kl, vl = qkv(u_lat)
    out_ctx = None
    if with_ctx:
        p = jax.nn.softmax(jnp.einsum('bqhd,bkhd->bhqk', qc, kc) * scale, axis=-1)
        out_ctx = jnp.einsum('bhqk,bkhd->bqhd', p, vc).reshape(bsz, lc, NA_WIDTH)

    rows = n // GRID_W
    wr = min(NA_WIN_ROWS, rows)
    wc = NA_WIN_COLS
    grid = lambda z: z.reshape(bsz, rows, GRID_W, NA_HEADS, NA_HEAD_DIM)
    ql, kl, vl = grid(ql), grid(kl), grid(vl)
    r_ar = jnp.arange(rows)
    c_ar = jnp.arange(GRID_W)
    r0 = jnp.clip(r_ar - wr // 2, 0, rows - wr)
    c0 = jnp.clip(c_ar - wc // 2, 0, GRID_W - wc)
    row_idx = r0[:, None] + jnp.arange(wr)[None, :]
    kb = jnp.take(kl, row_idx, axis=1)
    vb = jnp.take(vl, row_idx, axis=1)
    s_loc = jnp.einsum('bijhd,birchd->bhijrc', ql, kb) * scale
    dr = row_idx - r_ar[:, None] + NA_WIN_ROWS - 1
    dc = jnp.clip(c_ar[None, :] - c_ar[:, None] + NA_WIN_COLS - 1, 0, 2 * NA_WIN_COLS - 2)
    bias = rpb[:, dr[:, None, :, None], dc[None, :, None, :]]
    in_win = (c_ar[None, :] >= c0[:, None]) & (c_ar[None, :] < c0[:, None] + wc)
    s_loc = jnp.where(in_win[:, None, :], s_loc + bias[None].astype(jnp.float32), -jnp.inf)
    s_ctx = jnp.einsum('bijhd,bkhd->bhijk', ql, kc) * scale
    nloc = wr * GRID_W
    s = jnp.concatenate([s_loc.reshape(bsz, NA_HEADS, rows, GRID_W, nloc), s_ctx], axis=-1)
    p = jax.nn.softmax(s, axis=-1)
    p_loc = p[..., :nloc].reshape(bsz, NA_HEADS, rows, GRID_W, wr, GRID_W)
    o = (jnp.einsum('bhijrc,birchd->bijhd', p_loc, vb)
         + jnp.einsum('bhijk,bkhd->bijhd', p[..., nloc:], vc))
    return out_ctx, o.reshape(bsz, n, NA_WIDTH)


def merge_branches(gate_logits, y_rw, y_hy, y_na, w_rw, w_hy, w_na, w_o):
    g_rw, g_hy, g_na = jnp.split(jax.nn.sigmoid(gate_logits.astype(jnp.float32)), N_BRANCH, axis=-1)
    m = g_rw * (y_rw @ w_rw) + g_hy * (y_hy @ w_hy) + g_na * (y_na @ w_na)
    return m @ w_o


def swiglu(h, w1, w3, w2):
    return (jax.nn.silu(h @ w1) * (h @ w3)) @ w2


def moe_swiglu(h, router, w1, w3, w2):
    logits = (h @ router).astype(jnp.float32)
    top_v, top_i = lax.top_k(logits, TOP_K)
    wts = jax.nn.softmax(top_v, axis=-1)
    gates = jnp.sum(jax.nn.one_hot(top_i, N_EXPERTS, dtype=jnp.float32) * wts[..., None], axis=-2)
    out = jnp.zeros(h.shape, jnp.float32)
    for e in range(N_EXPERTS):
        out = out + gates[..., e:e + 1] * swiglu(h, w1[e], w3[e], w2[e])
    return out.astype(h.dtype)


def setup_inputs(seed: int = 0) -> dict:
    key = jax.random.key(seed)
    keys = iter(jax.random.split(key, 64))
    f32 = jnp.float32
    L, D = DEPTH, D_MODEL

    def nrm(shape, scale):
        return jax.random.normal(next(keys), shape, f32) * scale

    def unif(shape, lo, hi):
        return jax.random.uniform(next(keys), shape, f32, lo, hi)

    return {
        'x': nrm((BATCH, SEQ, D), 1.0),
        'c': nrm((BATCH, D), 1.0),
        'ctx': nrm((BATCH, CTX_LEN, D), 1.0),
        'c_ctx': nrm((D,), 1.0),
        'ada_w': nrm((L, D, 6 * D), 0.5 * D ** -0.5),
        'ada_b': nrm((L, 6 * D), 0.02),
        'norm1_g': 1.0 + nrm((L, D), 0.05),
        'norm2_g': 1.0 + nrm((L, D), 0.05),
        'w_in': nrm((L, D, P_IN), D ** -0.5),
        'rw_shift': unif((L, 2, RW_IN), 0.0, 0.5),
        'rw_w0': unif((L, 2, RW_WIDTH), -6.0, -1.0),
        'rw_w2': nrm((L, 2, RW_DECAY_LORA, RW_WIDTH), 0.1),
        'rw_a0': nrm((L, 2, RW_WIDTH), 0.3),
        'rw_a2': nrm((L, 2, RW_ICLR_LORA, RW_WIDTH), 0.1),
        'rw_g2': nrm((L, RW_GATE_LORA, RW_WIDTH), RW_GATE_LORA ** -0.5),
        'rw_kk': 0.85 + nrm((L, RW_WIDTH), 0.05),
        'rw_ka': 1.0 + nrm((L, RW_WIDTH), 0.05),
        'rw_rk': nrm((L, RW_HEADS, RW_HEAD_DIM), 0.1),
        'rw_gn_g': 1.0 + nrm((L, RW_WIDTH), 0.05),
        'rw_gn_b': nrm((L, RW_WIDTH), 0.02),
        'hy_conv_w': nrm((L, HY_SHORT_K, HY_IN), 0.5),
        'hy_conv_b': nrm((L, HY_IN), 0.02),
        'hy_f_w1': nrm((L, HY_EMB_DIM, HY_FILTER_HIDDEN), HY_EMB_DIM ** -0.5),
        'hy_f_b1': nrm((L, HY_FILTER_HIDDEN), 0.1),
        'hy_f_w2': nrm((L, HY_FILTER_HIDDEN, HY_FILTER_HIDDEN), HY_FILTER_HIDDEN ** -0.5),
        'hy_f_b2': nrm((L, HY_FILTER_HIDDEN), 0.1),
        'hy_f_w3': nrm((L, HY_FILTER_HIDDEN, HY_ORDER * 2 * HY_WIDTH), HY_FILTER_HIDDEN ** -0.5),
        'hy_f_freq': 1.0 + nrm((L, HY_FILTER_HIDDEN), 0.05),
        'hy_skip': nrm((L, HY_ORDER, HY_WIDTH), 0.5),
        'na_q_gain': 1.0 + nrm((L, NA_HEAD_DIM), 0.05),
        'na_k_gain': 1.0 + nrm((L, NA_HEAD_DIM), 0.05),
        'na_rpb': nrm((L, NA_HEADS, 2 * NA_WIN_ROWS - 1, 2 * NA_WIN_COLS - 1), 0.1),
        'w_br_rw': nrm((L, RW_WIDTH, D), RW_WIDTH ** -0.5),
        'w_br_hy': nrm((L, HY_WIDTH, D), HY_WIDTH ** -0.5),
        'w_br_na': nrm((L, NA_WIDTH, D), NA_WIDTH ** -0.5),
        'w_out': nrm((L, D, D), D ** -0.5),
        'ff_w1': nrm((N_DENSE_LAYERS, D, FF_DENSE), D ** -0.5),
        'ff_w3': nrm((N_DENSE_LAYERS, D, FF_DENSE), D ** -0.5),
        'ff_w2': nrm((N_DENSE_LAYERS, FF_DENSE, D), FF_DENSE ** -0.5),
        'moe_router': nrm((N_MOE_LAYERS, D, N_EXPERTS), D ** -0.5),
        'moe_w1': nrm((N_MOE_LAYERS, N_EXPERTS, D, FF_EXPERT), D ** -0.5),
        'moe_w3': nrm((N_MOE_LAYERS, N_EXPERTS, D, FF_EXPERT), D ** -0.5),
        'moe_w2': nrm((N_MOE_LAYERS, N_EXPERTS, FF_EXPERT, D), FF_EXPERT ** -0.5),
    }


def reference(x, c, ctx, c_ctx, ada_w, ada_b, norm1_g, norm2_g, w_in,
              rw_shift, rw_w0, rw_w2, rw_a0, rw_a2, rw_g2, rw_kk, rw_ka, rw_rk, rw_gn_g, rw_gn_b,
              hy_conv_w, hy_conv_b, hy_f_w1, hy_f_b1, hy_f_w2, hy_f_b2, hy_f_w3, hy_f_freq, hy_skip,
              na_q_gain, na_k_gain, na_rpb, w_br_rw, w_br_hy, w_br_na, w_out,
              ff_w1, ff_w3, ff_w2, moe_router, moe_w1, moe_w3, moe_w2):
    lc = ctx.shape[1]
    act_lat = jax.nn.silu(c)
    act_ctx = jax.nn.silu(c_ctx)
    for li in range(DEPTH):
        last = li == DEPTH - 1
        mod_lat = jnp.split((act_lat @ ada_w[li] + ada_b[li])[:, None, :], 6, axis=-1)
        mod_ctx = jnp.split((act_ctx @ ada_w[li] + ada_b[li])[None, None, :], 6, axis=-1)

        h = jnp.concatenate([modulate(rms_norm(ctx, norm1_g[li]), mod_ctx[0], mod_ctx[1]),
                             modulate(rms_norm(x, norm1_g[li]), mod_lat[0], mod_lat[1])], axis=1)
        u_rw, u_hy, u_na, u_gate = jnp.split(h @ w_in[li], IN_SPLITS, axis=-1)
        rw_ctx, rw_lat = rwkv7_mixer(u_rw[:, :lc], u_rw[:, lc:], rw_shift[li], rw_w0[li], rw_w2[li],
                                     rw_a0[li], rw_a2[li], rw_g2[li], rw_kk[li], rw_ka[li], rw_rk[li],
                                     rw_gn_g[li], rw_gn_b[li], not last)
        hy_args = (hy_conv_w[li], hy_conv_b[li], hy_f_w1[li], hy_f_b1[li], hy_f_w2[li], hy_f_b2[li],
                   hy_f_w3[li], hy_f_freq[li], hy_skip[li])
        hy_lat = hyena_sequence(u_hy[:, lc:], *hy_args)
        na_ctx, na_lat = neighbourhood_attention(u_na[:, :lc], u_na[:, lc:], na_q_gain[li], na_k_gain[li],
                                                 na_rpb[li], not last)
        br_w = (w_br_rw[li], w_br_hy[li], w_br_na[li], w_out[li])
        x = x + mod_lat[2] * merge_branches(u_gate[:, lc:], rw_lat, hy_lat, na_lat, *br_w).astype(x.dtype)
        if not last:
            hy_ctx = hyena_sequence(u_hy[:, :lc], *hy_args)
            ctx = ctx + mod_ctx[2] * merge_branches(u_gate[:, :lc], rw_ctx, hy_ctx, na_ctx, *br_w).astype(ctx.dtype)

        h = modulate(rms_norm(x, norm2_g[li]), mod_lat[3], mod_lat[4])
        if not last:
            h = jnp.concatenate([modulate(rms_norm(ctx, norm2_g[li]), mod_ctx[3], mod_ctx[4]), h], axis=1)
        if li % 2 == 0:
            f = swiglu(h, ff_w1[li // 2], ff_w3[li // 2], ff_w2[li // 2])
        else:
            f = moe_swiglu(h, moe_router[li // 2], moe_w1[li // 2], moe_w3[li // 2], moe_w2[li // 2])
        if last:
            x = x + mod_lat[5] * f
        else:
            ctx = ctx + mod_ctx[5] * f[:, :lc]
            x = x + mod_lat[5] * f[:, lc:]
    return x
```

```python
import math
from contextlib import ExitStack
import numpy as np
import ml_dtypes
import concourse.bass as bass
import concourse.mybir as mybir
from concourse.bass_utils import run_bass_kernel_spmd

F32 = mybir.dt.float32
BF16 = mybir.dt.bfloat16
ALU = mybir.AluOpType
AF = mybir.ActivationFunctionType
AX = mybir.AxisListType

D = 1024
SEQ = 2048
LC = 256
T = LC + SEQ
NT = T // 128
DEPTH = 2
GRID_W = 64
RW_W = 384
RW_IN = 1536
HY_W = 256
HY_IN = 768
NA_W = 384
NA_IN = 1152
P_IN = 6528
TOKC = P_IN - RW_IN
FF_DENSE = 2816
FF_EXPERT = 3584
NE = 8
EPS = 1e-6
UROWS = T + 4


def urow(t):
    return 1 + t if t < LC else 3 + t


class Dep:
    __slots__ = ("w", "r")

    def __init__(self):
        self.w = None
        self.r = {}


class Emitter:
    EPOCH = 24000

    def __init__(self, nc):
        self.nc = nc
        self.engs = {"pe": nc.tensor, "act": nc.scalar, "dve": nc.vector, "pool": nc.gpsimd, "sp": nc.sync}
        self.sems = {}
        self.epoch = {}
        self.cnt = {}
        for e in ("pe", "act", "dve", "pool"):
            self.epoch[e] = 0
            self.cnt[e] = 0
            self.sems[(e, 0)] = nc.alloc_semaphore(f"s_{e}_0")
        self.waited = {e: {} for e in self.engs}
        self.dsems = {}
        self.dslot = {}
        for q, n in (("sp", 24), ("pool", 24), ("act", 8)):
            self.dsems[q] = []
            for i in range(n):
                key = ("d", q, i)
                self.sems[key] = nc.alloc_semaphore(f"d_{q}_{i}")
                self.dsems[q].append(key)
            self.dslot[q] = 0
        self.dlast = {}
        self.n_ins = 0
        self.pending_noinc = {}

    def _wait(self, e, evs):
        eng = self.engs[e]
        w = self.waited[e]
        for key, val in evs:
            if w.get(key, 0) >= val:
                continue
            eng.wait_ge(self.sems[key], val)
            w[key] = val

    def _collect(self, e, reads, writes):
        evs = []
        for d in reads:
            if d.w is not None:
                evs.append(d.w)
        for d in writes:
            if d.w is not None:
                evs.append(d.w)
            for key, val in d.r.items():
                evs.append((key, val))
        if e == "pe":
            evs = [ev for ev in evs if ev[0][0] != "pe"]
        return evs

    def op(self, e, fn, reads=(), writes=(), inc=True):
        self._wait(e, self._collect(e, reads, writes))
        if self.cnt[e] >= self.EPOCH and not self.pending_noinc.get(e):
            self.epoch[e] += 1
            self.cnt[e] = 0
            self.sems[(e, self.epoch[e])] = self.nc.alloc_semaphore(f"s_{e}_{self.epoch[e]}")
        key = (e, self.epoch[e])
        ins = fn(self.engs[e])
        if inc:
            self.cnt[e] += 1
            ins.then_inc(self.sems[key], 1)
            ev = (key, self.cnt[e])
            self.pending_noinc[e] = False
        else:
            ev = (key, self.cnt[e] + 1)
            self.pending_noinc[e] = True
        for d in reads:
            d.r[key] = ev[1]
        for d in writes:
            d.w = ev
            d.r = {}
        self.n_ins += 1
        return ins

    def dma(self, q, out, in_, reads=(), writes=(), **kw):
        self._wait(q, self._collect(q, reads, writes))
        slot = self.dslot[q]
        self.dslot[q] += 1
        n = len(self.dsems[q])
        key = self.dsems[q][slot % n]
        use = slot // n
        if use > 0:
            self._wait(q, [(key, 16 * use)])
        ins = self.engs[q].dma_start(out=out, in_=in_, **kw)
        ins.then_inc(self.sems[key], 16)
        ev = (key, 16 * (use + 1))
        self.dlast[key] = ev[1]
        for d in reads:
            d.r[key] = ev[1]
        for d in writes:
            d.w = ev
            d.r = {}
        self.n_ins += 1
        return ins

    def barrier(self):
        assert not any(self.pending_noinc.values()), "barrier with pending non-incrementing op"
        evs = []
        for e in ("pe", "act", "dve", "pool"):
            if self.cnt[e] > 0:
                evs.append(((e, self.epoch[e]), self.cnt[e]))
        for key, val in self.dlast.items():
            evs.append((key, val))
        for e in self.engs:
            self._wait(e, [ev for ev in evs if ev[0][0] != e or e == "pool"])


def bf16(a):
    return np.asarray(a, dtype=np.float32).astype(ml_dtypes.bfloat16)


_CONST_CACHE = {}


def host_constants():
    if _CONST_CACHE:
        return _CONST_CACHE
    c = {}
    c["ident_b"] = bf16(np.eye(128))
    c["ident_f"] = np.eye(128, dtype=np.float32)
    c["ones_f"] = np.ones((128, 128), np.float32)
    c["zeros_f"] = np.zeros((128, 1024), np.float32)
    for n, tag in ((SEQ, "l"), (LC, "c")):
        N2 = 2 * n
        t = np.arange(n, dtype=np.float64)
        th = 2 * np.pi * (np.arange(n, dtype=np.float64) + 0.5) / N2
        ang = np.outer(t, th)
        cm = np.cos(ang)
        sm = np.sin(ang)
        nch = n // 128

        def tile_tf(m):
            return np.ascontiguousarray(m.reshape(nch, 128, nch, 128).transpose(2, 1, 0, 3))

        c["cm_" + tag] = bf16(tile_tf(cm))
        c["smn_" + tag] = bf16(tile_tf(-sm))
        sc = 2.0 / N2
        c["icm_" + tag] = bf16(tile_tf((cm * sc).T))
        c["ismn_" + tag] = bf16(tile_tf((-sm * sc).T))
        pos = np.arange(n, dtype=np.float32)
        tt = pos / np.float32(max(n - 1, 1))
        bands = 16
        f = np.linspace(1e-4, bands - 1, bands, dtype=np.float32)
        a2 = (np.float32(2 * math.pi / n) * pos[:, None] * f[None, :]).astype(np.float32)
        z = np.concatenate([tt[:, None], np.cos(a2), -np.sin(a2)], axis=-1).astype(np.float32)
        c["hyz_" + tag] = np.ascontiguousarray(z.T)
        max_decay = math.log(1e-2) / 0.3
        min_decay = math.log(1e-2) / 1.5
        deltas = np.abs(np.linspace(min_decay, max_decay, HY_W, dtype=np.float32))
        dec = np.exp(-tt[:, None] * deltas[None, :]).astype(np.float32)
        c["hydec_" + tag] = np.ascontiguousarray(dec.reshape(nch, 128, HY_W).transpose(1, 0, 2))
    tpos = np.arange(SEQ)
    inv = (10000.0 ** (-np.arange(16, dtype=np.float32) / 16)).astype(np.float32)
    cosT = np.zeros((64, SEQ), np.float32)
    sinT = np.zeros((64, SEQ), np.float32)
    for half, posv in ((0, tpos // GRID_W), (1, tpos % GRID_W)):
        ang = posv.astype(np.float32)[None, :] * inv[:, None]
        for j in range(2):
            cosT[half * 32 + j * 16: half * 32 + (j + 1) * 16] = np.cos(ang)
            sinT[half * 32 + j * 16: half * 32 + (j + 1) * 16] = np.sin(ang)
    c["rope_cos"] = cosT
    c["rope_sin"] = sinT
    Pm = np.zeros((64, 64), np.float32)
    for blk in range(2):
        for i in range(16):
            Pm[blk * 32 + i, blk * 32 + 16 + i] = -1.0
            Pm[blk * 32 + 16 + i, blk * 32 + i] = 1.0
    c["rope_PT"] = np.ascontiguousarray(Pm.T)
    s_i = np.arange(64)[:, None]
    t_i = np.arange(64)[None, :]
    cm_ = np.ones((64, T), np.float32); cm_[:, ::64] = 0.0
    c["rw_cmask"] = cm_
    c["mk_f_strict"] = (t_i > s_i).astype(np.float32)
    c["mk_f_incl"] = (t_i >= s_i).astype(np.float32)
    c["mk_b_strict"] = (t_i < s_i).astype(np.float32)
    c["mk_b_incl"] = (t_i <= s_i).astype(np.float32)
    cq = np.arange(64)
    c0 = np.clip(cq - 8, 0, 48)
    inw = (cq[None, :] >= c0[:, None]) & (cq[None, :] < c0[:, None] + 16)
    c["na_mask"] = np.where(inw, 0.0, -30000.0).astype(np.float32)
    _CONST_CACHE.update(c)
    return c


def na_bias_gather(rpb):
    cq = np.arange(64)
    dc = np.clip(cq[None, :] - cq[:, None] + 15, 0, 30)
    g = rpb[:, :, :, dc]
    return np.ascontiguousarray(np.transpose(g, (0, 3, 1, 2, 4)))


def row_bc(ap_row, nparts=128):
    return bass.AP(ap_row.tensor, ap_row.offset, [[0, nparts]] + [list(x) for x in ap_row.ap[1:]])


class Builder:
    def __init__(self, layers=(0, 1), taps=(), inject=()):
        self.nc = nc = bass.Bass("TRN2", target_bir_lowering=False)
        self.em = Emitter(nc)
        self.layers = layers
        self.taps = set(taps)
        self.inject = set(inject)
        self.uid = 0
        self.I = {}
        self.outs = {}
        self.ps = [nc.alloc_psum_tensor(f"psb{i}", [128, 512], F32) for i in range(8)]
        self.psd = [Dep() for _ in range(8)]
        self.psi = 0
        self.ps_n = 8

    def inp(self, name, shape, dtype=F32):
        a = self.nc.dram_tensor(name, list(shape), dtype, kind="ExternalInput").ap()
        self.I[name] = a
        return a

    def out(self, name, shape, dtype=F32):
        a = self.nc.dram_tensor(name, list(shape), dtype, kind="ExternalOutput").ap()
        self.outs[name] = a
        return a

    def scratch(self, name, shape, dtype=F32):
        return self.nc.dram_tensor(name, list(shape), dtype).ap()

    def sb(self, st, name, shape, dtype=F32):
        self.uid += 1
        return st.enter_context(self.nc.sbuf_tensor(f"{name}_{self.uid}", list(shape), dtype))

    def psum(self):
        i = self.psi % self.ps_n
        self.psi = (i + 1) % self.ps_n
        return self.ps[i], self.psd[i]

    def load_const(self, st, name, dtype=F32, q="sp"):
        a = self.I[name]
        t = self.sb(st, name, list(a.shape), dtype)
        d = Dep()
        self.em.dma(q, t[:], a, writes=[d])
        return t, d

    def load_bc(self, st, name, row_ap, n, nparts=128):
        t = self.sb(st, name, [nparts, n])
        d = Dep()
        self.em.dma("sp", t[:], row_bc(row_ap, nparts), writes=[d])
        return t, d

    def declare(self):
        L = DEPTH
        self.inp("x", [SEQ, D]); self.inp("ctx", [LC, D])
        self.inp("c_lay", [128, 8]); self.inp("cctx_lay", [128, 8])
        self.inp("ada_w", [L, D, 6 * D]); self.inp("ada_b", [L, 6 * D])
        self.inp("norm1_g", [L, D]); self.inp("norm2_g", [L, D])
        self.inp("w_in", [L, D, P_IN])
        self.inp("rw_shift", [L, 2, RW_IN]); self.inp("rw_w0", [L, 2, RW_W]); self.inp("rw_w2", [L, 2, 64, RW_W])
        self.inp("rw_a0", [L, 2, RW_W]); self.inp("rw_a2", [L, 2, 64, RW_W]); self.inp("rw_g2", [L, 128, RW_W])
        self.inp("rw_kk", [L, RW_W]); self.inp("rw_ka", [L, RW_W]); self.inp("rw_rk", [L, RW_W])
        self.inp("rw_gn_g", [L, RW_W]); self.inp("rw_gn_b", [L, RW_W])
        self.inp("hy_conv_w", [L, 3, HY_IN]); self.inp("hy_conv_b", [L, HY_IN])
        self.inp("hy_f_w1", [L, 33, 64]); self.inp("hy_f_b1", [L, 64]); self.inp("hy_f_w2", [L, 64, 64])
        self.inp("hy_f_b2", [L, 64]); self.inp("hy_f_w3", [L, 64, 1024]); self.inp("hy_f_freq", [L, 64])
        self.inp("hy_skip", [L, 2, HY_W])
        self.inp("na_q_gain", [L, 64]); self.inp("na_k_gain", [L, 64])
        self.inp("na_G", [L, 64, 6, 15, 64])
        self.inp("w_br_rw", [L, RW_W, D]); self.inp("w_br_hy", [L, HY_W, D]); self.inp("w_br_na", [L, NA_W, D])
        self.inp("w_out", [L, D, D])
        self.inp("ff_w1", [1, D, FF_DENSE]); self.inp("ff_w3", [1, D, FF_DENSE]); self.inp("ff_w2", [1, FF_DENSE, D])
        self.inp("moe_routerT", [1, NE, D])
        self.inp("moe_w1", [1, NE, D, FF_EXPERT]); self.inp("moe_w3", [1, NE, D, FF_EXPERT])
        self.inp("moe_w2", [1, NE, FF_EXPERT, D])
        hc = host_constants()
        for k, v in hc.items():
            self.inp(k, list(v.shape), BF16 if v.dtype == ml_dtypes.bfloat16 else F32)
        for name in self.inject:
            self.inp("inj_" + name, INJ_SHAPES[name])
        self.y = self.out("y", [SEQ, D])
        self.modv = self.scratch("modv", [DEPTH, 2, 6 * D])
        self.xres = self.scratch("xres", [T, D])
        self.u_rwT = self.scratch("u_rwT", [RW_IN, T])
        self.u_tok = self.scratch("u_tok", [UROWS, TOKC])
        self.y_rw = self.scratch("y_rw", [T, RW_W])
        self.y_hy = self.scratch("y_hy", [T, HY_W])
        self.y_na = self.scratch("y_na", [T, NA_W])
        self.d_modv = Dep(); self.d_xres = Dep(); self.d_urw = Dep(); self.d_utok = Dep()
        self.d_yrw = Dep(); self.d_yhy = Dep(); self.d_yna = Dep()
        self.xres_valid = False

    def tap(self, name, src_ap, shape, reads=()):
        if name in self.taps:
            o = self.out("tap_" + name, shape)
            self.em.dma("sp", o, src_ap, reads=list(reads))

    def res_rows(self, t):
        if self.xres_valid:
            return self.xres[t * 128:(t + 1) * 128, :]
        if t < 2:
            return self.I["ctx"][t * 128:(t + 1) * 128, :]
        return self.I["x"][(t - 2) * 128:(t - 1) * 128, :]

    def setup_globals(self):
        self.gst = ExitStack()
        st = self.gst
        self.ident_b, self.d_ident_b = self.load_const(st, "ident_b", BF16)
        self.ident_f, self.d_ident_f = self.load_const(st, "ident_f", F32)
        self.eps_t = self.sb(st, "eps_t", [128, 1]); self.d_eps = Dep()
        self.em.op("dve", lambda e: e.memset(self.eps_t[:], EPS), writes=[self.d_eps])
        with ExitStack() as zst:
            z, dz = self.load_const(zst, "zeros_f", F32)
            for r in (0, LC + 1, LC + 2, UROWS - 1):
                for c0 in range(0, TOKC, 1024):
                    n = min(1024, TOKC - c0)
                    self.em.dma("sp", self.u_tok[r:r + 1, c0:c0 + n], z[0:1, 0:n], reads=[dz], writes=[self.d_utok])
            self.em.barrier()

    def adaln(self):
        em = self.em
        with ExitStack() as st:
            cT = self.sb(st, "cT", [128, 2, 8]); dc = Dep()
            em.dma("sp", cT[:, 0, :], self.I["c_lay"], writes=[dc])
            em.dma("sp", cT[:, 1, :], self.I["cctx_lay"], writes=[dc])
            aT = self.sb(st, "aT", [128, 2, 8]); da = Dep()
            em.op("act", lambda e: e.activation(aT[:], cT[:], AF.Silu), reads=[dc], writes=[da])
            wb = [self.sb(st, f"adaw{i}", [128, 8, 512]) for i in range(2)]
            dwb = [Dep(), Dep()]
            msb = self.sb(st, "modsb", [2, 6 * D]); dm = Dep()
            bsb = self.sb(st, "adab", [2, 6 * D]); db = Dep()
            it = 0
            for li in self.layers:
                em.dma("sp", bsb[:], row_bc(self.I["ada_b"][li:li + 1, :], 2), writes=[db])
                wv = self.I["ada_w"][li].rearrange("(k p) n -> p k n", p=128)
                for nb in range(12):
                    w = wb[it % 2]; dw = dwb[it % 2]; it += 1
                    em.dma("sp" if nb % 2 == 0 else "act", w[:], wv[:, :, nb * 512:(nb + 1) * 512], writes=[dw])
                    ps, dp = self.psum()
                    for k in range(8):
                        em.op("pe", lambda e, k=k, w=w, ps=ps: e.matmul(ps[0:2, :], aT[:, :, k], w[:, k, :], start=(k == 0), stop=(k == 7)),
                              reads=[da, dw], writes=[dp], inc=(k == 7))
                    em.op("dve", lambda e, ps=ps, nb=nb: e.tensor_tensor(msb[:, nb * 512:(nb + 1) * 512], ps[0:2, :], bsb[:, nb * 512:(nb + 1) * 512], ALU.add),
                          reads=[dp, db], writes=[dm])
                em.dma("sp", self.modv[li], msb[:], reads=[dm], writes=[self.d_modv])
                self.tap(f"modv{li}", self.modv[li], [2, 6 * D], reads=[self.d_modv])
            em.barrier()

    def mod_row(self, li, which, chunk):
        return self.modv[li, which:which + 1, chunk * D:(chunk + 1) * D]

    def norm_hT(self, li, nidx, hT, dhT, tiles, cb=None):
        em = self.em
        with ExitStack() as st:
            gname = "norm1_g" if nidx == 1 else "norm2_g"
            sh_c, sc_c = (0, 1) if nidx == 1 else (3, 4)
            G, dG = self.load_bc(st, "G", self.I[gname][li:li + 1, :], D)
            AB = {}
            for which in (0, 1):
                if which == 1 and all(t >= 2 for t in tiles):
                    continue
                S_, dS = self.load_bc(st, "S", self.mod_row(li, which, sc_c), D)
                B_, dB = self.load_bc(st, "Bm", self.mod_row(li, which, sh_c), D)
                em.op("dve", lambda e, S_=S_: e.scalar_tensor_tensor(S_[:], S_[:], 1.0, G[:], ALU.add, ALU.mult),
                      reads=[dS, dG, self.d_modv], writes=[dS])
                AB[which] = (S_, dS, B_, dB)
            NBN = 3
            xt = [self.sb(st, f"xt{i}", [128, D]) for i in range(NBN)]; dxt = [Dep() for _ in range(NBN)]
            junk = self.sb(st, "junk", [128, D]); dj = Dep()
            hm = [self.sb(st, f"hm{i}", [128, D]) for i in range(NBN)]; dhm = [Dep() for _ in range(NBN)]
            hb = [self.sb(st, f"hb{i}", [128, D], BF16) for i in range(NBN)]; dhb = [Dep() for _ in range(NBN)]
            ss = self.sb(st, "ss", [128, 2 * len(tiles)]); dss_ = [Dep() for _ in range(len(tiles))]
            def ntile(i, t):
                b = i % NBN
                which = 1 if t < 2 else 0
                A_, dA, B_, dB = AB[which]
                em.dma("sp", xt[b][:], self.res_rows(t), reads=[self.d_xres], writes=[dxt[b]])
                s0 = ss[:, 2 * i:2 * i + 1]; s1 = ss[:, 2 * i + 1:2 * i + 2]; dss = dss_[i]
                em.op("act", lambda e, b=b, s0=s0: e.activation(junk[:], xt[b][:], AF.Square, accum_out=s0),
                      reads=[dxt[b]], writes=[dj, dss])
                em.op("act", lambda e, s0=s0, s1=s1: e.activation(s1, s0, AF.Sqrt, bias=self.eps_t[:], scale=1.0 / D), reads=[dss, self.d_eps], writes=[dss])
                yield
                em.op("dve", lambda e, s1=s1: e.reciprocal(s1, s1), reads=[dss], writes=[dss])
                em.op("dve", lambda e, b=b, s1=s1, A_=A_: e.scalar_tensor_tensor(hm[b][:], xt[b][:], s1, A_[:], ALU.mult, ALU.mult),
                      reads=[dxt[b], dss, dA], writes=[dhm[b]])
                yield
                em.op("pool", lambda e, b=b, B_=B_: e.tensor_tensor(hm[b][:], hm[b][:], B_[:], ALU.add), reads=[dhm[b], dB], writes=[dhm[b]])
                yield
                em.op("act", lambda e, b=b: e.activation(hb[b][:], hm[b][:], AF.Copy), reads=[dhm[b]], writes=[dhb[b]])
                if cb is not None:
                    cb(st, t, hm[b], dhm[b])
                yield
                ps, dp = self.psum()
                pb = ps[:].bitcast(BF16)
                for k in range(8):
                    em.op("pe", lambda e, k=k, b=b, pb=pb: e.transpose(pb[:, k * 128:(k + 1) * 128], hb[b][:, k * 128:(k + 1) * 128], self.ident_b[:]),
                          reads=[dhb[b], self.d_ident_b], writes=[dp])
                yield
                em.op("dve", lambda e, t=t, pb=pb: e.tensor_copy(hT[:, :, t * 128:(t + 1) * 128], pb.rearrange("p (k n) -> p k n", k=8)),
                      reads=[dp], writes=[dhT])

            gens = [ntile(i, t) for i, t in enumerate(tiles)]
            active = []
            gi = 0
            while gi < len(gens) or active:
                while len(active) < NBN and gi < len(gens):
                    active.append(gens[gi]); gi += 1
                for g in list(active):
                    try:
                        next(g)
                    except StopIteration:
                        active.remove(g)
            em.barrier()

    def proj_in(self, li, tiles):
        em = self.em
        with ExitStack() as st:
            hT = self.sb(st, "hT", [128, 8, T], BF16); dhT = Dep()
            self.norm_hT(li, 1, hT, dhT, tiles)
            wv = self.I["w_in"][li].rearrange("(k p) n -> p k n", p=128)
            wb = [self.sb(st, f"winb{i}", [128, 8, 512], BF16) for i in range(2)]; dwb = [Dep(), Dep()]
            og = [self.sb(st, f"og{i}", [128, 512]) for i in range(4)]; dog = [Dep() for _ in range(4)]
            oi = 0
            t0 = tiles[0] * 128
            tokblocks = [(s, min(512, T - s)) for s in range(t0, T, 512)]
            nblk = (P_IN + 511) // 512
            for cb in range(nblk):
                c0 = cb * 512
                cn = min(512, P_IN - c0)
                w = wb[cb % 2]; dw = dwb[cb % 2]
                em.dma("pool", w[:, :, 0:cn], wv[:, :, c0:c0 + cn], writes=[dw])
                if c0 < RW_IN:
                    for mc in range(cn // 128):
                        for (s, n) in tokblocks:
                            ps, dp = self.psum()
                            for k in range(8):
                                em.op("pe", lambda e, k=k, w=w, ps=ps, mc=mc, s=s, n=n: e.matmul(ps[:, 0:n], w[:, k, mc * 128:(mc + 1) * 128], hT[:, k, s:s + n], start=(k == 0), stop=(k == 7)),
                                      reads=[dw, dhT], writes=[dp], inc=(k == 7))
                            o = og[oi % 4]; do = dog[oi % 4]; oi += 1
                            eng = "act" if oi % 2 else "dve"
                            if eng == "act":
                                em.op("act", lambda e, o=o, ps=ps, n=n: e.activation(o[:, 0:n], ps[:, 0:n], AF.Copy), reads=[dp], writes=[do])
                            else:
                                em.op("dve", lambda e, o=o, ps=ps, n=n: e.tensor_copy(o[:, 0:n], ps[:, 0:n]), reads=[dp], writes=[do])
                            r0 = c0 + mc * 128
                            em.dma("sp", self.u_rwT[r0:r0 + 128, s:s + n], o[:, 0:n], reads=[do], writes=[self.d_urw])
                else:
                    for t in tiles:
                        ps, dp = self.psum()
                        for k in range(8):
                            em.op("pe", lambda e, k=k, w=w, ps=ps, t=t, cn=cn: e.matmul(ps[:, 0:cn], hT[:, k, t * 128:(t + 1) * 128], w[:, k, 0:cn], start=(k == 0), stop=(k == 7)),
                                  reads=[dw, dhT], writes=[dp], inc=(k == 7))
                        o = og[oi % 4]; do = dog[oi % 4]; oi += 1
                        eng = "act" if oi % 2 else "dve"
                        if eng == "act":
                            em.op("act", lambda e, o=o, ps=ps, cn=cn: e.activation(o[:, 0:cn], ps[:, 0:cn], AF.Copy), reads=[dp], writes=[do])
                        else:
                            em.op("dve", lambda e, o=o, ps=ps, cn=cn: e.tensor_copy(o[:, 0:cn], ps[:, 0:cn]), reads=[dp], writes=[do])
                        r0 = urow(t * 128)
                        em.dma("sp", self.u_tok[r0:r0 + 128, c0 - RW_IN:c0 - RW_IN + cn], o[:, 0:cn], reads=[do], writes=[self.d_utok])
            em.barrier()
        self.tap(f"urw{li}", self.u_rwT, [RW_IN, T], reads=[self.d_urw])
        self.tap(f"utok{li}", self.u_tok, [UROWS, TOKC], reads=[self.d_utok])


    def merge(self, li, last):
        em = self.em
        tiles = list(range(2, NT)) if last else list(range(NT))
        ysrc = {"rw": (self.y_rw, self.d_yrw), "hy": (self.y_hy, self.d_yhy), "na": (self.y_na, self.d_yna)}
        for nm in ("rw", "hy", "na"):
            if ("y_" + nm) in self.inject:
                ysrc[nm] = (self.I["inj_y_" + nm], Dep())
        with ExitStack() as st:
            wbr = self.sb(st, "wbr", [128, 8, D], BF16); dwbr = Dep()
            em.dma("pool", wbr[:, 0:3, :], self.I["w_br_rw"][li].rearrange("(k p) n -> p k n", p=128), writes=[dwbr])
            em.dma("pool", wbr[:, 3:5, :], self.I["w_br_hy"][li].rearrange("(k p) n -> p k n", p=128), writes=[dwbr])
            em.dma("pool", wbr[:, 5:8, :], self.I["w_br_na"][li].rearrange("(k p) n -> p k n", p=128), writes=[dwbr])
            wo = self.sb(st, "wo", [128, 8, D], BF16); dwo = Dep()
            em.dma("pool", wo[:], self.I["w_out"][li].rearrange("(k p) n -> p k n", p=128), writes=[dwo])
            G1 = {}
            for which in (0, 1):
                if which == 1 and last:
                    continue
                G1[which] = self.load_bc(st, "G1", self.mod_row(li, which, 2), D)
            yc = [self.sb(st, f"yc{i}", [128, D]) for i in range(3)]; dyc = [Dep() for _ in range(3)]
            ycb_ = [self.sb(st, f"ycb{i}", [128, D], BF16) for i in range(3)]; dycb_ = [Dep() for _ in range(3)]
            ycT_ = [self.sb(st, f"ycT{i}", [128, 8, 128], BF16) for i in range(3)]; dycT_ = [Dep() for _ in range(3)]
            gt = [self.sb(st, f"gt{i}", [128, 3 * D]) for i in range(3)]; dgt = [Dep() for _ in range(3)]
            m_ = [self.sb(st, f"m{i}", [128, D]) for i in range(3)]; dm_ = [Dep() for _ in range(3)]
            tmp_ = [self.sb(st, f"mtmp{i}", [128, 512]) for i in range(3)]; dtmp_ = [Dep() for _ in range(3)]
            mb_ = [self.sb(st, f"mb{i}", [128, D], BF16) for i in range(3)]; dmb_ = [Dep() for _ in range(3)]
            mT_ = [self.sb(st, f"mT{i}", [128, 8, 128], BF16) for i in range(3)]; dmT_ = [Dep() for _ in range(3)]
            xt = [self.sb(st, f"mxt{i}", [128, D]) for i in range(3)]; dxt = [Dep() for _ in range(3)]
            xo = [self.sb(st, f"mxo{i}", [128, D]) for i in range(3)]; dxo = [Dep() for _ in range(3)]
            brk = {0: (0, 3), 1: (3, 5), 2: (5, 8)}
            def tile_gen(i, t):
                b = i % 3
                ycb, dycb, ycT, dycT = ycb_[b], dycb_[b], ycT_[b], dycT_[b]
                m, dm, tmp, dtmp, mb, dmb, mT, dmT = m_[b], dm_[b], tmp_[b], dtmp_[b], mb_[b], dmb_[b], mT_[b], dmT_[b]
                r0 = t * 128
                em.dma("sp", yc[b][:, 0:384], ysrc["rw"][0][r0:r0 + 128, :], reads=[ysrc["rw"][1]], writes=[dyc[b]])
                em.dma("sp", yc[b][:, 384:640], ysrc["hy"][0][r0:r0 + 128, :], reads=[ysrc["hy"][1]], writes=[dyc[b]])
                em.dma("sp", yc[b][:, 640:1024], ysrc["na"][0][r0:r0 + 128, :], reads=[ysrc["na"][1]], writes=[dyc[b]])
                ur = urow(r0)
                em.dma("sp", gt[b][:], self.u_tok[ur:ur + 128, HY_IN + NA_IN:TOKC], reads=[self.d_utok], writes=[dgt[b]])
                em.dma("sp", xt[b][:], self.res_rows(t), reads=[self.d_xres], writes=[dxt[b]])
                em.op("act", lambda e, b=b: e.activation(gt[b][:], gt[b][:], AF.Sigmoid), reads=[dgt[b]], writes=[dgt[b]])
                em.op("pool", lambda e, b=b: e.tensor_copy(ycb[:], yc[b][:]), reads=[dyc[b]], writes=[dycb])
                ps, dp = self.psum(); pb = ps[:].bitcast(BF16)
                for k in range(8):
                    em.op("pe", lambda e, k=k, pb=pb: e.transpose(pb[:, k * 128:(k + 1) * 128], ycb[:, k * 128:(k + 1) * 128], self.ident_b[:]),
                          reads=[dycb, self.d_ident_b], writes=[dp])
                yield
                em.op("act", lambda e, pb=pb: e.activation(ycT[:], pb.rearrange("p (k n) -> p k n", k=8), AF.Copy), reads=[dp], writes=[dycT])
                yield
                for br in range(3):
                    k0, k1 = brk[br]
                    for nb in range(2):
                        ps, dp = self.psum()
                        for k in range(k0, k1):
                            em.op("pe", lambda e, k=k, ps=ps, nb=nb, k0=k0, k1=k1: e.matmul(ps[:], ycT[:, k, :], wbr[:, k, nb * 512:(nb + 1) * 512], start=(k == k0), stop=(k == k1 - 1)),
                                  reads=[dycT, dwbr], writes=[dp], inc=(k == k1 - 1))
                        gsl = gt[b][:, br * D + nb * 512: br * D + (nb + 1) * 512]
                        msl = m[:, nb * 512:(nb + 1) * 512]
                        if br == 0:
                            em.op("dve", lambda e, ps=ps, gsl=gsl, msl=msl: e.tensor_tensor(msl, ps[:], gsl, ALU.mult), reads=[dp, dgt[b]], writes=[dm])
                        else:
                            em.op("dve", lambda e, ps=ps, gsl=gsl: e.tensor_tensor(tmp[:], ps[:], gsl, ALU.mult), reads=[dp, dgt[b]], writes=[dtmp])
                            em.op("dve", lambda e, msl=msl: e.tensor_tensor(msl, msl, tmp[:], ALU.add), reads=[dtmp, dm], writes=[dm])
                        yield
                em.op("act", lambda e: e.activation(mb[:], m[:], AF.Copy), reads=[dm], writes=[dmb])
                ps, dp = self.psum(); pb = ps[:].bitcast(BF16)
                for k in range(8):
                    em.op("pe", lambda e, k=k, pb=pb: e.transpose(pb[:, k * 128:(k + 1) * 128], mb[:, k * 128:(k + 1) * 128], self.ident_b[:]),
                          reads=[dmb, self.d_ident_b], writes=[dp])
                yield
                em.op("act", lambda e, pb=pb: e.activation(mT[:], pb.rearrange("p (k n) -> p k n", k=8), AF.Copy), reads=[dp], writes=[dmT])
                yield
                Gt, dG = G1[1 if t < 2 else 0]
                for nb in range(2):
                    ps, dp = self.psum()
                    for k in range(8):
                        em.op("pe", lambda e, k=k, ps=ps, nb=nb: e.matmul(ps[:], mT[:, k, :], wo[:, k, nb * 512:(nb + 1) * 512], start=(k == 0), stop=(k == 7)),
                              reads=[dmT, dwo], writes=[dp], inc=(k == 7))
                    sl = slice(nb * 512, (nb + 1) * 512)
                    em.op("dve", lambda e, ps=ps, sl=sl, Gt=Gt, b=b: e.tensor_tensor(xo[b][:, sl], ps[:], Gt[:, sl], ALU.mult), reads=[dp, dG, self.d_modv], writes=[dxo[b]])
                    em.op("pool", lambda e, sl=sl, b=b: e.tensor_tensor(xo[b][:, sl], xo[b][:, sl], xt[b][:, sl], ALU.add), reads=[dxo[b], dxt[b]], writes=[dxo[b]])
                    yield
                em.dma("sp", self.xres[r0:r0 + 128, :], xo[b][:], reads=[dxo[b]], writes=[Dep()])

            gens = [tile_gen(i, t) for i, t in enumerate(tiles)]
            active = []
            gi = 0
            while gi < len(gens) or active:
                while len(active) < 3 and gi < len(gens):
                    active.append(gens[gi]); gi += 1
                for g in list(active):
                    try:
                        next(g)
                    except StopIteration:
                        active.remove(g)
            em.barrier()
        if last and not self.xres_valid:
            pass
        if not self.xres_valid:
            self.xres_valid = True
        self.tap(f"xmix{li}", self.xres, [T, D], reads=[self.d_xres])

    def ffn(self, li, last):
        em = self.em
        moe = (li % 2 == 1)
        tiles = list(range(2, NT)) if last else list(range(NT))
        t0 = tiles[0] * 128
        ntok = len(tiles) * 128
        if moe:
            FF, FB, E = FF_EXPERT, 512, NE
            W1, W3, W2 = self.I["moe_w1"][li // 2], self.I["moe_w3"][li // 2], self.I["moe_w2"][li // 2]
        else:
            FF, FB, E = FF_DENSE, 256, 1
            W1, W3, W2 = self.I["ff_w1"], self.I["ff_w3"], self.I["ff_w2"]
        nfc = FB // 128
        with ExitStack() as st:
            hT = self.sb(st, "fhT", [128, 8, T], BF16); dhT = Dep()
            gates = self.sb(st, "gates", [128, NT, NE]); dgates = Dep()
            if moe:
                with ExitStack() as st2:
                    Rb = self.sb(st2, "Rb", [128, NE, D]); dRb = Dep()
                    for e_ in range(NE):
                        em.dma("sp", Rb[:, e_, :], row_bc(self.I["moe_routerT"][li // 2, e_:e_ + 1, :]), writes=[dRb])
                    lg = self.sb(st2, "lg", [128, NT, NE]); dlg = Dep()
                    jk = self.sb(st2, "rjunk", [128, D]); djk = Dep()

                    def cb(st_, t, hm, dhm):
                        for e_ in range(NE):
                            em.op("dve", lambda e, e_=e_, hm=hm: e.tensor_tensor(jk[:], hm[:], Rb[:, e_, :], ALU.mult), reads=[dhm, dRb], writes=[djk])
                            em.op("dve", lambda e, e_=e_, t=t: e.reduce_sum(lg[:, t, e_:e_ + 1], jk[:], AX.X), reads=[djk], writes=[dlg])
                    self.norm_hT(li, 2, hT, dhT, tiles, cb=cb)
                    nt = len(tiles); ta = tiles[0]
                    L = lg[:, ta:ta + nt, :]
                    m1 = self.sb(st2, "m1", [128, NT, 1]); m2 = self.sb(st2, "m2", [128, NT, 1])
                    eq1 = self.sb(st2, "eq1", [128, NT, NE]); eq2 = self.sb(st2, "eq2", [128, NT, NE]); l2 = self.sb(st2, "l2", [128, NT, NE])
                    w1_ = self.sb(st2, "w1_", [128, NT, 1]); w2_ = self.sb(st2, "w2_", [128, NT, 1])
                    dd = Dep()
                    sl = lambda a: a[:, ta:ta + nt, :]
                    bc = lambda a: a[:, ta:ta + nt, :].to_broadcast([128, nt, NE])
                    em.op("dve", lambda e: e.reduce_max(m1[:, ta:ta + nt, 0], L, AX.X), reads=[dlg], writes=[dd])
                    em.op("dve", lambda e: e.tensor_tensor(sl(eq1), L, bc(m1), ALU.is_equal), reads=[dd, dlg], writes=[dd])
                    em.op("dve", lambda e: e.scalar_tensor_tensor(sl(l2), sl(eq1), -1e30, L, ALU.mult, ALU.add), reads=[dd, dlg], writes=[dd])
                    em.op("dve", lambda e: e.reduce_max(m2[:, ta:ta + nt, 0], sl(l2), AX.X), reads=[dd], writes=[dd])
                    em.op("dve", lambda e: e.tensor_tensor(sl(eq2), sl(l2), bc(m2), ALU.is_equal), reads=[dd], writes=[dd])
                    em.op("dve", lambda e: e.tensor_tensor(sl(w2_), sl(m2), sl(m1), ALU.subtract), reads=[dd], writes=[dd])
                    em.op("act", lambda e: e.activation(sl(w2_), sl(w2_), AF.Exp), reads=[dd], writes=[dd])
                    em.op("dve", lambda e: e.tensor_scalar(sl(w1_), sl(w2_), 1.0, None, ALU.add), reads=[dd], writes=[dd])
                    em.op("dve", lambda e: e.reciprocal(sl(w1_), sl(w1_)), reads=[dd], writes=[dd])
                    em.op("dve", lambda e: e.tensor_tensor(sl(w2_), sl(w2_), sl(w1_), ALU.mult), reads=[dd], writes=[dd])
                    em.op("dve", lambda e: e.tensor_tensor(sl(eq1), sl(eq1), bc(w1_), ALU.mult), reads=[dd], writes=[dd])
                    em.op("dve", lambda e: e.tensor_tensor(sl(eq2), sl(eq2), bc(w2_), ALU.mult), reads=[dd], writes=[dd])
                    em.op("dve", lambda e: e.tensor_tensor(sl(gates), sl(eq1), sl(eq2), ALU.add), reads=[dd], writes=[dgates])
                    em.barrier()
            else:
                self.norm_hT(li, 2, hT, dhT, tiles)
            acc = self.sb(st, "acc", [128, len(tiles), D]); dacc = Dep()
            w1b = [self.sb(st, f"w1b{i}", [128, 8, FB], BF16) for i in range(2)]
            w3b = [self.sb(st, f"w3b{i}", [128, 8, FB], BF16) for i in range(2)]
            w2b = [self.sb(st, f"w2b{i}", [128, nfc, D], BF16) for i in range(2)]
            dwb = [Dep(), Dep()]
            gT = self.sb(st, "gT", [128, nfc, ntok], BF16); dgT = Dep()
            sil = [self.sb(st, f"sil{i}", [128, 512]) for i in range(2)]; dsil = [Dep(), Dep()]
            tokblocks = [(s, min(512, ntok - s)) for s in range(0, ntok, 512)]
            blk = 0
            si = 0
            for e_ in range(E):
                w1v = (W1[e_] if moe else W1[0]).rearrange("(k p) f -> p k f", p=128)
                w3v = (W3[e_] if moe else W3[0]).rearrange("(k p) f -> p k f", p=128)
                w2v = (W2[e_] if moe else W2[0]).rearrange("(c p) n -> p c n", p=128)
                for fb in range(FF // FB):
                    b = blk % 2
                    em.dma("pool", w1b[b][:], w1v[:, :, fb * FB:(fb + 1) * FB], writes=[dwb[b]])
                    em.dma("pool", w3b[b][:], w3v[:, :, fb * FB:(fb + 1) * FB], writes=[dwb[b]])
                    em.dma("pool", w2b[b][:], w2v[:, fb * nfc:(fb + 1) * nfc, :], writes=[dwb[b]])
                    for (s, n) in tokblocks:
                        for fc in range(nfc):
                            p1, dp1 = self.psum(); p3, dp3 = self.psum()
                            for k in range(8):
                                em.op("pe", lambda e, k=k, p1=p1, fc=fc, s=s, n=n, b=b: e.matmul(p1[:, 0:n], w1b[b][:, k, fc * 128:(fc + 1) * 128], hT[:, k, t0 + s:t0 + s + n], start=(k == 0), stop=(k == 7)),
                                      reads=[dwb[b], dhT], writes=[dp1], inc=(k == 7))
                            for k in range(8):
                                em.op("pe", lambda e, k=k, p3=p3, fc=fc, s=s, n=n, b=b: e.matmul(p3[:, 0:n], w3b[b][:, k, fc * 128:(fc + 1) * 128], hT[:, k, t0 + s:t0 + s + n], start=(k == 0), stop=(k == 7)),
                                      reads=[dwb[b], dhT], writes=[dp3], inc=(k == 7))
                            sb_ = sil[si % 2]; dsb = dsil[si % 2]; si += 1
                            em.op("act", lambda e, sb_=sb_, p1=p1, n=n: e.activation(sb_[:, 0:n], p1[:, 0:n], AF.Silu), reads=[dp1], writes=[dsb])
                            em.op("dve", lambda e, sb_=sb_, p3=p3, n=n, fc=fc, s=s: e.tensor_tensor(gT[:, fc, s:s + n], p3[:, 0:n], sb_[:, 0:n], ALU.mult),
                                  reads=[dp3, dsb], writes=[dgT])
                    for ti, t in enumerate(tiles):
                        for nb in range(2):
                            ps, dp = self.psum()
                            for fc in range(nfc):
                                em.op("pe", lambda e, fc=fc, ps=ps, ti=ti, nb=nb, b=b: e.matmul(ps[:], gT[:, fc, ti * 128:(ti + 1) * 128], w2b[b][:, fc, nb * 512:(nb + 1) * 512], start=(fc == 0), stop=(fc == nfc - 1)),
                                      reads=[dgT, dwb[b]], writes=[dp], inc=(fc == nfc - 1))
                            asl = acc[:, ti, nb * 512:(nb + 1) * 512]
                            if moe:
                                gcol = gates[:, t, e_:e_ + 1]
                                if blk == 0:
                                    em.op("dve", lambda e, ps=ps, asl=asl, gcol=gcol: e.tensor_scalar(asl, ps[:], gcol, None, ALU.mult), reads=[dp, dgates], writes=[dacc])
                                else:
                                    em.op("dve", lambda e, ps=ps, asl=asl, gcol=gcol: e.scalar_tensor_tensor(asl, ps[:], gcol, asl, ALU.mult, ALU.add), reads=[dp, dgates, dacc], writes=[dacc])
                            else:
                                if blk == 0:
                                    em.op("dve", lambda e, ps=ps, asl=asl: e.tensor_copy(asl, ps[:]), reads=[dp], writes=[dacc])
                                else:
                                    em.op("dve", lambda e, ps=ps, asl=asl: e.tensor_tensor(asl, ps[:], asl, ALU.add), reads=[dp, dacc], writes=[dacc])
                    blk += 1
            G2 = {}
            for which in (0, 1):
                if which == 1 and last:
                    continue
                G2[which] = self.load_bc(st, "G2", self.mod_row(li, which, 5), D)
            xt = [self.sb(st, f"fxt{i}", [128, D]) for i in range(2)]; dxt = [Dep(), Dep()]
            for ti, t in enumerate(tiles):
                b = ti % 2
                Gt, dG = G2[1 if t < 2 else 0]
                em.dma("sp", xt[b][:], self.res_rows(t), reads=[self.d_xres], writes=[dxt[b]])
                em.op("dve", lambda e, ti=ti, Gt=Gt: e.tensor_tensor(acc[:, ti, :], acc[:, ti, :], Gt[:], ALU.mult), reads=[dacc, dG, self.d_modv], writes=[dacc])
                em.op("pool", lambda e, ti=ti, b=b: e.tensor_tensor(acc[:, ti, :], acc[:, ti, :], xt[b][:], ALU.add), reads=[dacc, dxt[b]], writes=[dacc])
                if last:
                    em.dma("sp", self.y[(t - 2) * 128:(t - 1) * 128, :], acc[:, ti, :], reads=[dacc], writes=[Dep()])
                else:
                    em.dma("sp", self.xres[t * 128:(t + 1) * 128, :], acc[:, ti, :], reads=[dacc], writes=[Dep()])
            em.barrier()
        self.tap(f"xffn{li}", self.xres, [T, D], reads=[self.d_xres])


    def na(self, li, last):
        em = self.em
        C_Q = HY_IN
        with ExitStack() as st:
            qT = self.sb(st, "qT", [64, 6, T], BF16); dqT = Dep()
            kT = self.sb(st, "kT", [64, 6, T], BF16); dkT = Dep()
            V = self.sb(st, "V", [128, NT, NA_W], BF16); dV = Dep()
            Vs = self.sb(st, "Vs", [128, 15, NA_W], BF16); dVs = Dep()
            Gm = self.sb(st, "Gm", [64, 6, 15, 64]); dGm = Dep()
            em.dma("sp", Gm[:], self.I["na_G"][li], writes=[dGm])
            mk, dmk = self.load_const(st, "na_mask")
            for h in range(6):
                em.op("pool", lambda e, h=h: e.tensor_tensor(Gm[:, h, :, :], Gm[:, h, :, :], mk[:].rearrange("p (a c) -> p a c", a=1).to_broadcast([64, 15, 64]), ALU.add),
                      reads=[dGm, dmk], writes=[dGm])
            gain = self.sb(st, "nagain", [128, 12, 64]); dgain = Dep()
            for j in range(12):
                src = self.I["na_q_gain"] if j < 6 else self.I["na_k_gain"]
                em.dma("sp", gain[:, j, :], row_bc(src[li:li + 1, :]), writes=[dgain])
            em.op("dve", lambda e: e.tensor_scalar(gain[:, 0:6, :], gain[:, 0:6, :], 0.125, None, ALU.mult), reads=[dgain], writes=[dgain])
            with ExitStack() as st2:
                ut = [self.sb(st2, f"nau{i}", [128, NA_IN]) for i in range(2)]; dut = [Dep(), Dep()]
                sq = self.sb(st2, "nasq", [128, 768]); dsq = Dep()
                ssm = self.sb(st2, "nass", [128, 12, 1]); dssm = Dep()
                qkn = self.sb(st2, "qkn", [128, 768]); dqkn = Dep()
                qkb = self.sb(st2, "qkb", [128, 768], BF16); dqkb = Dep()
                vst = [self.sb(st2, f"vst{i}", [128, NA_W]) for i in range(2)]; dvst = [Dep(), Dep()]
                for t in range(NT):
                    b = t % 2
                    ur = urow(t * 128)
                    em.dma("sp", ut[b][:], self.u_tok[ur:ur + 128, C_Q:C_Q + NA_IN], reads=[self.d_utok], writes=[dut[b]])
                    em.op("act", lambda e, b=b: e.activation(sq[:], ut[b][:, 0:768], AF.Square), reads=[dut[b]], writes=[dsq])
                    em.op("dve", lambda e: e.reduce_sum(ssm[:, :, 0], sq[:].rearrange("p (j d) -> p j d", j=12), AX.X), reads=[dsq], writes=[dssm])
                    em.op("act", lambda e: e.activation(ssm[:, :, 0], ssm[:, :, 0], AF.Sqrt, bias=self.eps_t[:], scale=1.0 / 64), reads=[dssm, self.d_eps], writes=[dssm])
                    em.op("dve", lambda e: e.reciprocal(ssm[:, :, 0], ssm[:, :, 0]), reads=[dssm], writes=[dssm])
                    em.op("dve", lambda e, b=b: e.tensor_tensor(qkn[:].rearrange("p (j d) -> p j d", j=12), ut[b][:, 0:768].rearrange("p (j d) -> p j d", j=12), ssm[:].to_broadcast([128, 12, 64]), ALU.mult),
                          reads=[dut[b], dssm], writes=[dqkn])
                    em.op("pool", lambda e: e.tensor_tensor(qkb[:].rearrange("p (j d) -> p j d", j=12), qkn[:].rearrange("p (j d) -> p j d", j=12), gain[:], ALU.mult), reads=[dqkn, dgain], writes=[dqkb])
                    em.op("act", lambda e, b=b, t=t: e.activation(V[:, t, :], ut[b][:, 768:1152], AF.Copy), reads=[dut[b]], writes=[dV])
                    for qk, (dst, ddst) in enumerate(((qT, dqT), (kT, dkT))):
                        ps, dp = self.psum(); pb = ps[:].bitcast(BF16)
                        for h in range(6):
                            em.op("pe", lambda e, h=h, pb=pb, qk=qk: e.transpose(pb[0:64, h * 128:(h + 1) * 128], qkb[:, (qk * 6 + h) * 64:(qk * 6 + h + 1) * 64], self.ident_b[:]),
                                  reads=[dqkb, self.d_ident_b], writes=[dp])
                        em.op("dve" if qk == 0 else "act",
                              (lambda e, pb=pb, dst=dst, t=t: e.tensor_copy(dst[:, :, t * 128:(t + 1) * 128], pb[0:64, 0:768].rearrange("p (h n) -> p h n", h=6))) if qk == 0 else
                              (lambda e, pb=pb, dst=dst, t=t: e.activation(dst[:, :, t * 128:(t + 1) * 128], pb[0:64, 0:768].rearrange("p (h n) -> p h n", h=6), AF.Copy)),
                              reads=[dp], writes=[ddst])
                for j in range(15):
                    b = j % 2
                    ur = urow(LC + 64 + j * 128)
                    em.dma("sp", vst[b][:], self.u_tok[ur:ur + 128, C_Q + 768:C_Q + NA_IN], reads=[self.d_utok], writes=[dvst[b]])
                    em.op("act", lambda e, b=b, j=j: e.activation(Vs[:, j, :], vst[b][:], AF.Copy), reads=[dvst[b]], writes=[dVs])
                em.barrier()
            NB = 4
            sal = [self.sb(st, f"sal{i}", [128, 768]) for i in range(NB)]; dsal = [Dep() for _ in range(NB)]
            pbf = [self.sb(st, f"pbf{i}", [128, 768], BF16) for i in range(NB)]; dpbf = [Dep() for _ in range(NB)]
            PT = [self.sb(st, f"PT{i}", [128, 768], BF16) for i in range(NB)]; dPT = [Dep() for _ in range(NB)]
            stt = [self.sb(st, f"nast{i}", [128, 4]) for i in range(NB)]; dstt = [Dep() for _ in range(NB)]
            orow = [self.sb(st, f"orow{i}", [128, NA_W]) for i in range(3)]; dorow = [Dep() for _ in range(3)]
            ucount = [0]

            def unit(nq, qsl_of, segs, vsegs, ob, ocol, tail=None):
                u = ucount[0]; ucount[0] += 1
                b = u % NB
                bkA, dA = self.ps[2 * b], self.psd[2 * b]
                bkB, dB = self.ps[2 * b + 1], self.psd[2 * b + 1]
                ntot = sum(s_[1] for s_ in segs)
                off = 0
                outs = []
                for (rhs, ncol, bias) in segs:
                    bk, dbk = (bkB, dB) if bias is not None else (bkA, dA)
                    em.op("pe", lambda e, bk=bk, rhs=rhs, ncol=ncol: e.matmul(bk[0:nq, 0:ncol], qsl_of, rhs, start=True, stop=True), reads=[dqT, dkT], writes=[dbk])
                    outs.append((bk, dbk, ncol, bias, off))
                    off += ncol
                yield
                for (bk, dbk, ncol, bias, off) in outs:
                    if bias is not None:
                        em.op("dve", lambda e, bk=bk, off=off, ncol=ncol, bias=bias: e.tensor_tensor(sal[b][0:nq, off:off + ncol], bk[0:nq, 0:ncol], bias, ALU.add), reads=[dbk, dGm], writes=[dsal[b]])
                    else:
                        em.op("act", lambda e, bk=bk, off=off, ncol=ncol: e.activation(sal[b][0:nq, off:off + ncol], bk[0:nq, 0:ncol], AF.Copy), reads=[dbk], writes=[dsal[b]])
                yield
                em.op("dve", lambda e: e.reduce_max(stt[b][0:nq, 1:2], sal[b][0:nq, 0:ntot], AX.X, negate=True), reads=[dsal[b]], writes=[dstt[b]])
                yield
                em.op("act", lambda e: e.activation(pbf[b][0:nq, 0:ntot], sal[b][0:nq, 0:ntot], AF.Exp, bias=stt[b][0:nq, 1:2], scale=1.0, accum_out=stt[b][0:nq, 2:3]),
                      reads=[dsal[b], dstt[b]], writes=[dpbf[b], dstt[b]])
                yield
                nw = ntot // 128
                pb = bkA[:].bitcast(BF16)
                for w in range(nw):
                    em.op("pe", lambda e, w=w: e.transpose(pb[:, 512 + w * nq:512 + (w + 1) * nq], pbf[b][0:nq, w * 128:(w + 1) * 128], self.ident_b[0:nq, 0:nq]),
                          reads=[dpbf[b], self.d_ident_b], writes=[dA])
                yield
                em.op("dve", lambda e: e.tensor_copy(PT[b][:, 0:nw * nq], pb[:, 512:512 + nw * nq]), reads=[dA], writes=[dPT[b]])
                em.op("dve", lambda e: e.reciprocal(stt[b][0:nq, 3:4], stt[b][0:nq, 2:3]), reads=[dstt[b]], writes=[dstt[b]])
                yield
                for w in range(nw):
                    em.op("pe", lambda e, w=w: e.matmul(bkA[0:nq, 448:512], PT[b][:, w * nq:(w + 1) * nq], vsegs[w], start=(w == 0), stop=(w == nw - 1)),
                          reads=[dPT[b], dV, dVs], writes=[dA], inc=(w == nw - 1))
                yield
                em.op("dve", lambda e: e.tensor_scalar(orow[ob][0:nq, ocol:ocol + 64], bkA[0:nq, 448:512], stt[b][0:nq, 3:4], None, ALU.mult), reads=[dA, dstt[b]], writes=[dorow[ob]])
                if tail is not None:
                    tail()
                yield

            units = []
            oi = 0
            if not last:
                for qt in range(2):
                    ob = oi % 3; oi += 1
                    for h in range(6):
                        tail = None
                        if h == 5:
                            tail = (lambda qt=qt, ob=ob: em.dma("sp", self.y_na[qt * 128:(qt + 1) * 128, :], orow[ob][:], reads=[dorow[ob]], writes=[self.d_yna]))
                        units.append(dict(nq=128, qsl_of=qT[:, h, qt * 128:(qt + 1) * 128], segs=[(kT[:, h, 0:LC], LC, None)],
                                          vsegs=[V[:, 0, h * 64:(h + 1) * 64], V[:, 1, h * 64:(h + 1) * 64]], ob=ob, ocol=h * 64, tail=tail))
            for i in range(32):
                r0 = min(max(i - 4, 0), 24)
                dr0 = r0 - i + 7
                ob = oi % 3; oi += 1
                for h in range(6):
                    q0 = LC + i * 64
                    k0 = LC + r0 * 64
                    bias = Gm[:, h, dr0:dr0 + 8, :].rearrange("p a c -> p (a c)")
                    if r0 % 2 == 0:
                        vl = [V[:, 2 + r0 // 2 + w, h * 64:(h + 1) * 64] for w in range(4)]
                    else:
                        vl = [Vs[:, (r0 - 1) // 2 + w, h * 64:(h + 1) * 64] for w in range(4)]
                    vl += [V[:, 0, h * 64:(h + 1) * 64], V[:, 1, h * 64:(h + 1) * 64]]
                    tail = None
                    if h == 5:
                        tail = (lambda i=i, ob=ob: em.dma("sp", self.y_na[LC + i * 64:LC + (i + 1) * 64, :], orow[ob][0:64, :], reads=[dorow[ob]], writes=[self.d_yna]))
                    units.append(dict(nq=64, qsl_of=qT[:, h, q0:q0 + 64], segs=[(kT[:, h, k0:k0 + 512], 512, bias), (kT[:, h, 0:LC], LC, None)],
                                      vsegs=vl, ob=ob, ocol=h * 64, tail=tail))
            active = []
            nxt_u = 0
            while nxt_u < len(units) or active:
                while len(active) < NB - 1 and nxt_u < len(units):
                    active.append(unit(**units[nxt_u])); nxt_u += 1
                for g in list(active):
                    try:
                        next(g)
                    except StopIteration:
                        active.remove(g)
            em.barrier()
        self.tap(f"yna{li}", self.y_na, [T, NA_W], reads=[self.d_yna])


    def hyena(self, li, last):
        seqs = [(SEQ, "l", LC)] + ([] if last else [(LC, "c", 0)])
        for (n, tag, tok0) in seqs:
            self.hyena_seq(li, n, tag, tok0)
        self.tap(f"yhy{li}", self.y_hy, [T, HY_W], reads=[self.d_yhy])

    def hyena_seq(self, li, n, tag, tok0):
        em = self.em
        nch = n // 128
        PI = math.pi
        I = self.I
        with ExitStack() as st:
            Hre = self.sb(st, "Hre", [128, nch, 512], BF16); Him = self.sb(st, "Him", [128, nch, 512], BF16); dH = Dep()
            NTB = 3
            tb = [[self.sb(st, f"tb{i}{j}", [128, nch, 128], BF16) for j in range(2)] for i in range(NTB)]
            dtb = [Dep() for _ in range(NTB)]
            tbi = 0
            with ExitStack() as st2:
                w1 = self.sb(st2, "hyw1", [33, 64]); w2 = self.sb(st2, "hyw2", [64, 64]); w3 = self.sb(st2, "hyw3", [64, 1024]); dw = Dep()
                em.dma("sp", w1[:], I["hy_f_w1"][li], writes=[dw]); em.dma("sp", w2[:], I["hy_f_w2"][li], writes=[dw]); em.dma("sp", w3[:], I["hy_f_w3"][li], writes=[dw])
                cols = self.sb(st2, "hycols", [64, 8]); dc = Dep()
                for j, nm in enumerate(("hy_f_freq", "hy_f_b1", "hy_f_b2")):
                    em.dma("sp", cols[:, j:j + 1], I[nm][li:li + 1, :].rearrange("o n -> n o"), writes=[dc], allow_slow_non_contiguous=True)
                em.op("dve", lambda e: e.tensor_tensor(cols[:, 3:4], cols[:, 1:2], cols[:, 0:1], ALU.mult), reads=[dc], writes=[dc])
                em.op("dve", lambda e: e.tensor_tensor(cols[:, 4:5], cols[:, 2:3], cols[:, 0:1], ALU.mult), reads=[dc], writes=[dc])
                em.op("dve", lambda e: e.memset(cols[:, 5:6], -PI), writes=[dc])
                h2T = self.sb(st2, "h2T", [64, n]); dh1 = Dep(); dh2 = Dep()
                st2a = ExitStack()
                zT = self.sb(st2a, "hyzT", [33, n]); dz = Dep()
                em.dma("sp", zT[:], I["hyz_" + tag], writes=[dz])
                h1T = self.sb(st2a, "h1T", [64, n])
                arg = self.sb(st2a, "hyarg", [64, 512]); t1 = self.sb(st2a, "hyt1", [64, 512]); darg = Dep(); dt1 = Dep()
                for (wm, kk_, src, dsrc, dst, ddst, bcol) in ((w1, 33, zT, dz, h1T, dh1, 3), (w2, 64, h1T, dh1, h2T, dh2, 4)):
                    for s in range(0, n, 512):
                        m = min(512, n - s)
                        ps, dp = self.psum()
                        em.op("pe", lambda e, ps=ps, wm=wm, kk_=kk_, src=src, s=s, m=m: e.matmul(ps[0:64, 0:m], wm[0:kk_, :], src[0:kk_, s:s + m], start=True, stop=True), reads=[dw, dsrc], writes=[dp])
                        em.op("dve", lambda e, ps=ps, m=m, bcol=bcol: e.tensor_scalar(arg[:, 0:m], ps[0:64, 0:m], cols[:, 0:1], cols[:, bcol:bcol + 1], ALU.mult, ALU.add), reads=[dp, dc], writes=[darg])
                        em.op("dve", lambda e, m=m: e.tensor_scalar(t1[:, 0:m], arg[:, 0:m], PI, -2 * PI, ALU.is_gt, ALU.mult), reads=[darg], writes=[dt1])
                        em.op("dve", lambda e, m=m: e.tensor_tensor(t1[:, 0:m], t1[:, 0:m], arg[:, 0:m], ALU.add), reads=[darg, dt1], writes=[dt1])
                        em.op("dve", lambda e, m=m: e.tensor_scalar(arg[:, 0:m], arg[:, 0:m], -PI, 2 * PI, ALU.is_lt, ALU.mult), reads=[darg, dt1], writes=[darg])
                        em.op("dve", lambda e, m=m: e.tensor_tensor(t1[:, 0:m], t1[:, 0:m], arg[:, 0:m], ALU.add), reads=[darg, dt1], writes=[dt1])
                        em.op("dve", lambda e, m=m: e.tensor_scalar(t1[:, 0:m], t1[:, 0:m], PI, -PI, ALU.min, ALU.max), reads=[dt1], writes=[dt1])
                        em.op("act", lambda e, m=m, dst=dst, s=s: e.activation(dst[:, s:s + m], t1[:, 0:m], AF.Sin), reads=[dt1], writes=[ddst])
                em.barrier()
                st2a.close()
                hF = self.sb(st2, "hF", [128, nch, 1024]); dhF = Dep()
                dec = self.sb(st2, "hydec", [128, nch, 256]); ddec = Dep()
                em.dma("sp", dec[:], I["hydec_" + tag], writes=[ddec])
                ones, dones = self.load_const(st2, "ones_f")
                for pc in range(nch):
                    for blk in range(2):
                        ps, dp = self.psum()
                        em.op("pe", lambda e, ps=ps, pc=pc, blk=blk: e.matmul(ps[:], h2T[:, pc * 128:(pc + 1) * 128], w3[:, blk * 512:(blk + 1) * 512], start=True, stop=True), reads=[dh2, dw], writes=[dp])
                        em.op("dve", lambda e, ps=ps, pc=pc, blk=blk: e.tensor_tensor(hF[:, pc, blk * 512:(blk + 1) * 512].rearrange("p (a c) -> p a c", a=2), ps[:].rearrange("p (a c) -> p a c", a=2),
                                                                                     dec[:, pc, :].rearrange("p (a c) -> p a c", a=1).to_broadcast([128, 2, 256]), ALU.mult), reads=[dp, ddec], writes=[dhF])
                em.op("dve", lambda e: e.memset(hF[0:1, 0, 256:512], 0.0), reads=[dhF], writes=[dhF])
                em.op("dve", lambda e: e.memset(hF[0:1, 0, 768:1024], 0.0), reads=[dhF], writes=[dhF])
                ab = [self.sb(st2, f"hyabs{i}", [128, 1024]) for i in range(2)]; dab = [Dep(), Dep()]
                pl = [self.psum(), self.psum()]
                for pc in range(nch):
                    b = pc % 2
                    em.op("act", lambda e, pc=pc, b=b: e.activation(ab[b][:], hF[:, pc, :], AF.Abs), reads=[dhF], writes=[dab[b]])
                    for blk in range(2):
                        em.op("pe", lambda e, pc=pc, blk=blk, b=b: e.matmul(pl[blk][0][:], ones[:], ab[b][:, blk * 512:(blk + 1) * 512], start=(pc == 0), stop=(pc == nch - 1)), reads=[dab[b], dones], writes=[pl[blk][1]])
                rl = self.sb(st2, "hyrl", [128, 2, 256]); drl = Dep()
                for o in range(2):
                    em.op("dve", lambda e, o=o: e.tensor_copy(rl[:, o, :], pl[o][0][:, 0:256]), reads=[pl[o][1]], writes=[drl])
                    em.op("dve", lambda e, o=o: e.tensor_tensor(rl[:, o, :], rl[:, o, :], pl[o][0][:, 256:512], ALU.add), reads=[pl[o][1], drl], writes=[drl])
                em.op("dve", lambda e: e.reciprocal(rl[:], rl[:]), reads=[drl], writes=[drl])
                he = self.sb(st2, "hye", [128, nch, 512], BF16); ho = self.sb(st2, "hyo", [128, nch, 512], BF16); dhe = Dep()
                tmpf = self.sb(st2, "hytmp", [128, 2, 256]); dtmpf = Dep()
                for pc in range(nch):
                    hv = hF[:, pc, :].rearrange("p (o d c) -> p o d c", o=2, d=2)
                    em.op("dve", lambda e, hv=hv: e.tensor_tensor(tmpf[:], hv[:, :, 0, :], hv[:, :, 1, :], ALU.add), reads=[dhF], writes=[dtmpf])
                    em.op("pool", lambda e, pc=pc: e.tensor_tensor(he[:, pc, :].rearrange("p (o c) -> p o c", o=2), tmpf[:], rl[:], ALU.mult), reads=[dtmpf, drl], writes=[dhe])
                    em.op("dve", lambda e, hv=hv: e.tensor_tensor(tmpf[:], hv[:, :, 0, :], hv[:, :, 1, :], ALU.subtract), reads=[dhF, dhe], writes=[dtmpf])
                    em.op("pool", lambda e, pc=pc: e.tensor_tensor(ho[:, pc, :].rearrange("p (o c) -> p o c", o=2), tmpf[:], rl[:], ALU.mult), reads=[dtmpf, drl], writes=[dhe])
                for fc in range(nch):
                    b = tbi % NTB; tbi += 1
                    em.dma("sp", tb[b][0][:], I["cm_" + tag][fc], writes=[dtb[b]])
                    em.dma("sp", tb[b][1][:], I["smn_" + tag][fc], writes=[dtb[b]])
                    for j, (src, dstH) in enumerate(((he, Hre), (ho, Him))):
                        ps, dp = self.psum()
                        for tc in range(nch):
                            em.op("pe", lambda e, ps=ps, tc=tc, b=b, j=j, src=src: e.matmul(ps[:], tb[b][j][:, tc, :], src[:, tc, :], start=(tc == 0), stop=(tc == nch - 1)), reads=[dtb[b], dhe], writes=[dp], inc=(tc == nch - 1))
                        em.op("act" if j == 0 else "dve", (lambda e, ps=ps, dstH=dstH, fc=fc: e.activation(dstH[:, fc, :], ps[:], AF.Copy)) if j == 0 else
                              (lambda e, ps=ps, dstH=dstH, fc=fc: e.tensor_copy(dstH[:, fc, :], ps[:])), reads=[dp], writes=[dH])
                em.barrier()
            cw = self.sb(st, "hycw", [128, 3, HY_IN]); dcw = Dep()
            for j in range(3):
                em.dma("sp", cw[:, j, :], row_bc(I["hy_conv_w"][li, j:j + 1, :]), writes=[dcw])
            cbt, dcb = self.load_bc(st, "hycb", I["hy_conv_b"][li:li + 1, :], HY_IN)
            sk = self.sb(st, "hysk", [128, 2, HY_W]); dsk = Dep()
            for o in range(2):
                em.dma("sp", sk[:, o, :], row_bc(I["hy_skip"][li, o:o + 1, :]), writes=[dsk])
            z = self.sb(st, "hyz", [128, nch, HY_W]); dzz = Dep()
            zb = self.sb(st, "hyzb", [128, nch, HY_W], BF16); dzb = Dep()
            xg = self.sb(st, "hyxg", [128, nch, 2, HY_W]); dxg = Dep()
            Yre = self.sb(st, "Yre", [128, nch, HY_W], BF16); Yim = self.sb(st, "Yim", [128, nch, HY_W], BF16); dY = Dep()
            with ExitStack() as st3:
                pcn = [[self.sb(st3, f"hyl{i}{j}", [128, HY_IN]) for j in range(3)] for i in range(2)]; dpcn = [Dep(), Dep()]
                acc = self.sb(st3, "hyacc", [128, HY_IN]); dacc = Dep()
                tm = self.sb(st3, "hytm", [128, HY_IN]); dtm = Dep()
                for tc in range(nch):
                    b = tc % 2
                    ur = urow(tok0 + tc * 128)
                    for j in range(3):
                        em.dma("sp", pcn[b][j][:], self.u_tok[ur + j - 1:ur + j - 1 + 128, 0:HY_IN], reads=[self.d_utok], writes=[dpcn[b]])
                    em.op("dve", lambda e, b=b: e.tensor_tensor(acc[:], pcn[b][0][:], cw[:, 0, :], ALU.mult), reads=[dpcn[b], dcw], writes=[dacc])
                    for j in (1, 2):
                        em.op("pool", lambda e, b=b, j=j: e.tensor_tensor(tm[:], pcn[b][j][:], cw[:, j, :], ALU.mult), reads=[dpcn[b], dcw], writes=[dtm])
                        em.op("dve", lambda e: e.tensor_tensor(acc[:], acc[:], tm[:], ALU.add), reads=[dtm, dacc], writes=[dacc])
                    em.op("dve", lambda e, tc=tc: e.tensor_tensor(z[:, tc, :], acc[:, 0:256], cbt[:, 0:256], ALU.add), reads=[dacc, dcb], writes=[dzz])
                    em.op("act", lambda e, tc=tc: e.activation(zb[:, tc, :], z[:, tc, :], AF.Copy), reads=[dzz], writes=[dzb])
                    em.op("pool", lambda e, tc=tc: e.tensor_tensor(xg[:, tc, :, :].rearrange("p a c -> p (a c)"), acc[:, 256:768], cbt[:, 256:768], ALU.add), reads=[dacc, dcb], writes=[dxg])
                em.barrier()
            pa2 = [self.sb(st, f"hypa{i}", [128, HY_W]) for i in range(2)]; pb2 = [self.sb(st, f"hypb{i}", [128, HY_W]) for i in range(2)]
            pc2 = [self.sb(st, f"hypc{i}", [128, HY_W]) for i in range(2)]; pd2 = [self.sb(st, f"hypd{i}", [128, HY_W]) for i in range(2)]
            dpa2 = [Dep(), Dep()]; dpb2 = [Dep(), Dep()]; dpc2 = [Dep(), Dep()]; dpd2 = [Dep(), Dep()]
            ot = [self.sb(st, f"hyot{i}", [128, HY_W]) for i in range(2)]; dot = [Dep(), Dep()]
            for o in range(2):
                for fc in range(nch):
                    b = tbi % NTB; tbi += 1
                    em.dma("sp", tb[b][0][:], I["cm_" + tag][fc], writes=[dtb[b]])
                    em.dma("sp", tb[b][1][:], I["smn_" + tag][fc], writes=[dtb[b]])
                    pr, dpr = self.psum(); pi, dpi = self.psum()
                    for tc in range(nch):
                        em.op("pe", lambda e, pr=pr, tc=tc, b=b: e.matmul(pr[:, 0:256], tb[b][0][:, tc, :], zb[:, tc, :], start=(tc == 0), stop=(tc == nch - 1)), reads=[dtb[b], dzb], writes=[dpr], inc=(tc == nch - 1))
                    for tc in range(nch):
                        em.op("pe", lambda e, pi=pi, tc=tc, b=b: e.matmul(pi[:, 0:256], tb[b][1][:, tc, :], zb[:, tc, :], start=(tc == 0), stop=(tc == nch - 1)), reads=[dtb[b], dzb], writes=[dpi], inc=(tc == nch - 1))
                    hr = Hre[:, fc, o * 256:(o + 1) * 256]; hi = Him[:, fc, o * 256:(o + 1) * 256]
                    pa, pb_, pc_, pd_ = pa2[fc % 2], pb2[fc % 2], pc2[fc % 2], pd2[fc % 2]
                    dpa, dpb, dpc, dpd = dpa2[fc % 2], dpb2[fc % 2], dpc2[fc % 2], dpd2[fc % 2]
                    em.op("dve", lambda e, pr=pr, hr=hr: e.tensor_tensor(pa[:], pr[:, 0:256], hr, ALU.mult), reads=[dpr, dH], writes=[dpa])
                    em.op("dve", lambda e, pi=pi, hi=hi: e.tensor_tensor(pb_[:], pi[:, 0:256], hi, ALU.mult), reads=[dpi, dH], writes=[dpb])
                    em.op("pool", lambda e, fc=fc: e.tensor_tensor(Yre[:, fc, :], pa[:], pb_[:], ALU.subtract), reads=[dpa, dpb], writes=[dY])
                    em.op("dve", lambda e, pr=pr, hi=hi: e.tensor_tensor(pc_[:], pr[:, 0:256], hi, ALU.mult), reads=[dpr, dH], writes=[dpc])
                    em.op("dve", lambda e, pi=pi, hr=hr: e.tensor_tensor(pd_[:], pi[:, 0:256], hr, ALU.mult), reads=[dpi, dH], writes=[dpd])
                    em.op("pool", lambda e, fc=fc: e.tensor_tensor(Yim[:, fc, :], pc_[:], pd_[:], ALU.add), reads=[dpc, dpd], writes=[dY])
                for tc in range(nch):
                    b = tbi % NTB; tbi += 1
                    em.dma("sp", tb[b][0][:], I["icm_" + tag][tc], writes=[dtb[b]])
                    em.dma("sp", tb[b][1][:], I["ismn_" + tag][tc], writes=[dtb[b]])
                    ps, dp = self.psum()
                    for fc in range(nch):
                        em.op("pe", lambda e, ps=ps, fc=fc, b=b: e.matmul(ps[:, 0:256], tb[b][0][:, fc, :], Yre[:, fc, :], start=(fc == 0), stop=False), reads=[dtb[b], dY], writes=[dp])
                    for fc in range(nch):
                        em.op("pe", lambda e, ps=ps, fc=fc, b=b: e.matmul(ps[:, 0:256], tb[b][1][:, fc, :], Yim[:, fc, :], start=False, stop=(fc == nch - 1)), reads=[dtb[b], dY], writes=[dp], inc=(fc == nch - 1))
                    pa, dpa = pa2[tc % 2], dpa2[tc % 2]
                    em.op("pool", lambda e, tc=tc, o=o, pa=pa: e.tensor_tensor(pa[:], z[:, tc, :], sk[:, o, :], ALU.mult), reads=[dzz, dsk], writes=[dpa])
                    em.op("dve", lambda e, ps=ps, pa=pa: e.tensor_tensor(pa[:], pa[:], ps[:, 0:256], ALU.add), reads=[dp, dpa], writes=[dpa])
                    if o == 0:
                        em.op("pool", lambda e, tc=tc, o=o, pa=pa: e.tensor_tensor(z[:, tc, :], pa[:], xg[:, tc, o, :], ALU.mult), reads=[dpa, dxg], writes=[dzz])
                        em.op("act", lambda e, tc=tc: e.activation(zb[:, tc, :], z[:, tc, :], AF.Copy), reads=[dzz], writes=[dzb])
                    else:
                        ob = tc % 2
                        em.op("pool", lambda e, tc=tc, o=o, ob=ob, pa=pa: e.tensor_tensor(ot[ob][:], pa[:], xg[:, tc, o, :], ALU.mult), reads=[dpa, dxg], writes=[dot[ob]])
                        r0 = tok0 + tc * 128
                        em.dma("sp", self.y_hy[r0:r0 + 128, :], ot[ob][:], reads=[dot[ob]], writes=[self.d_yhy])
            em.barrier()


    def rwkv(self, li, last):
        em = self.em
        I = self.I
        NCH = T // 64
        CD = 0.6065306597126334
        U = self.u_rwT
        with ExitStack() as st:
            def colload(dst, j, src_row):
                em.dma("sp", dst[:, j:j + 1], src_row.rearrange("o n -> n o"), writes=[dcols], allow_slow_non_contiguous=True)

            def shift(dst, ddst, src, dsrc, P_, f0):
                mu = self.sb(st_sh, "mu", [P_, 4]); dmu = Dep()
                for j in range(2):
                    em.dma("sp", mu[:, j:j + 1], I["rw_shift"][li, j:j + 1, f0:f0 + P_].rearrange("o n -> n o"), writes=[dmu], allow_slow_non_contiguous=True)
                em.op("dve", lambda e: e.tensor_tensor(mu[:, 2:3], mu[:, 0:1], mu[:, 1:2], ALU.add), reads=[dmu], writes=[dmu])
                em.op("dve", lambda e: e.tensor_scalar(mu[:, 2:3], mu[:, 2:3], -1.0, 1.0, ALU.mult, ALU.add), reads=[dmu], writes=[dmu])
                em.op("dve", lambda e: e.tensor_scalar(dst[:], src[:], mu[:, 2:3], None, ALU.mult), reads=[dsrc, dmu], writes=[ddst])
                for (a0, a1) in ((0, LC), (LC, T)):
                    em.op("dve", lambda e, a0=a0, a1=a1: e.scalar_tensor_tensor(dst[:, a0 + 1:a1], src[:, a0:a1 - 1], mu[:, 0:1], dst[:, a0 + 1:a1], ALU.mult, ALU.add), reads=[dsrc, dmu, ddst], writes=[ddst])
                    em.op("dve", lambda e, a0=a0, a1=a1: e.scalar_tensor_tensor(dst[:, a0:a1 - 1], src[:, a0 + 1:a1], mu[:, 1:2], dst[:, a0:a1 - 1], ALU.mult, ALU.add), reads=[dsrc, dmu, ddst], writes=[ddst])

            st_sh = st
            lwt = self.sb(st, "lwt", [64, 2, T], BF16); las = self.sb(st, "las", [64, 2, T], BF16); lgs = self.sb(st, "lgs", [128, T], BF16); dsh = Dep()
            w2b = self.sb(st, "rw2b", [64, 2, RW_W], BF16); a2b = self.sb(st, "ra2b", [64, 2, RW_W], BF16); g2b = self.sb(st, "rg2b", [128, RW_W], BF16); dwl = Dep()
            for d in range(2):
                em.dma("pool", w2b[:, d, :], I["rw_w2"][li, d], writes=[dwl])
                em.dma("pool", a2b[:, d, :], I["rw_a2"][li, d], writes=[dwl])
            em.dma("pool", g2b[:], I["rw_g2"][li], writes=[dwl])
            raw = self.sb(st, "rwraw", [128, T]); draw = Dep()
            T1 = self.sb(st, "rwT1", [128, T]); dT1 = Dep()
            T2 = self.sb(st, "rwT2", [64, T]); dT2 = Dep()
            for d in range(2):
                for (f0, dstb, fn) in ((1152 + d * 64, lwt, AF.Tanh), (1280 + d * 64, las, AF.Copy)):
                    em.dma("sp", raw[0:64, :], U[f0:f0 + 64, :], reads=[self.d_urw], writes=[draw])
                    shift(T1[0:64, :], dT1, raw[0:64, :], draw, 64, f0)
                    em.op("act", lambda e, d=d, dstb=dstb, fn=fn: e.activation(dstb[:, d, :], T1[0:64, :], fn), reads=[dT1], writes=[dsh])
            em.dma("sp", raw[:], U[1408:1536, :], reads=[self.d_urw], writes=[draw])
            shift(T1, dT1, raw, draw, 128, 1408)
            em.op("act", lambda e: e.activation(lgs[:], T1[:], AF.Sigmoid), reads=[dT1], writes=[dsh])
            ropeC, drc = self.load_const(st, "rope_cos"); ropeS, drs = self.load_const(st, "rope_sin"); PT_, dPT = self.load_const(st, "rope_PT")
            ones, dones = self.load_const(st, "ones_f")
            cmask, dcmask = self.load_const(st, "rw_cmask")
            mks = {}
            for d, (a, b_, p) in enumerate((("mk_f_strict", "mk_f_incl", "mk_b_strict"), ("mk_b_strict", "mk_b_incl", "mk_f_strict"))):
                m2 = self.sb(st, f"mk2_{d}", [64, 128]); mp = self.sb(st, f"mkp_{d}", [64, 64]); dmk = Dep()
                em.dma("sp", m2[:, 0:64], I[a], writes=[dmk]); em.dma("sp", m2[:, 64:128], I[b_], writes=[dmk]); em.dma("sp", mp[:], I[p], writes=[dmk])
                mks[d] = (m2, mp, dmk)
            idf = self.ident_f
            r_ = self.sb(st, "rw_r", [64, T]); k_ = self.sb(st, "rw_k", [64, T]); kk = self.sb(st, "rw_kk", [64, T]); dr_ = Dep(); dk_ = Dep(); dkk = Dep()
            Vt = self.sb(st, "rw_Vt", [64, NCH, 64], BF16); dVt = Dep()
            yacc = self.sb(st, "rw_y", [64, NCH, 64]); dy = Dep()
            sg = self.sb(st, "rw_sg", [64, T]); kd = self.sb(st, "rw_kd", [64, T], BF16); bd = self.sb(st, "rw_bd", [64, T], BF16); dsg = Dep(); dkd = Dep(); dbd = Dep()
            TB = self.sb(st, "rw_TB", [64, T], BF16); dTB = Dep()
            coef = self.sb(st, "rw_coef", [64, NCH]); dcoef = Dep()
            cols = self.sb(st, "rw_cols", [64, 12]); dcols = Dep()
            gnb = self.sb(st, "rw_gn", [64, 2, 64]); dgn = Dep()
            st_ = self.sb(st, "rw_st", [64, NCH, 2])
            DD = []
            for d in range(2):
                o = {}
                o["AR"] = self.sb(st, f"rw_AR{d}", [64, NCH, 128], BF16); o["dAR"] = Dep()
                o["bdb"] = self.sb(st, f"rw_bdb{d}", [64, T], BF16); o["kdb"] = self.sb(st, f"rw_kdb{d}", [64, T], BF16); o["dbdb"] = Dep(); o["dkdb"] = Dep()
                o["Bh"] = self.sb(st, f"rw_Bh{d}", [64, NCH, 64], BF16); o["Kh"] = self.sb(st, f"rw_Kh{d}", [64, NCH, 64], BF16); o["dBh"] = Dep(); o["dKh"] = Dep()
                o["gC"] = self.sb(st, f"rw_gC{d}", [64, NCH]); o["dgC"] = Dep()
                o["S"] = [self.sb(st, f"rw_S{d}{i}", [64, 64]) for i in range(2)]; o["dS"] = [Dep(), Dep()]
                o["Sb"] = [self.sb(st, f"rw_Sb{d}{i}", [64, 64], BF16) for i in range(2)]; o["dSb"] = [Dep(), Dep()]
                o["MB"] = [self.sb(st, f"rw_MB{d}{i}", [64, 128], BF16) for i in range(2)]; o["dMB"] = [Dep(), Dep()]
                o["MK"] = [self.sb(st, f"rw_MK{d}{i}", [64, 128], BF16) for i in range(2)]; o["dMK"] = [Dep(), Dep()]
                o["QP"] = [self.sb(st, f"rw_QP{d}{i}", [64, 128], BF16) for i in range(4)]; o["dQP"] = [Dep() for _ in range(4)]
                o["R"] = [self.sb(st, f"rw_R{d}{i}", [64, 64], BF16) for i in range(2)]; o["dR"] = [Dep(), Dep()]
                o["X"] = [self.sb(st, f"rw_X{d}{i}", [64, 64], BF16) for i in range(2)]; o["dX"] = [Dep(), Dep()]
                o["U"] = [self.sb(st, f"rw_U{d}{i}", [64, 64], BF16) for i in range(2)]; o["dU"] = [Dep(), Dep()]
                DD.append(o)
            T13 = T1[0:64, :].rearrange("p (c t) -> p c t", t=64); T23 = T2[:].rearrange("p (c t) -> p c t", t=64)
            TB3 = TB[:].rearrange("p (c t) -> p c t", t=64)
            v3 = lambda a: a[:].rearrange("p (c t) -> p c t", t=64)
            idb = self.ident_b
            for d in range(2):
                DD[d]["bankI"] = (self.ps[4 + 2 * d], self.psd[4 + 2 * d])
                DD[d]["bankC"] = (self.ps[5 + 2 * d], self.psd[5 + 2 * d])
            self.ps_n = 4

            def transp_chunks(src3, dsrc, dst, ddst):
                for c0 in range(0, NCH, 8):
                    nb = min(8, NCH - c0)
                    ps, dp = self.psum(); pb = ps[:].bitcast(BF16)
                    for j in range(nb):
                        em.op("pe", lambda e, pb=pb, j=j, c0=c0: e.transpose(pb[0:64, j * 64:(j + 1) * 64], src3[:, c0 + j, :], idb[0:64, 0:64]), reads=[dsrc, self.d_ident_b], writes=[dp])
                    em.op("act", lambda e, pb=pb, c0=c0, nb=nb: e.activation(dst[:, c0:c0 + nb, :], pb[0:64, 0:nb * 64].rearrange("p (c k) -> p c k", k=64), AF.Copy), reads=[dp], writes=[ddst])

            for h in range(6):
                hs = slice(h * 64, (h + 1) * 64)
                for j, (nm, d) in enumerate((("rw_w0", 0), ("rw_w0", 1), ("rw_a0", 0), ("rw_a0", 1))):
                    colload(cols, j, I[nm][li, d:d + 1, hs])
                for j, nm in enumerate(("rw_kk", "rw_ka", "rw_rk")):
                    colload(cols, 4 + j, I[nm][li:li + 1, hs])
                em.op("dve", lambda e: e.tensor_scalar(cols[:, 7:8], cols[:, 5:6], -1.0, 1.0, ALU.mult, ALU.add), reads=[dcols], writes=[dcols])
                em.dma("sp", gnb[:, 0, :], row_bc(I["rw_gn_g"][li:li + 1, hs], 64), writes=[dgn])
                em.dma("sp", gnb[:, 1, :], row_bc(I["rw_gn_b"][li:li + 1, hs], 64), writes=[dgn])
                em.op("pool", lambda e: e.memset(yacc[:], 0.0), writes=[dy])
                for (f0, dst, ddst, rope) in ((h * 64, r_, dr_, True), (384 + h * 64, k_, dk_, True), (768 + h * 64, T2, dT2, False)):
                    em.dma("sp", raw[0:64, :], U[f0:f0 + 64, :], reads=[self.d_urw], writes=[draw])
                    shift(dst, ddst, raw[0:64, :], draw, 64, f0)
                    if rope:
                        for s in range(0, SEQ, 512):
                            ps, dp = self.psum()
                            em.op("pe", lambda e, ps=ps, s=s, dst=dst: e.matmul(ps[0:64, :], PT_[:], dst[:, LC + s:LC + s + 512], start=True, stop=True), reads=[ddst, dPT], writes=[dp])
                            em.op("dve", lambda e, ps=ps, s=s: e.tensor_tensor(T1[0:64, s:s + 512], ps[0:64, :], ropeS[:, s:s + 512], ALU.mult), reads=[dp, drs], writes=[dT1])
                            em.op("pool", lambda e, s=s, dst=dst: e.tensor_tensor(dst[:, LC + s:LC + s + 512], dst[:, LC + s:LC + s + 512], ropeC[:, s:s + 512], ALU.mult), reads=[ddst, drc], writes=[ddst])
                            em.op("dve", lambda e, s=s, dst=dst: e.tensor_tensor(dst[:, LC + s:LC + s + 512], dst[:, LC + s:LC + s + 512], T1[0:64, s:s + 512], ALU.add), reads=[ddst, dT1], writes=[ddst])
                em.op("act", lambda e: e.activation(TB[:], T2[:], AF.Copy), reads=[dT2], writes=[dTB])
                transp_chunks(TB3, dTB, Vt, dVt)
                em.op("dve", lambda e: e.tensor_scalar(kk[:], k_[:], cols[:, 4:5], None, ALU.mult), reads=[dk_, dcols], writes=[dkk])
                em.op("act", lambda e: e.activation(T1[0:64, :], kk[:], AF.Square), reads=[dkk], writes=[dT1])
                for s in range(0, T, 512):
                    n = min(512, T - s)
                    ps, dp = self.psum()
                    em.op("pe", lambda e, ps=ps, s=s, n=n: e.matmul(ps[0:64, 0:n], ones[0:64, 0:64], T1[0:64, s:s + n], start=True, stop=True), reads=[dT1, dones], writes=[dp])
                    em.op("act", lambda e, ps=ps, s=s, n=n: e.activation(T2[:, s:s + n], ps[0:64, 0:n], AF.Sqrt), reads=[dp], writes=[dT2])
                em.op("dve", lambda e: e.tensor_scalar(T2[:], T2[:], 1e-12, None, ALU.max), reads=[dT2], writes=[dT2])
                em.op("dve", lambda e: e.reciprocal(T2[:], T2[:]), reads=[dT2], writes=[dT2])
                em.op("dve", lambda e: e.tensor_tensor(kk[:], kk[:], T2[:], ALU.mult), reads=[dT2, dkk], writes=[dkk])
                for d in range(2):
                    o = DD[d]
                    AR, dAR, gC, dgC = o["AR"], o["dAR"], o["gC"], o["dgC"]
                    for s in range(0, T, 512):
                        n = min(512, T - s)
                        ps, dp = self.psum()
                        em.op("pe", lambda e, ps=ps, s=s, n=n, d=d: e.matmul(ps[0:64, 0:n], w2b[:, d, hs], lwt[:, d, s:s + n], start=True, stop=True), reads=[dwl, dsh], writes=[dp])
                        em.op("act", lambda e, ps=ps, s=s, n=n, d=d: e.activation(sg[:, s:s + n], ps[0:64, 0:n], AF.Sigmoid, bias=cols[:, d:d + 1]), reads=[dp, dcols], writes=[dsg])
                        ps, dp = self.psum()
                        em.op("pe", lambda e, ps=ps, s=s, n=n, d=d: e.matmul(ps[0:64, 0:n], a2b[:, d, hs], las[:, d, s:s + n], start=True, stop=True), reads=[dwl, dsh], writes=[dp])
                        em.op("act", lambda e, ps=ps, s=s, n=n, d=d: e.activation(bd[:, s:s + n], ps[0:64, 0:n], AF.Sigmoid, bias=cols[:, 2 + d:3 + d]), reads=[dp, dcols], writes=[dbd])
                    em.op("dve", lambda e: e.tensor_scalar(kd[:], bd[:], cols[:, 5:6], cols[:, 7:8], ALU.mult, ALU.add), reads=[dbd, dcols], writes=[dkd])
                    em.op("dve", lambda e: e.tensor_tensor(kd[:], kd[:], k_[:], ALU.mult), reads=[dkd, dk_], writes=[dkd])
                    em.op("pool", lambda e: e.tensor_tensor(bd[:], bd[:], kk[:], ALU.mult), reads=[dbd, dkk], writes=[dbd])
                    em.op("dve", lambda e: e.scalar_tensor_tensor(T1[0:64, :], kd[:], cols[:, 6:7], r_[:], ALU.mult, ALU.mult), reads=[dkd, dr_, dcols], writes=[dT1])
                    ps, dp = self.psum()
                    for c in range(NCH):
                        em.op("pe", lambda e, ps=ps, c=c: e.matmul(ps[0:64, c:c + 1], T13[:, c, :], ones[0:64, 0:1], start=True, stop=True), reads=[dT1, dones], writes=[dp])
                    if d == 0:
                        em.op("dve", lambda e, ps=ps: e.tensor_copy(coef[:], ps[0:64, 0:NCH]), reads=[dp], writes=[dcoef])
                    else:
                        em.op("dve", lambda e, ps=ps: e.tensor_tensor(coef[:], coef[:], ps[0:64, 0:NCH], ALU.add), reads=[dp, dcoef], writes=[dcoef])
                    em.op("dve", lambda e: e.tensor_tensor_scan(T2[:], cmask[:], sg[:], 0.0, ALU.mult, ALU.add), reads=[dsg, dcmask], writes=[dT2])
                    if d == 0:
                        em.op("dve", lambda e: e.tensor_tensor(T1[0:64, :], T2[:], sg[:], ALU.subtract), reads=[dT2, dsg], writes=[dT1])
                    else:
                        em.op("dve", lambda e: e.scalar_tensor_tensor(T13, T23, -1.0, T23[:, :, 63:64].to_broadcast([64, NCH, 64]), ALU.mult, ALU.add), reads=[dT2], writes=[dT1])
                        em.op("dve", lambda e: e.tensor_tensor(T2[:], T1[0:64, :], sg[:], ALU.add), reads=[dT1, dsg], writes=[dT2])
                    em.op("act", lambda e: e.activation(T1[0:64, :], T1[0:64, :], AF.Exp, scale=-CD), reads=[dT1], writes=[dT1])
                    em.op("dve", lambda e, AR=AR: e.scalar_tensor_tensor(AR[:, :, 0:64], v3(kk), -1.0, T13, ALU.mult, ALU.mult), reads=[dkk, dT1], writes=[dAR])
                    em.op("act", lambda e: e.activation(T1[0:64, :], T2[:], AF.Exp, scale=-CD), reads=[dT2, dAR], writes=[dT1])
                    em.op("dve", lambda e, AR=AR: e.tensor_tensor(AR[:, :, 64:128], v3(r_), T13, ALU.mult), reads=[dr_, dT1], writes=[dAR])
                    gsel = 63 if d == 0 else 0
                    em.op("dve", lambda e, gsel=gsel, gC=gC: e.tensor_copy(gC[:], T13[:, :, gsel]), reads=[dT1], writes=[dgC])
                    em.op("act", lambda e: e.activation(T2[:], T2[:], AF.Exp, scale=CD), reads=[dT2], writes=[dT2])
                    em.op("dve", lambda e, o=o: e.tensor_tensor(o["bdb"][:], bd[:], T2[:], ALU.mult), reads=[dbd, dT2], writes=[o["dbdb"]])
                    em.op("pool", lambda e, o=o: e.tensor_tensor(o["kdb"][:], kd[:], T2[:], ALU.mult), reads=[dkd, dT2], writes=[o["dkdb"]])
                    gbc = gC[:].rearrange("p (c o) -> p c o", o=1).to_broadcast([64, NCH, 64])
                    em.op("dve", lambda e, o=o, gbc=gbc: e.tensor_tensor(TB3, v3(o["bdb"]), gbc, ALU.mult), reads=[o["dbdb"], dgC], writes=[dTB])
                    transp_chunks(TB3, dTB, o["Bh"], o["dBh"])
                    em.op("dve", lambda e, o=o, gbc=gbc: e.tensor_tensor(TB3, v3(o["kdb"]), gbc, ALU.mult), reads=[o["dkdb"], dgC], writes=[dTB])
                    transp_chunks(TB3, dTB, o["Kh"], o["dKh"])
                    em.op("pool", lambda e, o=o: e.memset(o["S"][0][:], 0.0), writes=[o["dS"][0]])
                    em.op("pool", lambda e, o=o: e.memset(o["Sb"][0][:], 0.0), writes=[o["dSb"][0]])

                def indep(d, ci, c):
                    o = DD[d]
                    m2, mp, dmk = mks[d]
                    AR, dAR = o["AR"], o["dAR"]
                    MB, MK, QP, Rm = o["MB"], o["MK"], o["QP"], o["R"]
                    dMB, dMK, dQP, dRm = o["dMB"], o["dMK"], o["dQP"], o["dR"]
                    bd3 = v3(o["bdb"]); kd3 = v3(o["kdb"])
                    b = ci % 2
                    A = AR[:, c, 0:64]; BT = bd3[:, c, :]; KT = kd3[:, c, :]
                    bk, dbk = o["bankI"]
                    q0 = (ci * 2) % 4
                    em.op("pe", lambda e: e.matmul(bk[0:64, 0:128], BT, AR[:, c, :], start=True, stop=True), reads=[o["dbdb"], dAR], writes=[dbk])
                    em.op("pe", lambda e: e.matmul(bk[0:64, 128:256], KT, AR[:, c, :], start=True, stop=True), reads=[o["dkdb"], dAR], writes=[dbk])
                    em.op("pe", lambda e: e.matmul(bk[0:64, 256:320], A, BT, start=True, stop=True), reads=[o["dbdb"], dAR], writes=[dbk])
                    yield
                    em.op("dve", lambda e: e.tensor_tensor(MB[b][:], bk[0:64, 0:128], m2[:], ALU.mult), reads=[dbk, dmk], writes=[dMB[b]])
                    em.op("dve", lambda e: e.tensor_tensor(QP[q0][:, 64:128], bk[0:64, 256:320], mp[:], ALU.mult), reads=[dbk, dmk], writes=[dQP[q0]])
                    em.op("dve", lambda e: e.tensor_tensor(MK[b][:], bk[0:64, 128:256], m2[:], ALU.mult), reads=[dbk, dmk], writes=[dMK[b]])
                    yield
                    em.op("pool", lambda e: e.tensor_copy(QP[q0][:, 0:64], MB[b][:, 0:64]), reads=[dMB[b]], writes=[dQP[q0]])
                    em.op("pool", lambda e: e.tensor_tensor(Rm[b][:], MB[b][:, 0:64], idb[0:64, 0:64], ALU.add), reads=[dMB[b], self.d_ident_b], writes=[dRm[b]])
                    yield
                    cur = q0
                    for j in range(1, 6):
                        nxt = q0 + (1 if cur == q0 else 0)
                        pq, dpq = bk[:, 320:448], dbk
                        if j < 5:
                            em.op("pe", lambda e, pq=pq, cur=cur: e.matmul(pq[0:64, 0:64], QP[cur][:, 64:128], QP[cur][:, 0:64], start=True, stop=True), reads=[dQP[cur]], writes=[dpq])
                        em.op("pe", lambda e, pq=pq, cur=cur: e.matmul(pq[0:64, 64:128], QP[cur][:, 0:64], QP[cur][:, 64:128], start=True, stop=True), reads=[dQP[cur]], writes=[dpq])
                        lo = 0 if j < 5 else 64
                        em.op("act", lambda e, pq=pq, nxt=nxt, lo=lo: e.activation(QP[nxt][:, lo:128], pq[0:64, lo:128], AF.Copy), reads=[dpq], writes=[dQP[nxt]])
                        yield
                        pr, dpr = bk[:, 448:512], dbk
                        em.op("pe", lambda e, pr=pr, nxt=nxt: e.matmul(pr[0:64, 0:64], QP[nxt][:, 64:128], Rm[b][:], start=True, stop=True), reads=[dQP[nxt], dRm[b]], writes=[dpr])
                        em.op("dve", lambda e, pr=pr: e.tensor_tensor(Rm[b][:], pr[0:64, 0:64], Rm[b][:], ALU.add), reads=[dpr, dRm[b]], writes=[dRm[b]])
                        cur = nxt
                        yield

                def chain(d, ci, c):
                    o = DD[d]
                    AR, dAR = o["AR"], o["dAR"]
                    MB, MK, Rm, Xs, Us = o["MB"], o["MK"], o["R"], o["X"], o["U"]
                    dMB, dMK, dRm, dXs, dUs = o["dMB"], o["dMK"], o["dR"], o["dX"], o["dU"]
                    b = ci % 2
                    A = AR[:, c, 0:64]; Rr = AR[:, c, 64:128]
                    Sc, dSc = o["S"][ci % 2], o["dS"][ci % 2]
                    Sn, dSn = o["S"][(ci + 1) % 2], o["dS"][(ci + 1) % 2]
                    Sbc, dSbc = o["Sb"][ci % 2], o["dSb"][ci % 2]
                    Sbn, dSbn = o["Sb"][(ci + 1) % 2], o["dSb"][(ci + 1) % 2]
                    bc_, dbc = o["bankC"]
                    pX, dpX = bc_[:, 0:64], dbc
                    em.op("pe", lambda e: e.matmul(pX[0:64, 0:64], A, Sbc[:], start=True, stop=False), reads=[dAR, dSbc], writes=[dpX])
                    em.op("pe", lambda e: e.matmul(pX[0:64, 0:64], MK[b][:, 0:64], Vt[:, c, :], start=False, stop=True), reads=[dMK[b], dVt], writes=[dpX])
                    em.op("act", lambda e: e.activation(Xs[b][:], pX[0:64, 0:64], AF.Copy), reads=[dpX], writes=[dXs[b]])
                    yield
                    pU, dpU = bc_[:, 64:128], dbc
                    em.op("pe", lambda e: e.matmul(pU[0:64, 0:64], Rm[b][:], Xs[b][:], start=True, stop=True), reads=[dRm[b], dXs[b]], writes=[dpU])
                    em.op("act", lambda e: e.activation(Us[b][:], pU[0:64, 0:64], AF.Copy), reads=[dpU], writes=[dUs[b]])
                    yield
                    pS, dpS = bc_[:, 128:192], dbc
                    pY, dpY = bc_[:, 192:256], dbc
                    em.op("pe", lambda e: e.matmul(pS[0:64, 0:64], o["Bh"][:, c, :], Us[b][:], start=True, stop=False), reads=[o["dBh"], dUs[b]], writes=[dpS])
                    em.op("pe", lambda e: e.matmul(pS[0:64, 0:64], o["Kh"][:, c, :], Vt[:, c, :], start=False, stop=True), reads=[o["dKh"], dVt], writes=[dpS])
                    em.op("pe", lambda e: e.matmul(pY[0:64, 0:64], Rr, Sbc[:], start=True, stop=False), reads=[dAR, dSbc], writes=[dpY])
                    em.op("pe", lambda e: e.matmul(pY[0:64, 0:64], MB[b][:, 64:128], Us[b][:], start=False, stop=False), reads=[dMB[b], dUs[b]], writes=[dpY])
                    em.op("pe", lambda e: e.matmul(pY[0:64, 0:64], MK[b][:, 64:128], Vt[:, c, :], start=False, stop=True), reads=[dMK[b], dVt], writes=[dpY])
                    yield
                    em.op("dve", lambda e: e.scalar_tensor_tensor(Sbn[:], Sc[:], o["gC"][:, c:c + 1], pS[0:64, 0:64], ALU.mult, ALU.add), reads=[dpS, dSc, o["dgC"]], writes=[dSbn])
                    em.op("dve", lambda e: e.scalar_tensor_tensor(Sn[:], Sc[:], o["gC"][:, c:c + 1], pS[0:64, 0:64], ALU.mult, ALU.add), reads=[dpS, dSc, o["dgC"]], writes=[dSn])
                    em.op("dve", lambda e: e.tensor_tensor(yacc[:, c, :], pY[0:64, 0:64], yacc[:, c, :], ALU.add), reads=[dpY, dy], writes=[dy])
                    yield

                orders = [list(range(4)) + list(range(4, NCH)), list(range(3, -1, -1)) + list(range(NCH - 1, 3, -1))]

                def rr(gens):
                    gens = list(gens)
                    while gens:
                        for g in list(gens):
                            try:
                                next(g)
                            except StopIteration:
                                gens.remove(g)
                rr([indep(0, 0, orders[0][0]), indep(1, 0, orders[1][0])])
                for ci in range(NCH):
                    gs = [chain(0, ci, orders[0][ci]), chain(1, ci, orders[1][ci])]
                    if ci + 1 < NCH:
                        gs += [indep(0, ci + 1, orders[0][ci + 1]), indep(1, ci + 1, orders[1][ci + 1])]
                    rr(gs)
                dst_ = Dep()
                em.op("dve", lambda e: e.reduce_sum(st_[:, :, 0], yacc[:], AX.X), reads=[dy], writes=[dst_])
                em.op("dve", lambda e: e.tensor_scalar(st_[:, :, 0], st_[:, :, 0], 1.0 / 64, None, ALU.mult), reads=[dst_], writes=[dst_])
                em.op("dve", lambda e: e.tensor_tensor(yacc[:], yacc[:], st_[:, :, 0:1].to_broadcast([64, NCH, 64]), ALU.subtract), reads=[dst_, dy], writes=[dy])
                em.op("act", lambda e: e.activation(T13, yacc[:], AF.Square), reads=[dy], writes=[dT1])
                em.op("dve", lambda e: e.reduce_sum(st_[:, :, 1], T13, AX.X), reads=[dT1], writes=[dst_])
                em.op("dve", lambda e: e.tensor_scalar(st_[:, :, 1], st_[:, :, 1], 1.0 / 64, 64e-5, ALU.mult, ALU.add), reads=[dst_], writes=[dst_])
                em.op("act", lambda e: e.activation(st_[:, :, 1], st_[:, :, 1], AF.Sqrt), reads=[dst_], writes=[dst_])
                em.op("dve", lambda e: e.reciprocal(st_[:, :, 1], st_[:, :, 1]), reads=[dst_], writes=[dst_])
                em.op("dve", lambda e: e.tensor_tensor(yacc[:], yacc[:], st_[:, :, 1:2].to_broadcast([64, NCH, 64]), ALU.mult), reads=[dst_, dy], writes=[dy])
                em.op("dve", lambda e: e.tensor_tensor(yacc[:], yacc[:], gnb[:, 0:1, :].to_broadcast([64, NCH, 64]), ALU.mult), reads=[dgn, dy], writes=[dy])
                em.op("pool", lambda e: e.tensor_tensor(yacc[:], yacc[:], gnb[:, 1:2, :].to_broadcast([64, NCH, 64]), ALU.add), reads=[dgn, dy], writes=[dy])
                em.op("dve", lambda e: e.tensor_tensor(T13, Vt[:], coef[:].rearrange("p (c o) -> p c o", o=1).to_broadcast([64, NCH, 64]), ALU.mult), reads=[dVt, dcoef, dT1], writes=[dT1])
                em.op("dve", lambda e: e.tensor_tensor(yacc[:], yacc[:], T13, ALU.add), reads=[dT1, dy], writes=[dy])
                for c0 in range(0, NCH, 8):
                    nb = min(8, NCH - c0)
                    ps, dp = self.psum()
                    for j in range(nb):
                        c = c0 + j
                        em.op("pe", lambda e, ps=ps, j=j, c=c: e.matmul(ps[0:64, j * 64:(j + 1) * 64], lgs[:, c * 64:(c + 1) * 64], g2b[:, hs], start=True, stop=True), reads=[dsh, dwl], writes=[dp])
                    em.op("dve", lambda e, ps=ps, c0=c0, nb=nb: e.tensor_tensor(yacc[:, c0:c0 + nb, :], yacc[:, c0:c0 + nb, :], ps[0:64, 0:nb * 64].rearrange("p (c k) -> p c k", k=64), ALU.mult), reads=[dp, dy], writes=[dy])
                em.dma("sp", self.y_rw.rearrange("(c p) f -> p c f", p=64)[:, :, hs], yacc[:], reads=[dy], writes=[self.d_yrw])
            self.ps_n = 8
            em.barrier()
        self.tap(f"yrw{li}", self.y_rw, [T, RW_W], reads=[self.d_yrw])

    def build(self, stages=("proj", "na", "hy", "rw", "merge", "ffn")):
        self.declare()
        self.setup_globals()
        self.adaln()
        for li in self.layers:
            last = li == DEPTH - 1
            if "proj" in stages:
                self.proj_in(li, list(range(NT)))
            if "na" in stages:
                self.na(li, last)
            if "hy" in stages:
                self.hyena(li, last)
            if "rw" in stages:
                self.rwkv(li, last)
            if "merge" in stages:
                self.merge(li, last)
            if "ffn" in stages:
                self.ffn(li, last)
        self.em.barrier()
        return self.nc


INJ_SHAPES = {"y_rw": [T, RW_W], "y_hy": [T, HY_W], "y_na": [T, NA_W]}


def prepare_shared(inputs):
    f = lambda a: np.ascontiguousarray(np.asarray(a, dtype=np.float32))
    sh = {}
    for k in ("ada_w", "ada_b", "norm1_g", "norm2_g", "w_in", "rw_shift", "rw_w0", "rw_w2", "rw_a0", "rw_a2", "rw_g2",
              "rw_kk", "rw_ka", "rw_gn_g", "rw_gn_b", "hy_conv_w", "hy_conv_b", "hy_f_w1", "hy_f_b1", "hy_f_w2",
              "hy_f_b2", "hy_f_w3", "hy_f_freq", "hy_skip", "na_q_gain", "na_k_gain", "w_br_rw", "w_br_hy", "w_br_na",
              "w_out", "ff_w1", "ff_w3", "ff_w2", "moe_w1", "moe_w3", "moe_w2"):
        sh[k] = f(inputs[k])
    sh["rw_rk"] = f(inputs["rw_rk"]).reshape(DEPTH, RW_W)
    sh["moe_routerT"] = np.ascontiguousarray(np.transpose(f(inputs["moe_router"]), (0, 2, 1)))
    sh["na_G"] = na_bias_gather(f(inputs["na_rpb"]))
    sh["cctx_lay"] = np.ascontiguousarray(f(inputs["c_ctx"]).reshape(8, 128).T)
    sh.update(host_constants())
    return sh


def core_inputs(inputs, sh, b):
    m = dict(sh)
    m["x"] = np.ascontiguousarray(np.asarray(inputs["x"][b], dtype=np.float32))
    m["ctx"] = np.ascontiguousarray(np.asarray(inputs["ctx"][b], dtype=np.float32))
    m["c_lay"] = np.ascontiguousarray(np.asarray(inputs["c"][b], dtype=np.float32).reshape(8, 128).T)
    return m


def kernel(**inputs):
    bld = Builder()
    nc = bld.build()
    sh = prepare_shared(inputs)
    in_maps = [core_inputs(inputs, sh, b) for b in range(8)]
    res = run_bass_kernel_spmd(nc, in_maps, core_ids=list(range(8)))
    return np.stack([np.asarray(res.results[b]["y"]) for b in range(8)], axis=0).astype(np.float32)
```

```python
import math
from contextlib import ExitStack
import numpy as np
import ml_dtypes
import concourse.bass as bass
import concourse.mybir as mybir
from concourse.bass_utils import run_bass_kernel_spmd

F32 = mybir.dt.float32
BF16 = mybir.dt.bfloat16
ALU = mybir.AluOpType
AF = mybir.ActivationFunctionType
AX = mybir.AxisListType

D = 1024
SEQ = 2048
LC = 256
T = LC + SEQ
NT = T // 128
DEPTH = 2
GRID_W = 64
RW_W = 384
RW_IN = 1536
HY_W = 256
HY_IN = 768
NA_W = 384
NA_IN = 1152
P_IN = 6528
TOKC = P_IN - RW_IN
FF_DENSE = 2816
FF_EXPERT = 3584
NE = 8
EPS = 1e-6
UROWS = T + 4


def urow(t):
    return 1 + t if t < LC else 3 + t


class Dep:
    __slots__ = ("w", "r")

    def __init__(self):
        self.w = None
        self.r = {}


class Emitter:
    EPOCH = 24000

    def __init__(self, nc):
        self.nc = nc
        self.engs = {"pe": nc.tensor, "act": nc.scalar, "dve": nc.vector, "pool": nc.gpsimd, "sp": nc.sync}
        self.sems = {}
        self.epoch = {}
        self.cnt = {}
        for e in ("pe", "act", "dve", "pool"):
            self.epoch[e] = 0
            self.cnt[e] = 0
            self.sems[(e, 0)] = nc.alloc_semaphore(f"s_{e}_0")
        self.waited = {e: {} for e in self.engs}
        self.dsems = {}
        self.dslot = {}
        for q, n in (("sp", 24), ("pool", 24), ("act", 8)):
            self.dsems[q] = []
            for i in range(n):
                key = ("d", q, i)
                self.sems[key] = nc.alloc_semaphore(f"d_{q}_{i}")
                self.dsems[q].append(key)
            self.dslot[q] = 0
        self.dlast = {}
        self.n_ins = 0
        self.pending_noinc = {}

    def _wait(self, e, evs):
        eng = self.engs[e]
        w = self.waited[e]
        for key, val in evs:
            if w.get(key, 0) >= val:
                continue
            eng.wait_ge(self.sems[key], val)
            w[key] = val

    def _collect(self, e, reads, writes):
        evs = []
        for d in reads:
            if d.w is not None:
                evs.append(d.w)
        for d in writes:
            if d.w is not None:
                evs.append(d.w)
            for key, val in d.r.items():
                evs.append((key, val))
        if e == "pe":
            evs = [ev for ev in evs if ev[0][0] != "pe"]
        return evs

    def op(self, e, fn, reads=(), writes=(), inc=True):
        self._wait(e, self._collect(e, reads, writes))
        if self.cnt[e] >= self.EPOCH and not self.pending_noinc.get(e):
            self.epoch[e] += 1
            self.cnt[e] = 0
            self.sems[(e, self.epoch[e])] = self.nc.alloc_semaphore(f"s_{e}_{self.epoch[e]}")
        key = (e, self.epoch[e])
        ins = fn(self.engs[e])
        if inc:
            self.cnt[e] += 1
            ins.then_inc(self.sems[key], 1)
            ev = (key, self.cnt[e])
            self.pending_noinc[e] = False
        else:
            ev = (key, self.cnt[e] + 1)
            self.pending_noinc[e] = True
        for d in reads:
            d.r[key] = ev[1]
        for d in writes:
            d.w = ev
            d.r = {}
        self.n_ins += 1
        return ins

    def dma(self, q, out, in_, reads=(), writes=(), **kw):
        self._wait(q, self._collect(q, reads, writes))
        slot = self.dslot[q]
        self.dslot[q] += 1
        n = len(self.dsems[q])
        key = self.dsems[q][slot % n]
        use = slot // n
        if use > 0:
            self._wait(q, [(key, 16 * use)])
        ins = self.engs[q].dma_start(out=out, in_=in_, **kw)
        ins.then_inc(self.sems[key], 16)
        ev = (key, 16 * (use + 1))
        self.dlast[key] = ev[1]
        for d in reads:
            d.r[key] = ev[1]
        for d in writes:
            d.w = ev
            d.r = {}
        self.n_ins += 1
        return ins

    def barrier(self):
        assert not any(self.pending_noinc.values()), "barrier with pending non-incrementing op"
        evs = []
        for e in ("pe", "act", "dve", "pool"):
            if self.cnt[e] > 0:
                evs.append(((e, self.epoch[e]), self.cnt[e]))
        for key, val in self.dlast.items():
            evs.append((key, val))
        for e in self.engs:
            self._wait(e, [ev for ev in evs if ev[0][0] != e or e == "pool"])


def bf16(a):
    return np.asarray(a, dtype=np.float32).astype(ml_dtypes.bfloat16)


_CONST_CACHE = {}


def host_constants():
    if _CONST_CACHE:
        return _CONST_CACHE
    c = {}
    c["ident_b"] = bf16(np.eye(128))
    c["ident_f"] = np.eye(128, dtype=np.float32)
    c["ones_f"] = np.ones((128, 128), np.float32)
    c["zeros_f"] = np.zeros((128, 1024), np.float32)
    for n, tag in ((SEQ, "l"), (LC, "c")):
        N2 = 2 * n
        t = np.arange(n, dtype=np.float64)
        th = 2 * np.pi * (np.arange(n, dtype=np.float64) + 0.5) / N2
        ang = np.outer(t, th)
        cm = np.cos(ang)
        sm = np.sin(ang)
        nch = n // 128

        def tile_tf(m):
            return np.ascontiguousarray(m.reshape(nch, 128, nch, 128).transpose(2, 1, 0, 3))

        c["cm_" + tag] = bf16(tile_tf(cm))
        c["smn_" + tag] = bf16(tile_tf(-sm))
        sc = 2.0 / N2
        c["icm_" + tag] = bf16(tile_tf((cm * sc).T))
        c["ismn_" + tag] = bf16(tile_tf((-sm * sc).T))
        pos = np.arange(n, dtype=np.float32)
        tt = pos / np.float32(max(n - 1, 1))
        bands = 16
        f = np.linspace(1e-4, bands - 1, bands, dtype=np.float32)
        a2 = (np.float32(2 * math.pi / n) * pos[:, None] * f[None, :]).astype(np.float32)
        z = np.concatenate([tt[:, None], np.cos(a2), -np.sin(a2)], axis=-1).astype(np.float32)
        c["hyz_" + tag] = np.ascontiguousarray(z.T)
        max_decay = math.log(1e-2) / 0.3
        min_decay = math.log(1e-2) / 1.5
        deltas = np.abs(np.linspace(min_decay, max_decay, HY_W, dtype=np.float32))
        dec = np.exp(-tt[:, None] * deltas[None, :]).astype(np.float32)
        c["hydec_" + tag] = np.ascontiguousarray(dec.reshape(nch, 128, HY_W).transpose(1, 0, 2))
    tpos = np.arange(SEQ)
    inv = (10000.0 ** (-np.arange(16, dtype=np.float32) / 16)).astype(np.float32)
    cosT = np.zeros((64, SEQ), np.float32)
    sinT = np.zeros((64, SEQ), np.float32)
    for half, posv in ((0, tpos // GRID_W), (1, tpos % GRID_W)):
        ang = posv.astype(np.float32)[None, :] * inv[:, None]
        for j in range(2):
            cosT[half * 32 + j * 16: half * 32 + (j + 1) * 16] = np.cos(ang)
            sinT[half * 32 + j * 16: half * 32 + (j + 1) * 16] = np.sin(ang)
    c["rope_cos"] = cosT
    c["rope_sin"] = sinT
    Pm = np.zeros((64, 64), np.float32)
    for blk in range(2):
        for i in range(16):
            Pm[blk * 32 + i, blk * 32 + 16 + i] = -1.0
            Pm[blk * 32 + 16 + i, blk * 32 + i] = 1.0
    c["rope_PT"] = np.ascontiguousarray(Pm.T)
    s_i = np.arange(64)[:, None]
    t_i = np.arange(64)[None, :]
    cm_ = np.ones((64, T), np.float32); cm_[:, ::64] = 0.0
    c["rw_cmask"] = cm_
    c["mk_f_strict"] = (t_i > s_i).astype(np.float32)
    c["mk_f_incl"] = (t_i >= s_i).astype(np.float32)
    c["mk_b_strict"] = (t_i < s_i).astype(np.float32)
    c["mk_b_incl"] = (t_i <= s_i).astype(np.float32)
    cq = np.arange(64)
    c0 = np.clip(cq - 8, 0, 48)
    inw = (cq[None, :] >= c0[:, None]) & (cq[None, :] < c0[:, None] + 16)
    c["na_mask"] = np.where(inw, 0.0, -30000.0).astype(np.float32)
    _CONST_CACHE.update(c)
    return c


def na_bias_gather(rpb):
    cq = np.arange(64)
    dc = np.clip(cq[None, :] - cq[:, None] + 15, 0, 30)
    g = rpb[:, :, :, dc]
    return np.ascontiguousarray(np.transpose(g, (0, 3, 1, 2, 4)))


def row_bc(ap_row, nparts=128):
    return bass.AP(ap_row.tensor, ap_row.offset, [[0, nparts]] + [list(x) for x in ap_row.ap[1:]])


class Builder:
    def __init__(self, layers=(0, 1), taps=(), inject=()):
        self.nc = nc = bass.Bass("TRN2", target_bir_lowering=False)
        self.em = Emitter(nc)
        self.layers = layers
        self.taps = set(taps)
        self.inject = set(inject)
        self.uid = 0
        self.I = {}
        self.outs = {}
        self.ps = [nc.alloc_psum_tensor(f"psb{i}", [128, 512], F32) for i in range(8)]
        self.psd = [Dep() for _ in range(8)]
        self.psi = 0
        self.ps_n = 8

    def inp(self, name, shape, dtype=F32):
        a = self.nc.dram_tensor(name, list(shape), dtype, kind="ExternalInput").ap()
        self.I[name] = a
        return a

    def out(self, name, shape, dtype=F32):
        a = self.nc.dram_tensor(name, list(shape), dtype, kind="ExternalOutput").ap()
        self.outs[name] = a
        return a

    def scratch(self, name, shape, dtype=F32):
        return self.nc.dram_tensor(name, list(shape), dtype).ap()

    def sb(self, st, name, shape, dtype=F32):
        self.uid += 1
        return st.enter_context(self.nc.sbuf_tensor(f"{name}_{self.uid}", list(shape), dtype))

    def psum(self):
        i = self.psi % self.ps_n
        self.psi = (i + 1) % self.ps_n
        return self.ps[i], self.psd[i]

    def load_const(self, st, name, dtype=F32, q="sp"):
        a = self.I[name]
        t = self.sb(st, name, list(a.shape), dtype)
        d = Dep()
        self.em.dma(q, t[:], a, writes=[d])
        return t, d

    def load_bc(self, st, name, row_ap, n, nparts=128):
        t = self.sb(st, name, [nparts, n])
        d = Dep()
        self.em.dma("sp", t[:], row_bc(row_ap, nparts), writes=[d])
        return t, d

    def declare(self):
        L = DEPTH
        self.inp("x", [SEQ, D]); self.inp("ctx", [LC, D])
        self.inp("c_lay", [128, 8]); self.inp("cctx_lay", [128, 8])
        self.inp("ada_w", [L, D, 6 * D]); self.inp("ada_b", [L, 6 * D])
        self.inp("norm1_g", [L, D]); self.inp("norm2_g", [L, D])
        self.inp("w_in", [L, D, P_IN])
        self.inp("rw_shift", [L, 2, RW_IN]); self.inp("rw_w0", [L, 2, RW_W]); self.inp("rw_w2", [L, 2, 64, RW_W])
        self.inp("rw_a0", [L, 2, RW_W]); self.inp("rw_a2", [L, 2, 64, RW_W]); self.inp("rw_g2", [L, 128, RW_W])
        self.inp("rw_kk", [L, RW_W]); self.inp("rw_ka", [L, RW_W]); self.inp("rw_rk", [L, RW_W])
        self.inp("rw_gn_g", [L, RW_W]); self.inp("rw_gn_b", [L, RW_W])
        self.inp("hy_conv_w", [L, 3, HY_IN]); self.inp("hy_conv_b", [L, HY_IN])
        self.inp("hy_f_w1", [L, 33, 64]); self.inp("hy_f_b1", [L, 64]); self.inp("hy_f_w2", [L, 64, 64])
        self.inp("hy_f_b2", [L, 64]); self.inp("hy_f_w3", [L, 64, 1024]); self.inp("hy_f_freq", [L, 64])
        self.inp("hy_skip", [L, 2, HY_W])
        self.inp("na_q_gain", [L, 64]); self.inp("na_k_gain", [L, 64])
        self.inp("na_G", [L, 64, 6, 15, 64])
        self.inp("w_br_rw", [L, RW_W, D]); self.inp("w_br_hy", [L, HY_W, D]); self.inp("w_br_na", [L, NA_W, D])
        self.inp("w_out", [L, D, D])
        self.inp("ff_w1", [1, D, FF_DENSE]); self.inp("ff_w3", [1, D, FF_DENSE]); self.inp("ff_w2", [1, FF_DENSE, D])
        self.inp("moe_routerT", [1, NE, D])
        self.inp("moe_w1", [1, NE, D, FF_EXPERT]); self.inp("moe_w3", [1, NE, D, FF_EXPERT])
        self.inp("moe_w2", [1, NE, FF_EXPERT, D])
        hc = host_constants()
        for k, v in hc.items():
            self.inp(k, list(v.shape), BF16 if v.dtype == ml_dtypes.bfloat16 else F32)
        for name in self.inject:
            self.inp("inj_" + name, INJ_SHAPES[name])
        self.y = self.out("y", [SEQ, D])
        self.modv = self.scratch("modv", [DEPTH, 2, 6 * D])
        self.xres = self.scratch("xres", [T, D])
        self.u_rwT = self.scratch("u_rwT", [RW_IN, T])
        self.u_tok = self.scratch("u_tok", [UROWS, TOKC])
        self.y_rw = self.scratch("y_rw", [T, RW_W])
        self.y_hy = self.scratch("y_hy", [T, HY_W])
        self.y_na = self.scratch("y_na", [T, NA_W])
        self.d_modv = Dep(); self.d_xres = Dep(); self.d_urw = Dep(); self.d_utok = Dep()
        self.d_yrw = Dep(); self.d_yhy = Dep(); self.d_yna = Dep()
        self.xres_valid = False

    def tap(self, name, src_ap, shape, reads=()):
        if name in self.taps:
            o = self.out("tap_" + name, shape)
            self.em.dma("sp", o, src_ap, reads=list(reads))

    def res_rows(self, t):
        if self.xres_valid:
            return self.xres[t * 128:(t + 1) * 128, :]
        if t < 2:
            return self.I["ctx"][t * 128:(t + 1) * 128, :]
        return self.I["x"][(t - 2) * 128:(t - 1) * 128, :]

    def setup_globals(self):
        self.gst = ExitStack()
        st = self.gst
        self.ident_b, self.d_ident_b = self.load_const(st, "ident_b", BF16)
        self.ident_f, self.d_ident_f = self.load_const(st, "ident_f", F32)
        self.eps_t = self.sb(st, "eps_t", [128, 1]); self.d_eps = Dep()
        self.em.op("dve", lambda e: e.memset(self.eps_t[:], EPS), writes=[self.d_eps])
        with ExitStack() as zst:
            z, dz = self.load_const(zst, "zeros_f", F32)
            for r in (0, LC + 1, LC + 2, UROWS - 1):
                for c0 in range(0, TOKC, 1024):
                    n = min(1024, TOKC - c0)
                    self.em.dma("sp", self.u_tok[r:r + 1, c0:c0 + n], z[0:1, 0:n], reads=[dz], writes=[self.d_utok])
            self.em.barrier()

    def adaln(self):
        em = self.em
        with ExitStack() as st:
            cT = self.sb(st, "cT", [128, 2, 8]); dc = Dep()
            em.dma("sp", cT[:, 0, :], self.I["c_lay"], writes=[dc])
            em.dma("sp", cT[:, 1, :], self.I["cctx_lay"], writes=[dc])
            aT = self.sb(st, "aT", [128, 2, 8]); da = Dep()
            em.op("act", lambda e: e.activation(aT[:], cT[:], AF.Silu), reads=[dc], writes=[da])
            wb = [self.sb(st, f"adaw{i}", [128, 8, 512]) for i in range(2)]
            dwb = [Dep(), Dep()]
            msb = self.sb(st, "modsb", [2, 6 * D]); dm = Dep()
            bsb = self.sb(st, "adab", [2, 6 * D]); db = Dep()
            it = 0
            for li in self.layers:
                em.dma("sp", bsb[:], row_bc(self.I["ada_b"][li:li + 1, :], 2), writes=[db])
                wv = self.I["ada_w"][li].rearrange("(k p) n -> p k n", p=128)
                for nb in range(12):
                    w = wb[it % 2]; dw = dwb[it % 2]; it += 1
                    em.dma("sp" if nb % 2 == 0 else "act", w[:], wv[:, :, nb * 512:(nb + 1) * 512], writes=[dw])
                    ps, dp = self.psum()
                    for k in range(8):
                        em.op("pe", lambda e, k=k, w=w, ps=ps: e.matmul(ps[0:2, :], aT[:, :, k], w[:, k, :], start=(k == 0), stop=(k == 7)),
                              reads=[da, dw], writes=[dp], inc=(k == 7))
                    em.op("dve", lambda e, ps=ps, nb=nb: e.tensor_tensor(msb[:, nb * 512:(nb + 1) * 512], ps[0:2, :], bsb[:, nb * 512:(nb + 1) * 512], ALU.add),
                          reads=[dp, db], writes=[dm])
                em.dma("sp", self.modv[li], msb[:], reads=[dm], writes=[self.d_modv])
                self.tap(f"modv{li}", self.modv[li], [2, 6 * D], reads=[self.d_modv])
            em.barrier()

    def mod_row(self, li, which, chunk):
        return self.modv[li, which:which + 1, chunk * D:(chunk + 1) * D]

    def norm_hT(self, li, nidx, hT, dhT, tiles, cb=None):
        em = self.em
        with ExitStack() as st:
            gname = "norm1_g" if nidx == 1 else "norm2_g"
            sh_c, sc_c = (0, 1) if nidx == 1 else (3, 4)
            G, dG = self.load_bc(st, "G", self.I[gname][li:li + 1, :], D)
            AB = {}
            for which in (0, 1):
                if which == 1 and all(t >= 2 for t in tiles):
                    continue
                S_, dS = self.load_bc(st, "S", self.mod_row(li, which, sc_c), D)
                B_, dB = self.load_bc(st, "Bm", self.mod_row(li, which, sh_c), D)
                em.op("dve", lambda e, S_=S_: e.scalar_tensor_tensor(S_[:], S_[:], 1.0, G[:], ALU.add, ALU.mult),
                      reads=[dS, dG, self.d_modv], writes=[dS])
                AB[which] = (S_, dS, B_, dB)
            NBN = 3
            xt = [self.sb(st, f"xt{i}", [128, D]) for i in range(NBN)]; dxt = [Dep() for _ in range(NBN)]
            junk = self.sb(st, "junk", [128, D]); dj = Dep()
            hm = [self.sb(st, f"hm{i}", [128, D]) for i in range(NBN)]; dhm = [Dep() for _ in range(NBN)]
            hb = [self.sb(st, f"hb{i}", [128, D], BF16) for i in range(NBN)]; dhb = [Dep() for _ in range(NBN)]
            ss = self.sb(st, "ss", [128, 2 * len(tiles)]); dss_ = [Dep() for _ in range(len(tiles))]
            def ntile(i, t):
                b = i % NBN
                which = 1 if t < 2 else 0
                A_, dA, B_, dB = AB[which]
                em.dma("sp", xt[b][:], self.res_rows(t), reads=[self.d_xres], writes=[dxt[b]])
                s0 = ss[:, 2 * i:2 * i + 1]; s1 = ss[:, 2 * i + 1:2 * i + 2]; dss = dss_[i]
                em.op("act", lambda e, b=b, s0=s0: e.activation(junk[:], xt[b][:], AF.Square, accum_out=s0),
                      reads=[dxt[b]], writes=[dj, dss])
                em.op("act", lambda e, s0=s0, s1=s1: e.activation(s1, s0, AF.Sqrt, bias=self.eps_t[:], scale=1.0 / D), reads=[dss, self.d_eps], writes=[dss])
                yield
                em.op("dve", lambda e, s1=s1: e.reciprocal(s1, s1), reads=[dss], writes=[dss])
                em.op("dve", lambda e, b=b, s1=s1, A_=A_: e.scalar_tensor_tensor(hm[b][:], xt[b][:], s1, A_[:], ALU.mult, ALU.mult),
                      reads=[dxt[b], dss, dA], writes=[dhm[b]])
                yield
                em.op("pool", lambda e, b=b, B_=B_: e.tensor_tensor(hm[b][:], hm[b][:], B_[:], ALU.add), reads=[dhm[b], dB], writes=[dhm[b]])
                yield
                em.op("act", lambda e, b=b: e.activation(hb[b][:], hm[b][:], AF.Copy), reads=[dhm[b]], writes=[dhb[b]])
                if cb is not None:
                    cb(st, t, hm[b], dhm[b])
                yield
                ps, dp = self.psum()
                pb = ps[:].bitcast(BF16)
                for k in range(8):
                    em.op("pe", lambda e, k=k, b=b, pb=pb: e.transpose(pb[:, k * 128:(k + 1) * 128], hb[b][:, k * 128:(k + 1) * 128], self.ident_b[:]),
                          reads=[dhb[b], self.d_ident_b], writes=[dp])
                yield
                em.op("dve", lambda e, t=t, pb=pb: e.tensor_copy(hT[:, :, t * 128:(t + 1) * 128], pb.rearrange("p (k n) -> p k n", k=8)),
                      reads=[dp], writes=[dhT])

            gens = [ntile(i, t) for i, t in enumerate(tiles)]
            active = []
            gi = 0
            while gi < len(gens) or active:
                while len(active) < NBN and gi < len(gens):
                    active.append(gens[gi]); gi += 1
                for g in list(active):
                    try:
                        next(g)
                    except StopIteration:
                        active.remove(g)
            em.barrier()

    def proj_in(self, li, tiles):
        em = self.em
        with ExitStack() as st:
            hT = self.sb(st, "hT", [128, 8, T], BF16); dhT = Dep()
            self.norm_hT(li, 1, hT, dhT, tiles)
            wv = self.I["w_in"][li].rearrange("(k p) n -> p k n", p=128)
            wb = [self.sb(st, f"winb{i}", [128, 8, 512], BF16) for i in range(2)]; dwb = [Dep(), Dep()]
            og = [self.sb(st, f"og{i}", [128, 512]) for i in range(4)]; dog = [Dep() for _ in range(4)]
            oi = 0
            t0 = tiles[0] * 128
            tokblocks = [(s, min(512, T - s)) for s in range(t0, T, 512)]
            nblk = (P_IN + 511) // 512
            for cb in range(nblk):
                c0 = cb * 512
                cn = min(512, P_IN - c0)
                w = wb[cb % 2]; dw = dwb[cb % 2]
                em.dma("pool", w[:, :, 0:cn], wv[:, :, c0:c0 + cn], writes=[dw])
                if c0 < RW_IN:
                    for mc in range(cn // 128):
                        for (s, n) in tokblocks:
                            ps, dp = self.psum()
                            for k in range(8):
                                em.op("pe", lambda e, k=k, w=w, ps=ps, mc=mc, s=s, n=n: e.matmul(ps[:, 0:n], w[:, k, mc * 128:(mc + 1) * 128], hT[:, k, s:s + n], start=(k == 0), stop=(k == 7)),
                                      reads=[dw, dhT], writes=[dp], inc=(k == 7))
                            o = og[oi % 4]; do = dog[oi % 4]; oi += 1
                            eng = "act" if oi % 2 else "dve"
                            if eng == "act":
                                em.op("act", lambda e, o=o, ps=ps, n=n: e.activation(o[:, 0:n], ps[:, 0:n], AF.Copy), reads=[dp], writes=[do])
                            else:
                                em.op("dve", lambda e, o=o, ps=ps, n=n: e.tensor_copy(o[:, 0:n], ps[:, 0:n]), reads=[dp], writes=[do])
                            r0 = c0 + mc * 128
                            em.dma("sp", self.u_rwT[r0:r0 + 128, s:s + n], o[:, 0:n], reads=[do], writes=[self.d_urw])
                else:
                    for t in tiles:
                        ps, dp = self.psum()
                        for k in range(8):
                            em.op("pe", lambda e, k=k, w=w, ps=ps, t=t, cn=cn: e.matmul(ps[:, 0:cn], hT[:, k, t * 128:(t + 1) * 128], w[:, k, 0:cn], start=(k == 0), stop=(k == 7)),
                                  reads=[dw, dhT], writes=[dp], inc=(k == 7))
                        o = og[oi % 4]; do = dog[oi % 4]; oi += 1
                        eng = "act" if oi % 2 else "dve"
                        if eng == "act":
                            em.op("act", lambda e, o=o, ps=ps, cn=cn: e.activation(o[:, 0:cn], ps[:, 0:cn], AF.Copy), reads=[dp], writes=[do])
                        else:
                            em.op("dve", lambda e, o=o, ps=ps, cn=cn: e.tensor_copy(o[:, 0:cn], ps[:, 0:cn]), reads=[dp], writes=[do])
                        r0 = urow(t * 128)
                        em.dma("sp", self.u_tok[r0:r0 + 128, c0 - RW_IN:c0 - RW_IN + cn], o[:, 0:cn], reads=[do], writes=[self.d_utok])
            em.barrier()
        self.tap(f"urw{li}", self.u_rwT, [RW_IN, T], reads=[self.d_urw])
        self.tap(f"utok{li}", self.u_tok, [UROWS, TOKC], reads=[self.d_utok])


    def merge(self, li, last):
        em = self.em
        tiles = list(range(2, NT)) if last else list(range(NT))
        ysrc = {"rw": (self.y_rw, self.d_yrw), "hy": (self.y_hy, self.d_yhy), "na": (self.y_na, self.d_yna)}
        for nm in ("rw", "hy", "na"):
            if ("y_" + nm) in self.inject:
                ysrc[nm] = (self.I["inj_y_" + nm], Dep())
        with ExitStack() as st:
            wbr = self.sb(st, "wbr", [128, 8, D], BF16); dwbr = Dep()
            em.dma("pool", wbr[:, 0:3, :], self.I["w_br_rw"][li].rearrange("(k p) n -> p k n", p=128), writes=[dwbr])
            em.dma("pool", wbr[:, 3:5, :], self.I["w_br_hy"][li].rearrange("(k p) n -> p k n", p=128), writes=[dwbr])
            em.dma("pool", wbr[:, 5:8, :], self.I["w_br_na"][li].rearrange("(k p) n -> p k n", p=128), writes=[dwbr])
            wo = self.sb(st, "wo", [128, 8, D], BF16); dwo = Dep()
            em.dma("pool", wo[:], self.I["w_out"][li].rearrange("(k p) n -> p k n", p=128), writes=[dwo])
            G1 = {}
            for which in (0, 1):
                if which == 1 and last:
                    continue
                G1[which] = self.load_bc(st, "G1", self.mod_row(li, which, 2), D)
            yc = [self.sb(st, f"yc{i}", [128, D]) for i in range(3)]; dyc = [Dep() for _ in range(3)]
            ycb_ = [self.sb(st, f"ycb{i}", [128, D], BF16) for i in range(3)]; dycb_ = [Dep() for _ in range(3)]
            ycT_ = [self.sb(st, f"ycT{i}", [128, 8, 128], BF16) for i in range(3)]; dycT_ = [Dep() for _ in range(3)]
            gt = [self.sb(st, f"gt{i}", [128, 3 * D]) for i in range(3)]; dgt = [Dep() for _ in range(3)]
            m_ = [self.sb(st, f"m{i}", [128, D]) for i in range(3)]; dm_ = [Dep() for _ in range(3)]
            tmp_ = [self.sb(st, f"mtmp{i}", [128, 512]) for i in range(3)]; dtmp_ = [Dep() for _ in range(3)]
            mb_ = [self.sb(st, f"mb{i}", [128, D], BF16) for i in range(3)]; dmb_ = [Dep() for _ in range(3)]
            mT_ = [self.sb(st, f"mT{i}", [128, 8, 128], BF16) for i in range(3)]; dmT_ = [Dep() for _ in range(3)]
            xt = [self.sb(st, f"mxt{i}", [128, D]) for i in range(3)]; dxt = [Dep() for _ in range(3)]
            xo = [self.sb(st, f"mxo{i}", [128, D]) for i in range(3)]; dxo = [Dep() for _ in range(3)]
            brk = {0: (0, 3), 1: (3, 5), 2: (5, 8)}
            def tile_gen(i, t):
                b = i % 3
                ycb, dycb, ycT, dycT = ycb_[b], dycb_[b], ycT_[b], dycT_[b]
                m, dm, tmp, dtmp, mb, dmb, mT, dmT = m_[b], dm_[b], tmp_[b], dtmp_[b], mb_[b], dmb_[b], mT_[b], dmT_[b]
                r0 = t * 128
                em.dma("sp", yc[b][:, 0:384], ysrc["rw"][0][r0:r0 + 128, :], reads=[ysrc["rw"][1]], writes=[dyc[b]])
                em.dma("sp", yc[b][:, 384:640], ysrc["hy"][0][r0:r0 + 128, :], reads=[ysrc["hy"][1]], writes=[dyc[b]])
                em.dma("sp", yc[b][:, 640:1024], ysrc["na"][0][r0:r0 + 128, :], reads=[ysrc["na"][1]], writes=[dyc[b]])
                ur = urow(r0)
                em.dma("sp", gt[b][:], self.u_tok[ur:ur + 128, HY_IN + NA_IN:TOKC], reads=[self.d_utok], writes=[dgt[b]])
                em.dma("sp", xt[b][:], self.res_rows(t), reads=[self.d_xres], writes=[dxt[b]])
                em.op("act", lambda e, b=b: e.activation(gt[b][:], gt[b][:], AF.Sigmoid), reads=[dgt[b]], writes=[dgt[b]])
                em.op("pool", lambda e, b=b: e.tensor_copy(ycb[:], yc[b][:]), reads=[dyc[b]], writes=[dycb])
                ps, dp = self.psum(); pb = ps[:].bitcast(BF16)
                for k in range(8):
                    em.op("pe", lambda e, k=k, pb=pb: e.transpose(pb[:, k * 128:(k + 1) * 128], ycb[:, k * 128:(k + 1) * 128], self.ident_b[:]),
                          reads=[dycb, self.d_ident_b], writes=[dp])
                yield
                em.op("act", lambda e, pb=pb: e.activation(ycT[:], pb.rearrange("p (k n) -> p k n", k=8), AF.Copy), reads=[dp], writes=[dycT])
                yield
                for br in range(3):
                    k0, k1 = brk[br]
                    for nb in range(2):
                        ps, dp = self.psum()
                        for k in range(k0, k1):
                            em.op("pe", lambda e, k=k, ps=ps, nb=nb, k0=k0, k1=k1: e.matmul(ps[:], ycT[:, k, :], wbr[:, k, nb * 512:(nb + 1) * 512], start=(k == k0), stop=(k == k1 - 1)),
                                  reads=[dycT, dwbr], writes=[dp], inc=(k == k1 - 1))
                        gsl = gt[b][:, br * D + nb * 512: br * D + (nb + 1) * 512]
                        msl = m[:, nb * 512:(nb + 1) * 512]
                        if br == 0:
                            em.op("dve", lambda e, ps=ps, gsl=gsl, msl=msl: e.tensor_tensor(msl, ps[:], gsl, ALU.mult), reads=[dp, dgt[b]], writes=[dm])
                        else:
                            em.op("dve", lambda e, ps=ps, gsl=gsl: e.tensor_tensor(tmp[:], ps[:], gsl, ALU.mult), reads=[dp, dgt[b]], writes=[dtmp])
                            em.op("dve", lambda e, msl=msl: e.tensor_tensor(msl, msl, tmp[:], ALU.add), reads=[dtmp, dm], writes=[dm])
                        yield
                em.op("act", lambda e: e.activation(mb[:], m[:], AF.Copy), reads=[dm], writes=[dmb])
                ps, dp = self.psum(); pb = ps[:].bitcast(BF16)
                for k in range(8):
                    em.op("pe", lambda e, k=k, pb=pb: e.transpose(pb[:, k * 128:(k + 1) * 128], mb[:, k * 128:(k + 1) * 128], self.ident_b[:]),
                          reads=[dmb, self.d_ident_b], writes=[dp])
                yield
                em.op("act", lambda e, pb=pb: e.activation(mT[:], pb.rearrange("p (k n) -> p k n", k=8), AF.Copy), reads=[dp], writes=[dmT])
                yield
                Gt, dG = G1[1 if t < 2 else 0]
                for nb in range(2):
                    ps, dp = self.psum()
                    for k in range(8):
                        em.op("pe", lambda e, k=k, ps=ps, nb=nb: e.matmul(ps[:], mT[:, k, :], wo[:, k, nb * 512:(nb + 1) * 512], start=(k == 0), stop=(k == 7)),
                              reads=[dmT, dwo], writes=[dp], inc=(k == 7))
                    sl = slice(nb * 512, (nb + 1) * 512)
                    em.op("dve", lambda e, ps=ps, sl=sl, Gt=Gt, b=b: e.tensor_tensor(xo[b][:, sl], ps[:], Gt[:, sl], ALU.mult), reads=[dp, dG, self.d_modv], writes=[dxo[b]])
                    em.op("pool", lambda e, sl=sl, b=b: e.tensor_tensor(xo[b][:, sl], xo[b][:, sl], xt[b][:, sl], ALU.add), reads=[dxo[b], dxt[b]], writes=[dxo[b]])
                    yield
                em.dma("sp", self.xres[r0:r0 + 128, :], xo[b][:], reads=[dxo[b]], writes=[Dep()])

            gens = [tile_gen(i, t) for i, t in enumerate(tiles)]
            active = []
            gi = 0
            while gi < len(gens) or active:
                while len(active) < 3 and gi < len(gens):
                    active.append(gens[gi]); gi += 1
                for g in list(active):
                    try:
                        next(g)
                    except StopIteration:
                        active.remove(g)
            em.barrier()
        if last and not self.xres_valid:
            pass
        if not self.xres_valid:
            self.xres_valid = True
        self.tap(f"xmix{li}", self.xres, [T, D], reads=[self.d_xres])

    def ffn(self, li, last):
        em = self.em
        moe = (li % 2 == 1)
        tiles = list(range(2, NT)) if last else list(range(NT))
        t0 = tiles[0] * 128
        ntok = len(tiles) * 128
        if moe:
            FF, FB, E = FF_EXPERT, 512, NE
            W1, W3, W2 = self.I["moe_w1"][li // 2], self.I["moe_w3"][li // 2], self.I["moe_w2"][li // 2]
        else:
            FF, FB, E = FF_DENSE, 256, 1
            W1, W3, W2 = self.I["ff_w1"], self.I["ff_w3"], self.I["ff_w2"]
        nfc = FB // 128
        with ExitStack() as st:
            hT = self.sb(st, "fhT", [128, 8, T], BF16); dhT = Dep()
            gates = self.sb(st, "gates", [128, NT, NE]); dgates = Dep()
            if moe:
                with ExitStack() as st2:
                    Rb = self.sb(st2, "Rb", [128, NE, D]); dRb = Dep()
                    for e_ in range(NE):
                        em.dma("sp", Rb[:, e_, :], row_bc(self.I["moe_routerT"][li // 2, e_:e_ + 1, :]), writes=[dRb])
                    lg = self.sb(st2, "lg", [128, NT, NE]); dlg = Dep()
                    jk = self.sb(st2, "rjunk", [128, D]); djk = Dep()

                    def cb(st_, t, hm, dhm):
                        for e_ in range(NE):
                            em.op("dve", lambda e, e_=e_, hm=hm: e.tensor_tensor(jk[:], hm[:], Rb[:, e_, :], ALU.mult), reads=[dhm, dRb], writes=[djk])
                            em.op("dve", lambda e, e_=e_, t=t: e.reduce_sum(lg[:, t, e_:e_ + 1], jk[:], AX.X), reads=[djk], writes=[dlg])
                    self.norm_hT(li, 2, hT, dhT, tiles, cb=cb)
                    nt = len(tiles); ta = tiles[0]
                    L = lg[:, ta:ta + nt, :]
                    m1 = self.sb(st2, "m1", [128, NT, 1]); m2 = self.sb(st2, "m2", [128, NT, 1])
                    eq1 = self.sb(st2, "eq1", [128, NT, NE]); eq2 = self.sb(st2, "eq2", [128, NT, NE]); l2 = self.sb(st2, "l2", [128, NT, NE])
                    w1_ = self.sb(st2, "w1_", [128, NT, 1]); w2_ = self.sb(st2, "w2_", [128, NT, 1])
                    dd = Dep()
                    sl = lambda a: a[:, ta:ta + nt, :]
                    bc = lambda a: a[:, ta:ta + nt, :].to_broadcast([128, nt, NE])
                    em.op("dve", lambda e: e.reduce_max(m1[:, ta:ta + nt, 0], L, AX.X), reads=[dlg], writes=[dd])
                    em.op("dve", lambda e: e.tensor_tensor(sl(eq1), L, bc(m1), ALU.is_equal), reads=[dd, dlg], writes=[dd])
                    em.op("dve", lambda e: e.scalar_tensor_tensor(sl(l2), sl(eq1), -1e30, L, ALU.mult, ALU.add), reads=[dd, dlg], writes=[dd])
                    em.op("dve", lambda e: e.reduce_max(m2[:, ta:ta + nt, 0], sl(l2), AX.X), reads=[dd], writes=[dd])
                    em.op("dve", lambda e: e.tensor_tensor(sl(eq2), sl(l2), bc(m2), ALU.is_equal), reads=[dd], writes=[dd])
                    em.op("dve", lambda e: e.tensor_tensor(sl(w2_), sl(m2), sl(m1), ALU.subtract), reads=[dd], writes=[dd])
                    em.op("act", lambda e: e.activation(sl(w2_), sl(w2_), AF.Exp), reads=[dd], writes=[dd])
                    em.op("dve", lambda e: e.tensor_scalar(sl(w1_), sl(w2_), 1.0, None, ALU.add), reads=[dd], writes=[dd])
                    em.op("dve", lambda e: e.reciprocal(sl(w1_), sl(w1_)), reads=[dd], writes=[dd])
                    em.op("dve", lambda e: e.tensor_tensor(sl(w2_), sl(w2_), sl(w1_), ALU.mult), reads=[dd], writes=[dd])
                    em.op("dve", lambda e: e.tensor_tensor(sl(eq1), sl(eq1), bc(w1_), ALU.mult), reads=[dd], writes=[dd])
                    em.op("dve", lambda e: e.tensor_tensor(sl(eq2), sl(eq2), bc(w2_), ALU.mult), reads=[dd], writes=[dd])
                    em.op("dve", lambda e: e.tensor_tensor(sl(gates), sl(eq1), sl(eq2), ALU.add), reads=[dd], writes=[dgates])
                    em.barrier()
            else:
                self.norm_hT(li, 2, hT, dhT, tiles)
            acc = self.sb(st, "acc", [128, len(tiles), D]); dacc = Dep()
            w1b = [self.sb(st, f"w1b{i}", [128, 8, FB], BF16) for i in range(2)]
            w3b = [self.sb(st, f"w3b{i}", [128, 8, FB], BF16) for i in range(2)]
            w2b = [self.sb(st, f"w2b{i}", [128, nfc, D], BF16) for i in range(2)]
            dwb = [Dep(), Dep()]
            gT = self.sb(st, "gT", [128, nfc, ntok], BF16); dgT = Dep()
            sil = [self.sb(st, f"sil{i}", [128, 512]) for i in range(2)]; dsil = [Dep(), Dep()]
            tokblocks = [(s, min(512, ntok - s)) for s in range(0, ntok, 512)]
            blk = 0
            si = 0
            for e_ in range(E):
                w1v = (W1[e_] if moe else W1[0]).rearrange("(k p) f -> p k f", p=128)
                w3v = (W3[e_] if moe else W3[0]).rearrange("(k p) f -> p k f", p=128)
                w2v = (W2[e_] if moe else W2[0]).rearrange("(c p) n -> p c n", p=128)
                for fb in range(FF // FB):
                    b = blk % 2
                    em.dma("pool", w1b[b][:], w1v[:, :, fb * FB:(fb + 1) * FB], writes=[dwb[b]])
                    em.dma("pool", w3b[b][:], w3v[:, :, fb * FB:(fb + 1) * FB], writes=[dwb[b]])
                    em.dma("pool", w2b[b][:], w2v[:, fb * nfc:(fb + 1) * nfc, :], writes=[dwb[b]])
                    for (s, n) in tokblocks:
                        for fc in range(nfc):
                            p1, dp1 = self.psum(); p3, dp3 = self.psum()
                            for k in range(8):
                                em.op("pe", lambda e, k=k, p1=p1, fc=fc, s=s, n=n, b=b: e.matmul(p1[:, 0:n], w1b[b][:, k, fc * 128:(fc + 1) * 128], hT[:, k, t0 + s:t0 + s + n], start=(k == 0), stop=(k == 7)),
                                      reads=[dwb[b], dhT], writes=[dp1], inc=(k == 7))
                            for k in range(8):
                                em.op("pe", lambda e, k=k, p3=p3, fc=fc, s=s, n=n, b=b: e.matmul(p3[:, 0:n], w3b[b][:, k, fc * 128:(fc + 1) * 128], hT[:, k, t0 + s:t0 + s + n], start=(k == 0), stop=(k == 7)),
                                      reads=[dwb[b], dhT], writes=[dp3], inc=(k == 7))
                            sb_ = sil[si % 2]; dsb = dsil[si % 2]; si += 1
                            em.op("act", lambda e, sb_=sb_, p1=p1, n=n: e.activation(sb_[:, 0:n], p1[:, 0:n], AF.Silu), reads=[dp1], writes=[dsb])
                            em.op("dve", lambda e, sb_=sb_, p3=p3, n=n, fc=fc, s=s: e.tensor_tensor(gT[:, fc, s:s + n], p3[:, 0:n], sb_[:, 0:n], ALU.mult),
                                  reads=[dp3, dsb], writes=[dgT])
                    for ti, t in enumerate(tiles):
                        for nb in range(2):
                            ps, dp = self.psum()
                            for fc in range(nfc):
                                em.op("pe", lambda e, fc=fc, ps=ps, ti=ti, nb=nb, b=b: e.matmul(ps[:], gT[:, fc, ti * 128:(ti + 1) * 128], w2b[b][:, fc, nb * 512:(nb + 1) * 512], start=(fc == 0), stop=(fc == nfc - 1)),
                                      reads=[dgT, dwb[b]], writes=[dp], inc=(fc == nfc - 1))
                            asl = acc[:, ti, nb * 512:(nb + 1) * 512]
                            if moe:
                                gcol = gates[:, t, e_:e_ + 1]
                                if blk == 0:
                                    em.op("dve", lambda e, ps=ps, asl=asl, gcol=gcol: e.tensor_scalar(asl, ps[:], gcol, None, ALU.mult), reads=[dp, dgates], writes=[dacc])
                                else:
                                    em.op("dve", lambda e, ps=ps, asl=asl, gcol=gcol: e.scalar_tensor_tensor(asl, ps[:], gcol, asl, ALU.mult, ALU.add), reads=[dp, dgates, dacc], writes=[dacc])
                            else:
                                if blk == 0:
                                    em.op("dve", lambda e, ps=ps, asl=asl: e.tensor_copy(asl, ps[:]), reads=[dp], writes=[dacc])
                                else:
                                    em.op("dve", lambda e, ps=ps, asl=asl: e.tensor_tensor(asl, ps[:], asl, ALU.add), reads=[dp, dacc], writes=[dacc])
                    blk += 1
            G2 = {}
            for which in (0, 1):
                if which == 1 and last:
                    continue
                G2[which] = self.load_bc(st, "G2", self.mod_row(li, which, 5), D)
            xt = [self.sb(st, f"fxt{i}", [128, D]) for i in range(2)]; dxt = [Dep(), Dep()]
            for ti, t in enumerate(tiles):
                b = ti % 2
                Gt, dG = G2[1 if t < 2 else 0]
                em.dma("sp", xt[b][:], self.res_rows(t), reads=[self.d_xres], writes=[dxt[b]])
                em.op("dve", lambda e, ti=ti, Gt=Gt: e.tensor_tensor(acc[:, ti, :], acc[:, ti, :], Gt[:], ALU.mult), reads=[dacc, dG, self.d_modv], writes=[dacc])
                em.op("pool", lambda e, ti=ti, b=b: e.tensor_tensor(acc[:, ti, :], acc[:, ti, :], xt[b][:], ALU.add), reads=[dacc, dxt[b]], writes=[dacc])
                if last:
                    em.dma("sp", self.y[(t - 2) * 128:(t - 1) * 128, :], acc[:, ti, :], reads=[dacc], writes=[Dep()])
                else:
                    em.dma("sp", self.xres[t * 128:(t + 1) * 128, :], acc[:, ti, :], reads=[dacc], writes=[Dep()])
            em.barrier()
        self.tap(f"xffn{li}", self.xres, [T, D], reads=[self.d_xres])


    def na(self, li, last):
        em = self.em
        C_Q = HY_IN
        with ExitStack() as st:
            qT = self.sb(st, "qT", [64, 6, T], BF16); dqT = Dep()
            kT = self.sb(st, "kT", [64, 6, T], BF16); dkT = Dep()
            V = self.sb(st, "V", [128, NT, NA_W], BF16); dV = Dep()
            Vs = self.sb(st, "Vs", [128, 15, NA_W], BF16); dVs = Dep()
            Gm = self.sb(st, "Gm", [64, 6, 15, 64]); dGm = Dep()
            em.dma("sp", Gm[:], self.I["na_G"][li], writes=[dGm])
            mk, dmk = self.load_const(st, "na_mask")
            for h in range(6):
                em.op("pool", lambda e, h=h: e.tensor_tensor(Gm[:, h, :, :], Gm[:, h, :, :], mk[:].rearrange("p (a c) -> p a c", a=1).to_broadcast([64, 15, 64]), ALU.add),
                      reads=[dGm, dmk], writes=[dGm])
            gain = self.sb(st, "nagain", [128, 12, 64]); dgain = Dep()
            for j in range(12):
                src = self.I["na_q_gain"] if j < 6 else self.I["na_k_gain"]
                em.dma("sp", gain[:, j, :], row_bc(src[li:li + 1, :]), writes=[dgain])
            em.op("dve", lambda e: e.tensor_scalar(gain[:, 0:6, :], gain[:, 0:6, :], 0.125, None, ALU.mult), reads=[dgain], writes=[dgain])
            with ExitStack() as st2:
                ut = [self.sb(st2, f"nau{i}", [128, NA_IN]) for i in range(2)]; dut = [Dep(), Dep()]
                sq = self.sb(st2, "nasq", [128, 768]); dsq = Dep()
                ssm = self.sb(st2, "nass", [128, 12, 1]); dssm = Dep()
                qkn = self.sb(st2, "qkn", [128, 768]); dqkn = Dep()
                qkb = self.sb(st2, "qkb", [128, 768], BF16); dqkb = Dep()
                vst = [self.sb(st2, f"vst{i}", [128, NA_W]) for i in range(2)]; dvst = [Dep(), Dep()]
                for t in range(NT):
                    b = t % 2
                    ur = urow(t * 128)
                    em.dma("sp", ut[b][:], self.u_tok[ur:ur + 128, C_Q:C_Q + NA_IN], reads=[self.d_utok], writes=[dut[b]])
                    em.op("act", lambda e, b=b: e.activation(sq[:], ut[b][:, 0:768], AF.Square), reads=[dut[b]], writes=[dsq])
                    em.op("dve", lambda e: e.reduce_sum(ssm[:, :, 0], sq[:].rearrange("p (j d) -> p j d", j=12), AX.X), reads=[dsq], writes=[dssm])
                    em.op("act", lambda e: e.activation(ssm[:, :, 0], ssm[:, :, 0], AF.Sqrt, bias=self.eps_t[:], scale=1.0 / 64), reads=[dssm, self.d_eps], writes=[dssm])
                    em.op("dve", lambda e: e.reciprocal(ssm[:, :, 0], ssm[:, :, 0]), reads=[dssm], writes=[dssm])
                    em.op("dve", lambda e, b=b: e.tensor_tensor(qkn[:].rearrange("p (j d) -> p j d", j=12), ut[b][:, 0:768].rearrange("p (j d) -> p j d", j=12), ssm[:].to_broadcast([128, 12, 64]), ALU.mult),
                          reads=[dut[b], dssm], writes=[dqkn])
                    em.op("pool", lambda e: e.tensor_tensor(qkb[:].rearrange("p (j d) -> p j d", j=12), qkn[:].rearrange("p (j d) -> p j d", j=12), gain[:], ALU.mult), reads=[dqkn, dgain], writes=[dqkb])
                    em.op("act", lambda e, b=b, t=t: e.activation(V[:, t, :], ut[b][:, 768:1152], AF.Copy), reads=[dut[b]], writes=[dV])
                    for qk, (dst, ddst) in enumerate(((qT, dqT), (kT, dkT))):
                        ps, dp = self.psum(); pb = ps[:].bitcast(BF16)
                        for h in range(6):
                            em.op("pe", lambda e, h=h, pb=pb, qk=qk: e.transpose(pb[0:64, h * 128:(h + 1) * 128], qkb[:, (qk * 6 + h) * 64:(qk * 6 + h + 1) * 64], self.ident_b[:]),
                                  reads=[dqkb, self.d_ident_b], writes=[dp])
                        em.op("dve" if qk == 0 else "act",
                              (lambda e, pb=pb, dst=dst, t=t: e.tensor_copy(dst[:, :, t * 128:(t + 1) * 128], pb[0:64, 0:768].rearrange("p (h n) -> p h n", h=6))) if qk == 0 else
                              (lambda e, pb=pb, dst=dst, t=t: e.activation(dst[:, :, t * 128:(t + 1) * 128], pb[0:64, 0:768].rearrange("p (h n) -> p h n", h=6), AF.Copy)),
                              reads=[dp], writes=[ddst])
                for j in range(15):
                    b = j % 2
                    ur = urow(LC + 64 + j * 128)
                    em.dma("sp", vst[b][:], self.u_tok[ur:ur + 128, C_Q + 768:C_Q + NA_IN], reads=[self.d_utok], writes=[dvst[b]])
                    em.op("act", lambda e, b=b, j=j: e.activation(Vs[:, j, :], vst[b][:], AF.Copy), reads=[dvst[b]], writes=[dVs])
                em.barrier()
            NB = 4
            sal = [self.sb(st, f"sal{i}", [128, 768]) for i in range(NB)]; dsal = [Dep() for _ in range(NB)]
            pbf = [self.sb(st, f"pbf{i}", [128, 768], BF16) for i in range(NB)]; dpbf = [Dep() for _ in range(NB)]
            PT = [self.sb(st, f"PT{i}", [128, 768], BF16) for i in range(NB)]; dPT = [Dep() for _ in range(NB)]
            stt = [self.sb(st, f"nast{i}", [128, 4]) for i in range(NB)]; dstt = [Dep() for _ in range(NB)]
            orow = [self.sb(st, f"orow{i}", [128, NA_W]) for i in range(3)]; dorow = [Dep() for _ in range(3)]
            ucount = [0]

            def unit(nq, qsl_of, segs, vsegs, ob, ocol, tail=None):
                u = ucount[0]; ucount[0] += 1
                b = u % NB
                bkA, dA = self.ps[2 * b], self.psd[2 * b]
                bkB, dB = self.ps[2 * b + 1], self.psd[2 * b + 1]
                ntot = sum(s_[1] for s_ in segs)
                off = 0
                outs = []
                for (rhs, ncol, bias) in segs:
                    bk, dbk = (bkB, dB) if bias is not None else (bkA, dA)
                    em.op("pe", lambda e, bk=bk, rhs=rhs, ncol=ncol: e.matmul(bk[0:nq, 0:ncol], qsl_of, rhs, start=True, stop=True), reads=[dqT, dkT], writes=[dbk])
                    outs.append((bk, dbk, ncol, bias, off))
                    off += ncol
                yield
                for (bk, dbk, ncol, bias, off) in outs:
                    if bias is not None:
                        em.op("dve", lambda e, bk=bk, off=off, ncol=ncol, bias=bias: e.tensor_tensor(sal[b][0:nq, off:off + ncol], bk[0:nq, 0:ncol], bias, ALU.add), reads=[dbk, dGm], writes=[dsal[b]])
                    else:
                        em.op("act", lambda e, bk=bk, off=off, ncol=ncol: e.activation(sal[b][0:nq, off:off + ncol], bk[0:nq, 0:ncol], AF.Copy), reads=[dbk], writes=[dsal[b]])
                yield
                em.op("dve", lambda e: e.reduce_max(stt[b][0:nq, 1:2], sal[b][0:nq, 0:ntot], AX.X, negate=True), reads=[dsal[b]], writes=[dstt[b]])
                yield
                em.op("act", lambda e: e.activation(pbf[b][0:nq, 0:ntot], sal[b][0:nq, 0:ntot], AF.Exp, bias=stt[b][0:nq, 1:2], scale=1.0, accum_out=stt[b][0:nq, 2:3]),
                      reads=[dsal[b], dstt[b]], writes=[dpbf[b], dstt[b]])
                yield
                nw = ntot // 128
                pb = bkA[:].bitcast(BF16)
                for w in range(nw):
                    em.op("pe", lambda e, w=w: e.transpose(pb[:, 512 + w * nq:512 + (w + 1) * nq], pbf[b][0:nq, w * 128:(w + 1) * 128], self.ident_b[0:nq, 0:nq]),
                          reads=[dpbf[b], self.d_ident_b], writes=[dA])
                yield
                em.op("dve", lambda e: e.tensor_copy(PT[b][:, 0:nw * nq], pb[:, 512:512 + nw * nq]), reads=[dA], writes=[dPT[b]])
                em.op("dve", lambda e: e.reciprocal(stt[b][0:nq, 3:4], stt[b][0:nq, 2:3]), reads=[dstt[b]], writes=[dstt[b]])
                yield
                for w in range(nw):
                    em.op("pe", lambda e, w=w: e.matmul(bkA[0:nq, 448:512], PT[b][:, w * nq:(w + 1) * nq], vsegs[w], start=(w == 0), stop=(w == nw - 1)),
                          reads=[dPT[b], dV, dVs], writes=[dA], inc=(w == nw - 1))
                yield
                em.op("dve", lambda e: e.tensor_scalar(orow[ob][0:nq, ocol:ocol + 64], bkA[0:nq, 448:512], stt[b][0:nq, 3:4], None, ALU.mult), reads=[dA, dstt[b]], writes=[dorow[ob]])
                if tail is not None:
                    tail()
                yield

            units = []
            oi = 0
            if not last:
                for qt in range(2):
                    ob = oi % 3; oi += 1
                    for h in range(6):
                        tail = None
                        if h == 5:
                            tail = (lambda qt=qt, ob=ob: em.dma("sp", self.y_na[qt * 128:(qt + 1) * 128, :], orow[ob][:], reads=[dorow[ob]], writes=[self.d_yna]))
                        units.append(dict(nq=128, qsl_of=qT[:, h, qt * 128:(qt + 1) * 128], segs=[(kT[:, h, 0:LC], LC, None)],
                                          vsegs=[V[:, 0, h * 64:(h + 1) * 64], V[:, 1, h * 64:(h + 1) * 64]], ob=ob, ocol=h * 64, tail=tail))
            for i in range(32):
                r0 = min(max(i - 4, 0), 24)
                dr0 = r0 - i + 7
                ob = oi % 3; oi += 1
                for h in range(6):
                    q0 = LC + i * 64
                    k0 = LC + r0 * 64
                    bias = Gm[:, h, dr0:dr0 + 8, :].rearrange("p a c -> p (a c)")
                    if r0 % 2 == 0:
                        vl = [V[:, 2 + r0 // 2 + w, h * 64:(h + 1) * 64] for w in range(4)]
                    else:
                        vl = [Vs[:, (r0 - 1) // 2 + w, h * 64:(h + 1) * 64] for w in range(4)]
                    vl += [V[:, 0, h * 64:(h + 1) * 64], V[:, 1, h * 64:(h + 1) * 64]]
                    tail = None
                    if h == 5:
                        tail = (lambda i=i, ob=ob: em.dma("sp", self.y_na[LC + i * 64:LC + (i + 1) * 64, :], orow[ob][0:64, :], reads=[dorow[ob]], writes=[self.d_yna]))
                    units.append(dict(nq=64, qsl_of=qT[:, h, q0:q0 + 64], segs=[(kT[:, h, k0:k0 + 512], 512, bias), (kT[:, h, 0:LC], LC, None)],
                                      vsegs=vl, ob=ob, ocol=h * 64, tail=tail))
            active = []
            nxt_u = 0
            while nxt_u < len(units) or active:
                while len(active) < NB - 1 and nxt_u < len(units):
                    active.append(unit(**units[nxt_u])); nxt_u += 1
                for g in list(active):
                    try:
                        next(g)
                    except StopIteration:
                        active.remove(g)
            em.barrier()
        self.tap(f"yna{li}", self.y_na, [T, NA_W], reads=[self.d_yna])


    def hyena(self, li, last):
        seqs = [(SEQ, "l", LC)] + ([] if last else [(LC, "c", 0)])
        for (n, tag, tok0) in seqs:
            self.hyena_seq(li, n, tag, tok0)
        self.tap(f"yhy{li}", self.y_hy, [T, HY_W], reads=[self.d_yhy])

    def hyena_seq(self, li, n, tag, tok0):
        em = self.em
        nch = n // 128
        PI = math.pi
        I = self.I
        with ExitStack() as st:
            Hre = self.sb(st, "Hre", [128, nch, 512], BF16); Him = self.sb(st, "Him", [128, nch, 512], BF16); dH = Dep()
            NTB = 4
            tb = [[self.sb(st, f"tb{i}{j}", [128, nch, 128], BF16) for j in range(2)] for i in range(NTB)]
            dtb = [Dep() for _ in range(NTB)]
            tbi = 0
            with ExitStack() as st2:
                w1 = self.sb(st2, "hyw1", [33, 64]); w2 = self.sb(st2, "hyw2", [64, 64]); w3 = self.sb(st2, "hyw3", [64, 1024]); dw = Dep()
                em.dma("sp", w1[:], I["hy_f_w1"][li], writes=[dw]); em.dma("sp", w2[:], I["hy_f_w2"][li], writes=[dw]); em.dma("sp", w3[:], I["hy_f_w3"][li], writes=[dw])
                cols = self.sb(st2, "hycols", [64, 8]); dc = Dep()
                for j, nm in enumerate(("hy_f_freq", "hy_f_b1", "hy_f_b2")):
                    em.dma("sp", cols[:, j:j + 1], I[nm][li:li + 1, :].rearrange("o n -> n o"), writes=[dc], allow_slow_non_contiguous=True)
                em.op("dve", lambda e: e.tensor_tensor(cols[:, 3:4], cols[:, 1:2], cols[:, 0:1], ALU.mult), reads=[dc], writes=[dc])
                em.op("dve", lambda e: e.tensor_tensor(cols[:, 4:5], cols[:, 2:3], cols[:, 0:1], ALU.mult), reads=[dc], writes=[dc])
                em.op("dve", lambda e: e.memset(cols[:, 5:6], -PI), writes=[dc])
                h2T = self.sb(st2, "h2T", [64, n]); dh1 = Dep(); dh2 = Dep()
                st2a = ExitStack()
                zT = self.sb(st2a, "hyzT", [33, n]); dz = Dep()
                em.dma("sp", zT[:], I["hyz_" + tag], writes=[dz])
                h1T = self.sb(st2a, "h1T", [64, n])
                arg = self.sb(st2a, "hyarg", [64, 512]); t1 = self.sb(st2a, "hyt1", [64, 512]); darg = Dep(); dt1 = Dep()
                for (wm, kk_, src, dsrc, dst, ddst, bcol) in ((w1, 33, zT, dz, h1T, dh1, 3), (w2, 64, h1T, dh1, h2T, dh2, 4)):
                    for s in range(0, n, 512):
                        m = min(512, n - s)
                        ps, dp = self.psum()
                        em.op("pe", lambda e, ps=ps, wm=wm, kk_=kk_, src=src, s=s, m=m: e.matmul(ps[0:64, 0:m], wm[0:kk_, :], src[0:kk_, s:s + m], start=True, stop=True), reads=[dw, dsrc], writes=[dp])
                        em.op("dve", lambda e, ps=ps, m=m, bcol=bcol: e.tensor_scalar(arg[:, 0:m], ps[0:64, 0:m], cols[:, 0:1], cols[:, bcol:bcol + 1], ALU.mult, ALU.add), reads=[dp, dc], writes=[darg])
                        em.op("dve", lambda e, m=m: e.tensor_scalar(t1[:, 0:m], arg[:, 0:m], PI, -2 * PI, ALU.is_gt, ALU.mult), reads=[darg], writes=[dt1])
                        em.op("dve", lambda e, m=m: e.tensor_tensor(t1[:, 0:m], t1[:, 0:m], arg[:, 0:m], ALU.add), reads=[darg, dt1], writes=[dt1])
                        em.op("dve", lambda e, m=m: e.tensor_scalar(arg[:, 0:m], arg[:, 0:m], -PI, 2 * PI, ALU.is_lt, ALU.mult), reads=[darg, dt1], writes=[darg])
                        em.op("dve", lambda e, m=m: e.tensor_tensor(t1[:, 0:m], t1[:, 0:m], arg[:, 0:m], ALU.add), reads=[darg, dt1], writes=[dt1])
                        em.op("dve", lambda e, m=m: e.tensor_scalar(t1[:, 0:m], t1[:, 0:m], PI, -PI, ALU.min, ALU.max), reads=[dt1], writes=[dt1])
                        em.op("act", lambda e, m=m, dst=dst, s=s: e.activation(dst[:, s:s + m], t1[:, 0:m], AF.Sin), reads=[dt1], writes=[ddst])
                em.barrier()
                st2a.close()
                hF = self.sb(st2, "hF", [128, nch, 1024]); dhF = Dep()
                dec = self.sb(st2, "hydec", [128, nch, 256]); ddec = Dep()
                em.dma("sp", dec[:], I["hydec_" + tag], writes=[ddec])
                ones, dones = self.load_const(st2, "ones_f")
                for pc in range(nch):
                    for blk in range(2):
                        ps, dp = self.psum()
                        em.op("pe", lambda e, ps=ps, pc=pc, blk=blk: e.matmul(ps[:], h2T[:, pc * 128:(pc + 1) * 128], w3[:, blk * 512:(blk + 1) * 512], start=True, stop=True), reads=[dh2, dw], writes=[dp])
                        em.op("dve", lambda e, ps=ps, pc=pc, blk=blk: e.tensor_tensor(hF[:, pc, blk * 512:(blk + 1) * 512].rearrange("p (a c) -> p a c", a=2), ps[:].rearrange("p (a c) -> p a c", a=2),
                                                                                     dec[:, pc, :].rearrange("p (a c) -> p a c", a=1).to_broadcast([128, 2, 256]), ALU.mult), reads=[dp, ddec], writes=[dhF])
                em.op("dve", lambda e: e.memset(hF[0:1, 0, 256:512], 0.0), reads=[dhF], writes=[dhF])
                em.op("dve", lambda e: e.memset(hF[0:1, 0, 768:1024], 0.0), reads=[dhF], writes=[dhF])
                ab = [self.sb(st2, f"hyabs{i}", [128, 1024]) for i in range(2)]; dab = [Dep(), Dep()]
                pl = [self.psum(), self.psum()]
                for pc in range(nch):
                    b = pc % 2
                    em.op("act", lambda e, pc=pc, b=b: e.activation(ab[b][:], hF[:, pc, :], AF.Abs), reads=[dhF], writes=[dab[b]])
                    for blk in range(2):
                        em.op("pe", lambda e, pc=pc, blk=blk, b=b: e.matmul(pl[blk][0][:], ones[:], ab[b][:, blk * 512:(blk + 1) * 512], start=(pc == 0), stop=(pc == nch - 1)), reads=[dab[b], dones], writes=[pl[blk][1]])
                rl = self.sb(st2, "hyrl", [128, 2, 256]); drl = Dep()
                for o in range(2):
                    em.op("dve", lambda e, o=o: e.tensor_copy(rl[:, o, :], pl[o][0][:, 0:256]), reads=[pl[o][1]], writes=[drl])
                    em.op("dve", lambda e, o=o: e.tensor_tensor(rl[:, o, :], rl[:, o, :], pl[o][0][:, 256:512], ALU.add), reads=[pl[o][1], drl], writes=[drl])
                em.op("dve", lambda e: e.reciprocal(rl[:], rl[:]), reads=[drl], writes=[drl])
                he = self.sb(st2, "hye", [128, nch, 512], BF16); ho = self.sb(st2, "hyo", [128, nch, 512], BF16); dhe = Dep()
                tmpf = self.sb(st2, "hytmp", [128, 2, 256]); dtmpf = Dep()
                for pc in range(nch):
                    hv = hF[:, pc, :].rearrange("p (o d c) -> p o d c", o=2, d=2)
                    em.op("dve", lambda e, hv=hv: e.tensor_tensor(tmpf[:], hv[:, :, 0, :], hv[:, :, 1, :], ALU.add), reads=[dhF], writes=[dtmpf])
                    em.op("pool", lambda e, pc=pc: e.tensor_tensor(he[:, pc, :].rearrange("p (o c) -> p o c", o=2), tmpf[:], rl[:], ALU.mult), reads=[dtmpf, drl], writes=[dhe])
                    em.op("dve", lambda e, hv=hv: e.tensor_tensor(tmpf[:], hv[:, :, 0, :], hv[:, :, 1, :], ALU.subtract), reads=[dhF, dhe], writes=[dtmpf])
                    em.op("pool", lambda e, pc=pc: e.tensor_tensor(ho[:, pc, :].rearrange("p (o c) -> p o c", o=2), tmpf[:], rl[:], ALU.mult), reads=[dtmpf, drl], writes=[dhe])
                for fc in range(nch):
                    b = tbi % NTB; tbi += 1
                    em.dma("sp", tb[b][0][:], I["cm_" + tag][fc], writes=[dtb[b]])
                    em.dma("sp", tb[b][1][:], I["smn_" + tag][fc], writes=[dtb[b]])
                    for j, (src, dstH) in enumerate(((he, Hre), (ho, Him))):
                        ps, dp = self.psum()
                        for tc in range(nch):
                            em.op("pe", lambda e, ps=ps, tc=tc, b=b, j=j, src=src: e.matmul(ps[:], tb[b][j][:, tc, :], src[:, tc, :], start=(tc == 0), stop=(tc == nch - 1)), reads=[dtb[b], dhe], writes=[dp], inc=(tc == nch - 1))
                        em.op("act" if j == 0 else "dve", (lambda e, ps=ps, dstH=dstH, fc=fc: e.activation(dstH[:, fc, :], ps[:], AF.Copy)) if j == 0 else
                              (lambda e, ps=ps, dstH=dstH, fc=fc: e.tensor_copy(dstH[:, fc, :], ps[:])), reads=[dp], writes=[dH])
                em.barrier()
            cw = self.sb(st, "hycw", [128, 3, HY_IN]); dcw = Dep()
            for j in range(3):
                em.dma("sp", cw[:, j, :], row_bc(I["hy_conv_w"][li, j:j + 1, :]), writes=[dcw])
            cbt, dcb = self.load_bc(st, "hycb", I["hy_conv_b"][li:li + 1, :], HY_IN)
            sk = self.sb(st, "hysk", [128, 2, HY_W]); dsk = Dep()
            for o in range(2):
                em.dma("sp", sk[:, o, :], row_bc(I["hy_skip"][li, o:o + 1, :]), writes=[dsk])
            z = self.sb(st, "hyz", [128, nch, HY_W]); dzz = Dep()
            zb = self.sb(st, "hyzb", [128, nch, HY_W], BF16); dzb = Dep()
            xg = self.sb(st, "hyxg", [128, nch, 2, HY_W]); dxg = Dep()
            Yre = self.sb(st, "Yre", [128, nch, HY_W], BF16); Yim = self.sb(st, "Yim", [128, nch, HY_W], BF16); dY = Dep()
            with ExitStack() as st3:
                pcn = [[self.sb(st3, f"hyl{i}{j}", [128, HY_IN]) for j in range(3)] for i in range(2)]; dpcn = [Dep(), Dep()]
                acc = self.sb(st3, "hyacc", [128, HY_IN]); dacc = Dep()
                tm = self.sb(st3, "hytm", [128, HY_IN]); dtm = Dep()
                for tc in range(nch):
                    b = tc % 2
                    ur = urow(tok0 + tc * 128)
                    for j in range(3):
                        em.dma("sp", pcn[b][j][:], self.u_tok[ur + j - 1:ur + j - 1 + 128, 0:HY_IN], reads=[self.d_utok], writes=[dpcn[b]])
                    em.op("dve", lambda e, b=b: e.tensor_tensor(acc[:], pcn[b][0][:], cw[:, 0, :], ALU.mult), reads=[dpcn[b], dcw], writes=[dacc])
                    for j in (1, 2):
                        em.op("pool", lambda e, b=b, j=j: e.tensor_tensor(tm[:], pcn[b][j][:], cw[:, j, :], ALU.mult), reads=[dpcn[b], dcw], writes=[dtm])
                        em.op("dve", lambda e: e.tensor_tensor(acc[:], acc[:], tm[:], ALU.add), reads=[dtm, dacc], writes=[dacc])
                    em.op("dve", lambda e, tc=tc: e.tensor_tensor(z[:, tc, :], acc[:, 0:256], cbt[:, 0:256], ALU.add), reads=[dacc, dcb], writes=[dzz])
                    em.op("act", lambda e, tc=tc: e.activation(zb[:, tc, :], z[:, tc, :], AF.Copy), reads=[dzz], writes=[dzb])
                    em.op("pool", lambda e, tc=tc: e.tensor_tensor(xg[:, tc, :, :].rearrange("p a c -> p (a c)"), acc[:, 256:768], cbt[:, 256:768], ALU.add), reads=[dacc, dcb], writes=[dxg])
                em.barrier()
            pa2 = [self.sb(st, f"hypa{i}", [128, HY_W]) for i in range(2)]; pb2 = [self.sb(st, f"hypb{i}", [128, HY_W]) for i in range(2)]
            pc2 = [self.sb(st, f"hypc{i}", [128, HY_W]) for i in range(2)]; pd2 = [self.sb(st, f"hypd{i}", [128, HY_W]) for i in range(2)]
            dpa2 = [Dep(), Dep()]; dpb2 = [Dep(), Dep()]; dpc2 = [Dep(), Dep()]; dpd2 = [Dep(), Dep()]
            ot = [self.sb(st, f"hyot{i}", [128, HY_W]) for i in range(2)]; dot = [Dep(), Dep()]
            for o in range(2):
                for fc in range(nch):
                    b = tbi % NTB; tbi += 1
                    em.dma("sp", tb[b][0][:], I["cm_" + tag][fc], writes=[dtb[b]])
                    em.dma("sp", tb[b][1][:], I["smn_" + tag][fc], writes=[dtb[b]])
                    pr, dpr = self.psum(); pi, dpi = self.psum()
                    for tc in range(nch):
                        em.op("pe", lambda e, pr=pr, tc=tc, b=b: e.matmul(pr[:, 0:256], tb[b][0][:, tc, :], zb[:, tc, :], start=(tc == 0), stop=(tc == nch - 1)), reads=[dtb[b], dzb], writes=[dpr], inc=(tc == nch - 1))
                    for tc in range(nch):
                        em.op("pe", lambda e, pi=pi, tc=tc, b=b: e.matmul(pi[:, 0:256], tb[b][1][:, tc, :], zb[:, tc, :], start=(tc == 0), stop=(tc == nch - 1)), reads=[dtb[b], dzb], writes=[dpi], inc=(tc == nch - 1))
                    hr = Hre[:, fc, o * 256:(o + 1) * 256]; hi = Him[:, fc, o * 256:(o + 1) * 256]
                    pa, pb_, pc_, pd_ = pa2[fc % 2], pb2[fc % 2], pc2[fc % 2], pd2[fc % 2]
                    dpa, dpb, dpc, dpd = dpa2[fc % 2], dpb2[fc % 2], dpc2[fc % 2], dpd2[fc % 2]
                    em.op("dve", lambda e, pr=pr, hr=hr: e.tensor_tensor(pa[:], pr[:, 0:256], hr, ALU.mult), reads=[dpr, dH], writes=[dpa])
                    em.op("dve", lambda e, pi=pi, hi=hi: e.tensor_tensor(pb_[:], pi[:, 0:256], hi, ALU.mult), reads=[dpi, dH], writes=[dpb])
                    em.op("pool", lambda e, fc=fc: e.tensor_tensor(Yre[:, fc, :], pa[:], pb_[:], ALU.subtract), reads=[dpa, dpb], writes=[dY])
                    em.op("dve", lambda e, pr=pr, hi=hi: e.tensor_tensor(pc_[:], pr[:, 0:256], hi, ALU.mult), reads=[dpr, dH], writes=[dpc])
                    em.op("dve", lambda e, pi=pi, hr=hr: e.tensor_tensor(pd_[:], pi[:, 0:256], hr, ALU.mult), reads=[dpi, dH], writes=[dpd])
                    em.op("pool", lambda e, fc=fc: e.tensor_tensor(Yim[:, fc, :], pc_[:], pd_[:], ALU.add), reads=[dpc, dpd], writes=[dY])
                for tc in range(nch):
                    b = tbi % NTB; tbi += 1
                    em.dma("sp", tb[b][0][:], I["icm_" + tag][tc], writes=[dtb[b]])
                    em.dma("sp", tb[b][1][:], I["ismn_" + tag][tc], writes=[dtb[b]])
                    ps, dp = self.psum()
                    for fc in range(nch):
                        em.op("pe", lambda e, ps=ps, fc=fc, b=b: e.matmul(ps[:, 0:256], tb[b][0][:, fc, :], Yre[:, fc, :], start=(fc == 0), stop=False), reads=[dtb[b], dY], writes=[dp])
                    for fc in range(nch):
                        em.op("pe", lambda e, ps=ps, fc=fc, b=b: e.matmul(ps[:, 0:256], tb[b][1][:, fc, :], Yim[:, fc, :], start=False, stop=(fc == nch - 1)), reads=[dtb[b], dY], writes=[dp], inc=(fc == nch - 1))
                    pa, dpa = pa2[tc % 2], dpa2[tc % 2]
                    em.op("pool", lambda e, tc=tc, o=o, pa=pa: e.tensor_tensor(pa[:], z[:, tc, :], sk[:, o, :], ALU.mult), reads=[dzz, dsk], writes=[dpa])
                    em.op("dve", lambda e, ps=ps, pa=pa: e.tensor_tensor(pa[:], pa[:], ps[:, 0:256], ALU.add), reads=[dp, dpa], writes=[dpa])
                    if o == 0:
                        em.op("pool", lambda e, tc=tc, o=o, pa=pa: e.tensor_tensor(z[:, tc, :], pa[:], xg[:, tc, o, :], ALU.mult), reads=[dpa, dxg], writes=[dzz])
                        em.op("act", lambda e, tc=tc: e.activation(zb[:, tc, :], z[:, tc, :], AF.Copy), reads=[dzz], writes=[dzb])
                    else:
                        ob = tc % 2
                        em.op("pool", lambda e, tc=tc, o=o, ob=ob, pa=pa: e.tensor_tensor(ot[ob][:], pa[:], xg[:, tc, o, :], ALU.mult), reads=[dpa, dxg], writes=[dot[ob]])
                        r0 = tok0 + tc * 128
                        em.dma("sp", self.y_hy[r0:r0 + 128, :], ot[ob][:], reads=[dot[ob]], writes=[self.d_yhy])
            em.barrier()


    def rwkv(self, li, last):
        em = self.em
        I = self.I
        NCH = T // 64
        CD = 0.6065306597126334
        U = self.u_rwT
        with ExitStack() as st:
            def colload(dst, j, src_row):
                em.dma("sp", dst[:, j:j + 1], src_row.rearrange("o n -> n o"), writes=[dcols], allow_slow_non_contiguous=True)

            def shift(dst, ddst, src, dsrc, P_, f0):
                mu = self.sb(st_sh, "mu", [P_, 4]); dmu = Dep()
                for j in range(2):
                    em.dma("sp", mu[:, j:j + 1], I["rw_shift"][li, j:j + 1, f0:f0 + P_].rearrange("o n -> n o"), writes=[dmu], allow_slow_non_contiguous=True)
                em.op("dve", lambda e: e.tensor_tensor(mu[:, 2:3], mu[:, 0:1], mu[:, 1:2], ALU.add), reads=[dmu], writes=[dmu])
                em.op("dve", lambda e: e.tensor_scalar(mu[:, 2:3], mu[:, 2:3], -1.0, 1.0, ALU.mult, ALU.add), reads=[dmu], writes=[dmu])
                em.op("dve", lambda e: e.tensor_scalar(dst[:], src[:], mu[:, 2:3], None, ALU.mult), reads=[dsrc, dmu], writes=[ddst])
                for (a0, a1) in ((0, LC), (LC, T)):
                    em.op("dve", lambda e, a0=a0, a1=a1: e.scalar_tensor_tensor(dst[:, a0 + 1:a1], src[:, a0:a1 - 1], mu[:, 0:1], dst[:, a0 + 1:a1], ALU.mult, ALU.add), reads=[dsrc, dmu, ddst], writes=[ddst])
                    em.op("dve", lambda e, a0=a0, a1=a1: e.scalar_tensor_tensor(dst[:, a0:a1 - 1], src[:, a0 + 1:a1], mu[:, 1:2], dst[:, a0:a1 - 1], ALU.mult, ALU.add), reads=[dsrc, dmu, ddst], writes=[ddst])

            st_sh = st
            lwt = self.sb(st, "lwt", [64, 2, T], BF16); las = self.sb(st, "las", [64, 2, T], BF16); lgs = self.sb(st, "lgs", [128, T], BF16); dsh = Dep()
            w2b = self.sb(st, "rw2b", [64, 2, RW_W], BF16); a2b = self.sb(st, "ra2b", [64, 2, RW_W], BF16); g2b = self.sb(st, "rg2b", [128, RW_W], BF16); dwl = Dep()
            for d in range(2):
                em.dma("pool", w2b[:, d, :], I["rw_w2"][li, d], writes=[dwl])
                em.dma("pool", a2b[:, d, :], I["rw_a2"][li, d], writes=[dwl])
            em.dma("pool", g2b[:], I["rw_g2"][li], writes=[dwl])
            raw = self.sb(st, "rwraw", [128, T]); draw = Dep()
            T1 = self.sb(st, "rwT1", [128, T]); dT1 = Dep()
            T2 = self.sb(st, "rwT2", [64, T]); dT2 = Dep()
            for d in range(2):
                for (f0, dstb, fn) in ((1152 + d * 64, lwt, AF.Tanh), (1280 + d * 64, las, AF.Copy)):
                    em.dma("sp", raw[0:64, :], U[f0:f0 + 64, :], reads=[self.d_urw], writes=[draw])
                    shift(T1[0:64, :], dT1, raw[0:64, :], draw, 64, f0)
                    em.op("act", lambda e, d=d, dstb=dstb, fn=fn: e.activation(dstb[:, d, :], T1[0:64, :], fn), reads=[dT1], writes=[dsh])
            em.dma("sp", raw[:], U[1408:1536, :], reads=[self.d_urw], writes=[draw])
            shift(T1, dT1, raw, draw, 128, 1408)
            em.op("act", lambda e: e.activation(lgs[:], T1[:], AF.Sigmoid), reads=[dT1], writes=[dsh])
            ropeC, drc = self.load_const(st, "rope_cos"); ropeS, drs = self.load_const(st, "rope_sin"); PT_, dPT = self.load_const(st, "rope_PT")
            ones, dones = self.load_const(st, "ones_f")
            cmask, dcmask = self.load_const(st, "rw_cmask")
            mks = {}
            for d, (a, b_, p) in enumerate((("mk_f_strict", "mk_f_incl", "mk_b_strict"), ("mk_b_strict", "mk_b_incl", "mk_f_strict"))):
                m2 = self.sb(st, f"mk2_{d}", [64, 128]); mp = self.sb(st, f"mkp_{d}", [64, 64]); dmk = Dep()
                em.dma("sp", m2[:, 0:64], I[a], writes=[dmk]); em.dma("sp", m2[:, 64:128], I[b_], writes=[dmk]); em.dma("sp", mp[:], I[p], writes=[dmk])
                mks[d] = (m2, mp, dmk)
            idf = self.ident_f
            r_ = self.sb(st, "rw_r", [64, T]); k_ = self.sb(st, "rw_k", [64, T]); kk = self.sb(st, "rw_kk", [64, T]); dr_ = Dep(); dk_ = Dep(); dkk = Dep()
            Vt = self.sb(st, "rw_Vt", [64, NCH, 64], BF16); dVt = Dep()
            yacc = self.sb(st, "rw_y", [64, NCH, 64]); dy = Dep()
            sg = self.sb(st, "rw_sg", [64, T]); kd = self.sb(st, "rw_kd", [64, T], BF16); bd = self.sb(st, "rw_bd", [64, T], BF16); dsg = Dep(); dkd = Dep(); dbd = Dep()
            TB = self.sb(st, "rw_TB", [64, T], BF16); dTB = Dep()
            coef = self.sb(st, "rw_coef", [64, NCH]); dcoef = Dep()
            cols = self.sb(st, "rw_cols", [64, 12]); dcols = Dep()
            gnb = self.sb(st, "rw_gn", [64, 2, 64]); dgn = Dep()
            st_ = self.sb(st, "rw_st", [64, NCH, 2])
            DD = []
            for d in range(2):
                o = {}
                o["AR"] = self.sb(st, f"rw_AR{d}", [64, NCH, 128], BF16); o["dAR"] = Dep()
                o["bdb"] = self.sb(st, f"rw_bdb{d}", [64, T], BF16); o["kdb"] = self.sb(st, f"rw_kdb{d}", [64, T], BF16); o["dbdb"] = Dep(); o["dkdb"] = Dep()
                o["Bh"] = self.sb(st, f"rw_Bh{d}", [64, NCH, 64], BF16); o["Kh"] = self.sb(st, f"rw_Kh{d}", [64, NCH, 64], BF16); o["dBh"] = Dep(); o["dKh"] = Dep()
                o["gC"] = self.sb(st, f"rw_gC{d}", [64, NCH]); o["dgC"] = Dep()
                o["S"] = [self.sb(st, f"rw_S{d}{i}", [64, 64]) for i in range(2)]; o["dS"] = [Dep(), Dep()]
                o["Sb"] = [self.sb(st, f"rw_Sb{d}{i}", [64, 64], BF16) for i in range(2)]; o["dSb"] = [Dep(), Dep()]
                o["MB"] = [self.sb(st, f"rw_MB{d}{i}", [64, 128], BF16) for i in range(2)]; o["dMB"] = [Dep(), Dep()]
                o["MK"] = [self.sb(st, f"rw_MK{d}{i}", [64, 128], BF16) for i in range(2)]; o["dMK"] = [Dep(), Dep()]
                o["QP"] = [self.sb(st, f"rw_QP{d}{i}", [64, 128], BF16) for i in range(4)]; o["dQP"] = [Dep() for _ in range(4)]
                o["R"] = [self.sb(st, f"rw_R{d}{i}", [64, 64], BF16) for i in range(2)]; o["dR"] = [Dep(), Dep()]
                o["X"] = [self.sb(st, f"rw_X{d}{i}", [64, 64], BF16) for i in range(2)]; o["dX"] = [Dep(), Dep()]
                o["U"] = [self.sb(st, f"rw_U{d}{i}", [64, 64], BF16) for i in range(2)]; o["dU"] = [Dep(), Dep()]
                DD.append(o)
            T13 = T1[0:64, :].rearrange("p (c t) -> p c t", t=64); T23 = T2[:].rearrange("p (c t) -> p c t", t=64)
            TB3 = TB[:].rearrange("p (c t) -> p c t", t=64)
            v3 = lambda a: a[:].rearrange("p (c t) -> p c t", t=64)
            idb = self.ident_b
            for d in range(2):
                DD[d]["bankI"] = (self.ps[4 + 2 * d], self.psd[4 + 2 * d])
                DD[d]["bankC"] = (self.ps[5 + 2 * d], self.psd[5 + 2 * d])
            self.ps_n = 4

            def transp_chunks(src3, dsrc, dst, ddst):
                for c0 in range(0, NCH, 8):
                    nb = min(8, NCH - c0)
                    ps, dp = self.psum(); pb = ps[:].bitcast(BF16)
                    for j in range(nb):
                        em.op("pe", lambda e, pb=pb, j=j, c0=c0: e.transpose(pb[0:64, j * 64:(j + 1) * 64], src3[:, c0 + j, :], idb[0:64, 0:64]), reads=[dsrc, self.d_ident_b], writes=[dp])
                    em.op("act", lambda e, pb=pb, c0=c0, nb=nb: e.activation(dst[:, c0:c0 + nb, :], pb[0:64, 0:nb * 64].rearrange("p (c k) -> p c k", k=64), AF.Copy), reads=[dp], writes=[ddst])

            for h in range(6):
                hs = slice(h * 64, (h + 1) * 64)
                for j, (nm, d) in enumerate((("rw_w0", 0), ("rw_w0", 1), ("rw_a0", 0), ("rw_a0", 1))):
                    colload(cols, j, I[nm][li, d:d + 1, hs])
                for j, nm in enumerate(("rw_kk", "rw_ka", "rw_rk")):
                    colload(cols, 4 + j, I[nm][li:li + 1, hs])
                em.op("dve", lambda e: e.tensor_scalar(cols[:, 7:8], cols[:, 5:6], -1.0, 1.0, ALU.mult, ALU.add), reads=[dcols], writes=[dcols])
                em.dma("sp", gnb[:, 0, :], row_bc(I["rw_gn_g"][li:li + 1, hs], 64), writes=[dgn])
                em.dma("sp", gnb[:, 1, :], row_bc(I["rw_gn_b"][li:li + 1, hs], 64), writes=[dgn])
                em.op("pool", lambda e: e.memset(yacc[:], 0.0), writes=[dy])
                for (f0, dst, ddst, rope) in ((h * 64, r_, dr_, True), (384 + h * 64, k_, dk_, True), (768 + h * 64, T2, dT2, False)):
                    em.dma("sp", raw[0:64, :], U[f0:f0 + 64, :], reads=[self.d_urw], writes=[draw])
                    shift(dst, ddst, raw[0:64, :], draw, 64, f0)
                    if rope:
                        for s in range(0, SEQ, 512):
                            ps, dp = self.psum()
                            em.op("pe", lambda e, ps=ps, s=s, dst=dst: e.matmul(ps[0:64, :], PT_[:], dst[:, LC + s:LC + s + 512], start=True, stop=True), reads=[ddst, dPT], writes=[dp])
                            em.op("dve", lambda e, ps=ps, s=s: e.tensor_tensor(T1[0:64, s:s + 512], ps[0:64, :], ropeS[:, s:s + 512], ALU.mult), reads=[dp, drs], writes=[dT1])
                            em.op("pool", lambda e, s=s, dst=dst: e.tensor_tensor(dst[:, LC + s:LC + s + 512], dst[:, LC + s:LC + s + 512], ropeC[:, s:s + 512], ALU.mult), reads=[ddst, drc], writes=[ddst])
                            em.op("dve", lambda e, s=s, dst=dst: e.tensor_tensor(dst[:, LC + s:LC + s + 512], dst[:, LC + s:LC + s + 512], T1[0:64, s:s + 512], ALU.add), reads=[ddst, dT1], writes=[ddst])
                em.op("act", lambda e: e.activation(TB[:], T2[:], AF.Copy), reads=[dT2], writes=[dTB])
                transp_chunks(TB3, dTB, Vt, dVt)
                em.op("dve", lambda e: e.tensor_scalar(kk[:], k_[:], cols[:, 4:5], None, ALU.mult), reads=[dk_, dcols], writes=[dkk])
                em.op("act", lambda e: e.activation(T1[0:64, :], kk[:], AF.Square), reads=[dkk], writes=[dT1])
                for s in range(0, T, 512):
                    n = min(512, T - s)
                    ps, dp = self.psum()
                    em.op("pe", lambda e, ps=ps, s=s, n=n: e.matmul(ps[0:64, 0:n], ones[0:64, 0:64], T1[0:64, s:s + n], start=True, stop=True), reads=[dT1, dones], writes=[dp])
                    em.op("act", lambda e, ps=ps, s=s, n=n: e.activation(T2[:, s:s + n], ps[0:64, 0:n], AF.Sqrt), reads=[dp], writes=[dT2])
                em.op("dve", lambda e: e.tensor_scalar(T2[:], T2[:], 1e-12, None, ALU.max), reads=[dT2], writes=[dT2])
                em.op("dve", lambda e: e.reciprocal(T2[:], T2[:]), reads=[dT2], writes=[dT2])
                em.op("dve", lambda e: e.tensor_tensor(kk[:], kk[:], T2[:], ALU.mult), reads=[dT2, dkk], writes=[dkk])
                for d in range(2):
                    o = DD[d]
                    AR, dAR, gC, dgC = o["AR"], o["dAR"], o["gC"], o["dgC"]
                    for s in range(0, T, 512):
                        n = min(512, T - s)
                        ps, dp = self.psum()
                        em.op("pe", lambda e, ps=ps, s=s, n=n, d=d: e.matmul(ps[0:64, 0:n], w2b[:, d, hs], lwt[:, d, s:s + n], start=True, stop=True), reads=[dwl, dsh], writes=[dp])
                        em.op("act", lambda e, ps=ps, s=s, n=n, d=d: e.activation(sg[:, s:s + n], ps[0:64, 0:n], AF.Sigmoid, bias=cols[:, d:d + 1]), reads=[dp, dcols], writes=[dsg])
                        ps, dp = self.psum()
                        em.op("pe", lambda e, ps=ps, s=s, n=n, d=d: e.matmul(ps[0:64, 0:n], a2b[:, d, hs], las[:, d, s:s + n], start=True, stop=True), reads=[dwl, dsh], writes=[dp])
                        em.op("act", lambda e, ps=ps, s=s, n=n, d=d: e.activation(bd[:, s:s + n], ps[0:64, 0:n], AF.Sigmoid, bias=cols[:, 2 + d:3 + d]), reads=[dp, dcols], writes=[dbd])
                    em.op("dve", lambda e: e.tensor_scalar(kd[:], bd[:], cols[:, 5:6], cols[:, 7:8], ALU.mult, ALU.add), reads=[dbd, dcols], writes=[dkd])
                    em.op("dve", lambda e: e.tensor_tensor(kd[:], kd[:], k_[:], ALU.mult), reads=[dkd, dk_], writes=[dkd])
                    em.op("pool", lambda e: e.tensor_tensor(bd[:], bd[:], kk[:], ALU.mult), reads=[dbd, dkk], writes=[dbd])
                    em.op("dve", lambda e: e.scalar_tensor_tensor(T1[0:64, :], kd[:], cols[:, 6:7], r_[:], ALU.mult, ALU.mult), reads=[dkd, dr_, dcols], writes=[dT1])
                    ps, dp = self.psum()
                    for c in range(NCH):
                        em.op("pe", lambda e, ps=ps, c=c: e.matmul(ps[0:64, c:c + 1], T13[:, c, :], ones[0:64, 0:1], start=True, stop=True), reads=[dT1, dones], writes=[dp])
                    if d == 0:
                        em.op("dve", lambda e, ps=ps: e.tensor_copy(coef[:], ps[0:64, 0:NCH]), reads=[dp], writes=[dcoef])
                    else:
                        em.op("dve", lambda e, ps=ps: e.tensor_tensor(coef[:], coef[:], ps[0:64, 0:NCH], ALU.add), reads=[dp, dcoef], writes=[dcoef])
                    em.op("dve", lambda e: e.tensor_tensor_scan(T2[:], cmask[:], sg[:], 0.0, ALU.mult, ALU.add), reads=[dsg, dcmask], writes=[dT2])
                    if d == 0:
                        em.op("dve", lambda e: e.tensor_tensor(T1[0:64, :], T2[:], sg[:], ALU.subtract), reads=[dT2, dsg], writes=[dT1])
                    else:
                        em.op("dve", lambda e: e.scalar_tensor_tensor(T13, T23, -1.0, T23[:, :, 63:64].to_broadcast([64, NCH, 64]), ALU.mult, ALU.add), reads=[dT2], writes=[dT1])
                        em.op("dve", lambda e: e.tensor_tensor(T2[:], T1[0:64, :], sg[:], ALU.add), reads=[dT1, dsg], writes=[dT2])
                    em.op("act", lambda e: e.activation(T1[0:64, :], T1[0:64, :], AF.Exp, scale=-CD), reads=[dT1], writes=[dT1])
                    em.op("dve", lambda e, AR=AR: e.scalar_tensor_tensor(AR[:, :, 0:64], v3(kk), -1.0, T13, ALU.mult, ALU.mult), reads=[dkk, dT1], writes=[dAR])
                    em.op("act", lambda e: e.activation(T1[0:64, :], T2[:], AF.Exp, scale=-CD), reads=[dT2, dAR], writes=[dT1])
                    em.op("dve", lambda e, AR=AR: e.tensor_tensor(AR[:, :, 64:128], v3(r_), T13, ALU.mult), reads=[dr_, dT1], writes=[dAR])
                    gsel = 63 if d == 0 else 0
                    em.op("dve", lambda e, gsel=gsel, gC=gC: e.tensor_copy(gC[:], T13[:, :, gsel]), reads=[dT1], writes=[dgC])
                    em.op("act", lambda e: e.activation(T2[:], T2[:], AF.Exp, scale=CD), reads=[dT2], writes=[dT2])
                    em.op("dve", lambda e, o=o: e.tensor_tensor(o["bdb"][:], bd[:], T2[:], ALU.mult), reads=[dbd, dT2], writes=[o["dbdb"]])
                    em.op("pool", lambda e, o=o: e.tensor_tensor(o["kdb"][:], kd[:], T2[:], ALU.mult), reads=[dkd, dT2], writes=[o["dkdb"]])
                    gbc = gC[:].rearrange("p (c o) -> p c o", o=1).to_broadcast([64, NCH, 64])
                    em.op("dve", lambda e, o=o, gbc=gbc: e.tensor_tensor(TB3, v3(o["bdb"]), gbc, ALU.mult), reads=[o["dbdb"], dgC], writes=[dTB])
                    transp_chunks(TB3, dTB, o["Bh"], o["dBh"])
                    em.op("dve", lambda e, o=o, gbc=gbc: e.tensor_tensor(TB3, v3(o["kdb"]), gbc, ALU.mult), reads=[o["dkdb"], dgC], writes=[dTB])
                    transp_chunks(TB3, dTB, o["Kh"], o["dKh"])
                    em.op("pool", lambda e, o=o: e.memset(o["S"][0][:], 0.0), writes=[o["dS"][0]])
                    em.op("pool", lambda e, o=o: e.memset(o["Sb"][0][:], 0.0), writes=[o["dSb"][0]])

                def indep(d, ci, c):
                    o = DD[d]
                    m2, mp, dmk = mks[d]
                    AR, dAR = o["AR"], o["dAR"]
                    MB, MK, QP, Rm = o["MB"], o["MK"], o["QP"], o["R"]
                    dMB, dMK, dQP, dRm = o["dMB"], o["dMK"], o["dQP"], o["dR"]
                    bd3 = v3(o["bdb"]); kd3 = v3(o["kdb"])
                    b = ci % 2
                    A = AR[:, c, 0:64]; BT = bd3[:, c, :]; KT = kd3[:, c, :]
                    bk, dbk = o["bankI"]
                    q0 = (ci * 2) % 4
                    em.op("pe", lambda e: e.matmul(bk[0:64, 0:128], BT, AR[:, c, :], start=True, stop=True), reads=[o["dbdb"], dAR], writes=[dbk])
                    em.op("pe", lambda e: e.matmul(bk[0:64, 128:256], KT, AR[:, c, :], start=True, stop=True), reads=[o["dkdb"], dAR], writes=[dbk])
                    em.op("pe", lambda e: e.matmul(bk[0:64, 256:320], A, BT, start=True, stop=True), reads=[o["dbdb"], dAR], writes=[dbk])
                    yield
                    em.op("dve", lambda e: e.tensor_tensor(MB[b][:], bk[0:64, 0:128], m2[:], ALU.mult), reads=[dbk, dmk], writes=[dMB[b]])
                    em.op("dve", lambda e: e.tensor_tensor(QP[q0][:, 64:128], bk[0:64, 256:320], mp[:], ALU.mult), reads=[dbk, dmk], writes=[dQP[q0]])
                    em.op("dve", lambda e: e.tensor_tensor(MK[b][:], bk[0:64, 128:256], m2[:], ALU.mult), reads=[dbk, dmk], writes=[dMK[b]])
                    yield
                    em.op("pool", lambda e: e.tensor_copy(QP[q0][:, 0:64], MB[b][:, 0:64]), reads=[dMB[b]], writes=[dQP[q0]])
                    em.op("pool", lambda e: e.tensor_tensor(Rm[b][:], MB[b][:, 0:64], idb[0:64, 0:64], ALU.add), reads=[dMB[b], self.d_ident_b], writes=[dRm[b]])
                    yield
                    cur = q0
                    for j in range(1, 6):
                        nxt = q0 + (1 if cur == q0 else 0)
                        pq, dpq = bk[:, 320:448], dbk
                        if j < 5:
                            em.op("pe", lambda e, pq=pq, cur=cur: e.matmul(pq[0:64, 0:64], QP[cur][:, 64:128], QP[cur][:, 0:64], start=True, stop=True), reads=[dQP[cur]], writes=[dpq])
                        em.op("pe", lambda e, pq=pq, cur=cur: e.matmul(pq[0:64, 64:128], QP[cur][:, 0:64], QP[cur][:, 64:128], start=True, stop=True), reads=[dQP[cur]], writes=[dpq])
                        lo = 0 if j < 5 else 64
                        em.op("act", lambda e, pq=pq, nxt=nxt, lo=lo: e.activation(QP[nxt][:, lo:128], pq[0:64, lo:128], AF.Copy), reads=[dpq], writes=[dQP[nxt]])
                        yield
                        pr, dpr = bk[:, 448:512], dbk
                        em.op("pe", lambda e, pr=pr, nxt=nxt: e.matmul(pr[0:64, 0:64], QP[nxt][:, 64:128], Rm[b][:], start=True, stop=True), reads=[dQP[nxt], dRm[b]], writes=[dpr])
                        em.op("dve", lambda e, pr=pr: e.tensor_tensor(Rm[b][:], pr[0:64, 0:64], Rm[b][:], ALU.add), reads=[dpr, dRm[b]], writes=[dRm[b]])
                        cur = nxt
                        yield

                def chain(d, ci, c):
                    o = DD[d]
                    AR, dAR = o["AR"], o["dAR"]
                    MB, MK, Rm, Xs, Us = o["MB"], o["MK"], o["R"], o["X"], o["U"]
                    dMB, dMK, dRm, dXs, dUs = o["dMB"], o["dMK"], o["dR"], o["dX"], o["dU"]
                    b = ci % 2
                    A = AR[:, c, 0:64]; Rr = AR[:, c, 64:128]
                    Sc, dSc = o["S"][ci % 2], o["dS"][ci % 2]
                    Sn, dSn = o["S"][(ci + 1) % 2], o["dS"][(ci + 1) % 2]
                    Sbc, dSbc = o["Sb"][ci % 2], o["dSb"][ci % 2]
                    Sbn, dSbn = o["Sb"][(ci + 1) % 2], o["dSb"][(ci + 1) % 2]
                    bc_, dbc = o["bankC"]
                    pX, dpX = bc_[:, 0:64], dbc
                    em.op("pe", lambda e: e.matmul(pX[0:64, 0:64], A, Sbc[:], start=True, stop=False), reads=[dAR, dSbc], writes=[dpX])
                    em.op("pe", lambda e: e.matmul(pX[0:64, 0:64], MK[b][:, 0:64], Vt[:, c, :], start=False, stop=True), reads=[dMK[b], dVt], writes=[dpX])
                    em.op("act", lambda e: e.activation(Xs[b][:], pX[0:64, 0:64], AF.Copy), reads=[dpX], writes=[dXs[b]])
                    yield
                    pU, dpU = bc_[:, 64:128], dbc
                    em.op("pe", lambda e: e.matmul(pU[0:64, 0:64], Rm[b][:], Xs[b][:], start=True, stop=True), reads=[dRm[b], dXs[b]], writes=[dpU])
                    em.op("act", lambda e: e.activation(Us[b][:], pU[0:64, 0:64], AF.Copy), reads=[dpU], writes=[dUs[b]])
                    yield
                    pS, dpS = bc_[:, 128:192], dbc
                    pY, dpY = bc_[:, 192:256], dbc
                    em.op("pe", lambda e: e.matmul(pS[0:64, 0:64], o["Bh"][:, c, :], Us[b][:], start=True, stop=False), reads=[o["dBh"], dUs[b]], writes=[dpS])
                    em.op("pe", lambda e: e.matmul(pS[0:64, 0:64], o["Kh"][:, c, :], Vt[:, c, :], start=False, stop=True), reads=[o["dKh"], dVt], writes=[dpS])
                    em.op("pe", lambda e: e.matmul(pY[0:64, 0:64], Rr, Sbc[:], start=True, stop=False), reads=[dAR, dSbc], writes=[dpY])
                    em.op("pe", lambda e: e.matmul(pY[0:64, 0:64], MB[b][:, 64:128], Us[b][:], start=False, stop=False), reads=[dMB[b], dUs[b]], writes=[dpY])
                    em.op("pe", lambda e: e.matmul(pY[0:64, 0:64], MK[b][:, 64:128], Vt[:, c, :], start=False, stop=True), reads=[dMK[b], dVt], writes=[dpY])
                    yield
                    em.op("dve", lambda e: e.scalar_tensor_tensor(Sbn[:], Sc[:], o["gC"][:, c:c + 1], pS[0:64, 0:64], ALU.mult, ALU.add), reads=[dpS, dSc, o["dgC"]], writes=[dSbn])
                    em.op("dve", lambda e: e.scalar_tensor_tensor(Sn[:], Sc[:], o["gC"][:, c:c + 1], pS[0:64, 0:64], ALU.mult, ALU.add), reads=[dpS, dSc, o["dgC"]], writes=[dSn])
                    em.op("dve", lambda e: e.tensor_tensor(yacc[:, c, :], pY[0:64, 0:64], yacc[:, c, :], ALU.add), reads=[dpY, dy], writes=[dy])
                    yield

                orders = [list(range(4)) + list(range(4, NCH)), list(range(3, -1, -1)) + list(range(NCH - 1, 3, -1))]

                def rr(gens):
                    gens = list(gens)
                    while gens:
                        for g in list(gens):
                            try:
                                next(g)
                            except StopIteration:
                                gens.remove(g)
                rr([indep(0, 0, orders[0][0]), indep(1, 0, orders[1][0])])
                for ci in range(NCH):
                    gs = [chain(0, ci, orders[0][ci]), chain(1, ci, orders[1][ci])]
                    if ci + 1 < NCH:
                        gs += [indep(0, ci + 1, orders[0][ci + 1]), indep(1, ci + 1, orders[1][ci + 1])]
                    rr(gs)
                dst_ = Dep()
                em.op("dve", lambda e: e.reduce_sum(st_[:, :, 0], yacc[:], AX.X), reads=[dy], writes=[dst_])
                em.op("dve", lambda e: e.tensor_scalar(st_[:, :, 0], st_[:, :, 0], 1.0 / 64, None, ALU.mult), reads=[dst_], writes=[dst_])
                em.op("dve", lambda e: e.tensor_tensor(yacc[:], yacc[:], st_[:, :, 0:1].to_broadcast([64, NCH, 64]), ALU.subtract), reads=[dst_, dy], writes=[dy])
                em.op("act", lambda e: e.activation(T13, yacc[:], AF.Square), reads=[dy], writes=[dT1])
                em.op("dve", lambda e: e.reduce_sum(st_[:, :, 1], T13, AX.X), reads=[dT1], writes=[dst_])
                em.op("dve", lambda e: e.tensor_scalar(st_[:, :, 1], st_[:, :, 1], 1.0 / 64, 64e-5, ALU.mult, ALU.add), reads=[dst_], writes=[dst_])
                em.op("act", lambda e: e.activation(st_[:, :, 1], st_[:, :, 1], AF.Sqrt), reads=[dst_], writes=[dst_])
                em.op("dve", lambda e: e.reciprocal(st_[:, :, 1], st_[:, :, 1]), reads=[dst_], writes=[dst_])
                em.op("dve", lambda e: e.tensor_tensor(yacc[:], yacc[:], st_[:, :, 1:2].to_broadcast([64, NCH, 64]), ALU.mult), reads=[dst_, dy], writes=[dy])
                em.op("dve", lambda e: e.tensor_tensor(yacc[:], yacc[:], gnb[:, 0:1, :].to_broadcast([64, NCH, 64]), ALU.mult), reads=[dgn, dy], writes=[dy])
                em.op("pool", lambda e: e.tensor_tensor(yacc[:], yacc[:], gnb[:, 1:2, :].to_broadcast([64, NCH, 64]), ALU.add), reads=[dgn, dy], writes=[dy])
                em.op("dve", lambda e: e.tensor_tensor(T13, Vt[:], coef[:].rearrange("p (c o) -> p c o", o=1).to_broadcast([64, NCH, 64]), ALU.mult), reads=[dVt, dcoef, dT1], writes=[dT1])
                em.op("dve", lambda e: e.tensor_tensor(yacc[:], yacc[:], T13, ALU.add), reads=[dT1, dy], writes=[dy])
                for c0 in range(0, NCH, 8):
                    nb = min(8, NCH - c0)
                    ps, dp = self.psum()
                    for j in range(nb):
                        c = c0 + j
                        em.op("pe", lambda e, ps=ps, j=j, c=c: e.matmul(ps[0:64, j * 64:(j + 1) * 64], lgs[:, c * 64:(c + 1) * 64], g2b[:, hs], start=True, stop=True), reads=[dsh, dwl], writes=[dp])
                    em.op("dve", lambda e, ps=ps, c0=c0, nb=nb: e.tensor_tensor(yacc[:, c0:c0 + nb, :], yacc[:, c0:c0 + nb, :], ps[0:64, 0:nb * 64].rearrange("p (c k) -> p c k", k=64), ALU.mult), reads=[dp, dy], writes=[dy])
                em.dma("sp", self.y_rw.rearrange("(c p) f -> p c f", p=64)[:, :, hs], yacc[:], reads=[dy], writes=[self.d_yrw])
            self.ps_n = 8
            em.barrier()
        self.tap(f"yrw{li}", self.y_rw, [T, RW_W], reads=[self.d_yrw])

    def build(self, stages=("proj", "na", "hy", "rw", "merge", "ffn")):
        self.declare()
        self.setup_globals()
        self.adaln()
        for li in self.layers:
            last = li == DEPTH - 1
            if "proj" in stages:
                self.proj_in(li, list(range(NT)))
            if "na" in stages:
                self.na(li, last)
            if "hy" in stages:
                self.hyena(li, last)
            if "rw" in stages:
                self.rwkv(li, last)
            if "merge" in stages:
                self.merge(li, last)
            if "ffn" in stages:
                self.ffn(li, last)
        self.em.barrier()
        return self.nc


INJ_SHAPES = {"y_rw": [T, RW_W], "y_hy": [T, HY_W], "y_na": [T, NA_W]}


def prepare_shared(inputs):
    f = lambda a: np.ascontiguousarray(np.asarray(a, dtype=np.float32))
    sh = {}
    for k in ("ada_w", "ada_b", "norm1_g", "norm2_g", "w_in", "rw_shift", "rw_w0", "rw_w2", "rw_a0", "rw_a2", "rw_g2",
              "rw_kk", "rw_ka", "rw_gn_g", "rw_gn_b", "hy_conv_w", "hy_conv_b", "hy_f_w1", "hy_f_b1", "hy_f_w2",
              "hy_f_b2", "hy_f_w3", "hy_f_freq", "hy_skip", "na_q_gain", "na_k_gain", "w_br_rw", "w_br_hy", "w_br_na",
              "w_out", "ff_w1", "ff_w3", "ff_w2", "moe_w1", "moe_w3", "moe_w2"):
        sh[k] = f(inputs[k])
    sh["rw_rk"] = f(inputs["rw_rk"]).reshape(DEPTH, RW_W)
    sh["moe_routerT"] = np.ascontiguousarray(np.transpose(f(inputs["moe_router"]), (0, 2, 1)))
    sh["na_G"] = na_bias_gather(f(inputs["na_rpb"]))
    sh["cctx_lay"] = np.ascontiguousarray(f(inputs["c_ctx"]).reshape(8, 128).T)
    sh.update(host_constants())
    return sh


def core_inputs(inputs, sh, b):
    m = dict(sh)
    m["x"] = np.ascontiguousarray(np.asarray(inputs["x"][b], dtype=np.float32))
    m["ctx"] = np.ascontiguousarray(np.asarray(inputs["ctx"][b], dtype=np.float32))
    m["c_lay"] = np.ascontiguousarray(np.asarray(inputs["c"][b], dtype=np.float32).reshape(8, 128).T)
    return m


def kernel(**inputs):
    bld = Builder()
    nc = bld.build()
    sh = prepare_shared(inputs)
    in_maps = [core_inputs(inputs, sh, b) for b in range(8)]
    res = run_bass_kernel_spmd(nc, in_maps, core_ids=list(range(8)))
    return np.stack([np.asarray(res.results[b]["y"]) for b in range(8)], axis=0).astype(np.float32)
```
